# Optimizing a Trainium2 kernel written in Bass

```python
import jax
import jax.numpy as jnp
from jax import lax
import numpy as np

D_MODEL = 1024
BATCH = 32
SEQ = 2048
DEPTH = 2

CHUNK = 64
PLE_DIM = 256
NORM_EPS = 1e-6
N_EVEN = (DEPTH + 1) // 2
N_ODD = DEPTH // 2
A_HEADS = 8
A_HEAD_DIM = 64
A_KV_DIM = 64
A_WIDTH = A_HEADS * A_HEAD_DIM
IDX_HEADS = 4
IDX_DIM = 64
TOPK_MAX = 256
B_HEADS = 8
B_HEAD_DIM = 64
B_WIDTH = B_HEADS * B_HEAD_DIM
DECAY_LORA = 64
AAA_LORA = 64
GATE_LORA = 128
RWKV_GN_EPS = 64e-5
A_COLS = (A_WIDTH, A_KV_DIM, A_KV_DIM, IDX_HEADS * IDX_DIM, IDX_DIM, IDX_HEADS)
B_COLS = (B_WIDTH, B_WIDTH, B_WIDTH, DECAY_LORA, AAA_LORA, GATE_LORA)
A_TOTAL = sum(A_COLS)
B_TOTAL = sum(B_COLS)
MIX_WIDTH = A_WIDTH + B_WIDTH
C_INNER = 2 * D_MODEL
C_HEADS = 4
C_HEAD_DIM = C_INNER // C_HEADS
C_CONV = 4
C_QKV_BLOCK = 4
C_GN_EPS = 1e-5
FFN_HIDDEN = -(-(8 * D_MODEL) // (3 * 256)) * 256

kernel_name = "hybrid_dsa_rwkv7_mlstm_trunk"


def _split_cols(u, sizes):
    cuts = [int(c) for c in np.cumsum(sizes)[:-1]]
    return jnp.split(u, cuts, axis=-1)


def rms_norm(x, g, eps=NORM_EPS):
    xf = x.astype(jnp.float32)
    y = xf * lax.rsqrt(jnp.mean(xf * xf, axis=-1, keepdims=True) + eps)
    return (y * g).astype(x.dtype)


def head_norm(y, eps):
    y = y.astype(jnp.float32)
    mean = jnp.mean(y, axis=-1, keepdims=True)
    var = jnp.mean(jnp.square(y - mean), axis=-1, keepdims=True)
    return (y - mean) * lax.rsqrt(var + eps)


def token_shift(u, mu):
    prev = jnp.pad(u, ((0, 0), (1, 0), (0, 0)))[:, :-1]
    return u + mu * (prev - u)


def dsa_attention(q, k, v, iq, ik, iw, q_gain, k_gain):
    B_, S_ = q.shape[0], q.shape[1]
    n_chunks = S_ // CHUNK
    k_sel = min(TOPK_MAX, S_ // 4)
    q = rms_norm(q, q_gain)
    k = rms_norm(k, k_gain)
    iw = iw * (IDX_HEADS ** -0.5 * IDX_DIM ** -0.5)
    key_pos = jnp.arange(S_)

    def to_chunks(t):
        return jnp.moveaxis(t.reshape((B_, n_chunks, CHUNK) + t.shape[2:]), 1, 0)

    def one_chunk(args):
        c, qc, iqc, iwc = args
        limit = (c + 1) * CHUNK
        logits = jnp.einsum('bthd,bsd->bths', iqc, ik, preferred_element_type=jnp.float32)
        score = jnp.einsum('bth,bths->bts', iwc.astype(jnp.float32), jax.nn.relu(logits))
        score = jnp.where(key_pos[None, None, :] < limit, score, -jnp.inf)
        _, idx = lax.top_k(score, k_sel)
        valid = idx < limit
        kg = jax.vmap(lambda kb, ib: kb[ib])(k, idx)
        vg = jax.vmap(lambda vb, ib: vb[ib])(v, idx)
        s = jnp.einsum('bthd,btjd->bthj', qc, kg, preferred_element_type=jnp.float32) * (A_HEAD_DIM ** -0.5)
        s = jnp.where(valid[:, :, None, :], s, -jnp.inf)
        pr = jax.nn.softmax(s, axis=-1)
        return jnp.einsum('bthj,btjd->bthd', pr.astype(vg.dtype), vg)

    out = lax.map(one_chunk, (jnp.arange(n_chunks), to_chunks(q), to_chunks(iq), to_chunks(iw)))
    return jnp.moveaxis(out, 0, 1).reshape(B_, S_, A_WIDTH)


def rwkv7_time_mix(u, mu, w0, w2, a0, a2, g2, k_k, k_a, r_k, gn_g, gn_b):
    B_, S_ = u.shape[0], u.shape[1]
    u = token_shift(u, mu)
    r, k, v, xw, xa, xg = _split_cols(u, B_COLS)
    w_raw = -jax.nn.softplus(-(w0 + jnp.tanh(xw) @ w2)) - 0.5
    decay = jnp.exp(-jnp.exp(w_raw.astype(jnp.float32)))
    a = jax.nn.sigmoid(a0 + xa @ a2)
    g = jax.nn.sigmoid(xg) @ g2
    heads = lambda t: t.astype(jnp.float32).reshape(B_, S_, B_HEADS, B_HEAD_DIM)
    kk = heads(k * k_k)
    kk = kk / jnp.maximum(jnp.linalg.norm(kk, axis=-1, keepdims=True), 1e-12)
    k = k * (1.0 + (a - 1.0) * k_a)
    r_h, k_h, v_h, a_h, w_h = heads(r), heads(k), heads(v), heads(a), heads(decay)

    def step(state, inp):
        r_t, w_t, k_t, v_t, kk_t, a_t = inp
        sa = jnp.einsum('bhvk,bhk->bhv', state, -kk_t)
        state = (state * w_t[:, :, None, :] + sa[..., None] * (kk_t * a_t)[:, :, None, :]
                 + v_t[..., None] * k_t[:, :, None, :])
        return state, jnp.einsum('bhvk,bhk->bhv', state, r_t)

    xs = tuple(jnp.moveaxis(t, 1, 0) for t in (r_h, w_h, k_h, v_h, kk, a_h))
    state0 = jnp.zeros((B_, B_HEADS, B_HEAD_DIM, B_HEAD_DIM), jnp.float32)
    _, y = lax.scan(step, state0, xs)
    y = jnp.moveaxis(y, 0, 1)
    y = head_norm(y, RWKV_GN_EPS).reshape(B_, S_, B_WIDTH) * gn_g + gn_b
    bonus = jnp.sum(r_h * k_h * r_k, axis=-1, keepdims=True) * v_h
    y = (y + bonus.reshape(B_, S_, B_WIDTH)) * g
    return y.astype(u.dtype)


def mlstm_chunkwise(q, k, v, i_pre, f_pre):
    B_, S_, H, Dh = q.shape
    n_chunks = S_ // CHUNK
    f32 = jnp.float32
    cq = lambda t: t.astype(f32).reshape(B_, n_chunks, CHUNK, H, Dh).transpose(1, 0, 3, 2, 4)
    cg = lambda t: t.astype(f32).reshape(B_, n_chunks, CHUNK, H).transpose(1, 0, 3, 2)
    log_f = jax.nn.log_sigmoid(f_pre.astype(f32))
    tril = jnp.tril(jnp.ones((CHUNK, CHUNK), dtype=bool))

    def step(carry, inp):
        C, nv, m = carry
        qc, kc, vc, ic, lfc = inp
        b = jnp.cumsum(lfc, axis=-1)
        d = b[..., :, None] - b[..., None, :] + ic[..., None, :]
        d = jnp.where(tril, d, -jnp.inf)
        inter = b + m[..., None]
        m_t = jnp.maximum(inter, jnp.max(d, axis=-1))
        wts = jnp.exp(d - m_t[..., None])
        sc = jnp.exp(inter - m_t)
        qk = jnp.einsum('bhtd,bhsd->bhts', qc, kc) * wts
        num = jnp.einsum('bhts,bhsd->bhtd', qk, vc) + sc[..., None] * jnp.einsum('bhtd,bhde->bhte', qc, C)
        den = jnp.sum(qk, axis=-1) + sc * jnp.einsum('bhtd,bhd->bht', qc, nv)
        h = num / jnp.maximum(jnp.abs(den), jnp.exp(-m_t))[..., None]
        b_last = b[..., -1]
        g_s = b_last[..., None] - b + ic
        m_new = jnp.maximum(b_last + m, jnp.max(g_s, axis=-1))
        dc = jnp.exp(b_last + m - m_new)
        ws = jnp.exp(g_s - m_new[..., None])
        C = dc[..., None, None] * C + jnp.einsum('bhs,bhsd,bhse->bhde', ws, kc, vc)
        nv = dc[..., None] * nv + jnp.einsum('bhs,bhsd->bhd', ws, kc)
        return (C, nv, m_new), h

    carry0 = (jnp.zeros((B_, H, Dh, Dh), f32), jnp.zeros((B_, H, Dh), f32), jnp.zeros((B_, H), f32))
    _, hs = lax.scan(step, carry0, (cq(q), cq(k), cq(v), cg(i_pre), cg(log_f)))
    return hs.transpose(1, 0, 3, 2, 4).reshape(B_, S_, H, Dh)


def even_mixer(h, w_in, a_q_gain, a_k_gain, b_mu, b_w0, b_w2, b_a0, b_a2, b_g2, b_k_k, b_k_a,
               b_r_k, b_gn_g, b_gn_b, w_out):
    B_, S_ = h.shape[0], h.shape[1]
    u = h @ w_in
    qa, ka, va, iq, ik, iw = _split_cols(u[..., :A_TOTAL], A_COLS)
    ya = dsa_attention(qa.reshape(B_, S_, A_HEADS, A_HEAD_DIM), ka, va,
                       iq.reshape(B_, S_, IDX_HEADS, IDX_DIM), ik, iw, a_q_gain, a_k_gain)
    yb = rwkv7_time_mix(u[..., A_TOTAL:], b_mu, b_w0, b_w2, b_a0, b_a2, b_g2, b_k_k, b_k_a,
                        b_r_k, b_gn_g, b_gn_b)
    return jnp.concatenate([ya, yb.astype(ya.dtype)], axis=-1) @ w_out


def odd_mixer(h, w_up, conv_w, conv_b, wq, wk, wv, w_if, b_i, b_f, mh_g, skip, w_down):
    B_, S_ = h.shape[0], h.shape[1]
    xm, z = jnp.split(h @ w_up, 2, axis=-1)
    xc = lax.conv_general_dilated(xm, conv_w, (1,), [(C_CONV - 1, 0)],
                                  dimension_numbers=('NWC', 'WIO', 'NWC'),
                                  feature_group_count=C_INNER) + conv_b
    xc = jax.nn.silu(xc)

    def headwise(t, w):
        tb = t.reshape(B_, S_, C_INNER // C_QKV_BLOCK, C_QKV_BLOCK)
        return jnp.einsum('bsgi,gij->bsgj', tb, w).reshape(B_, S_, C_INNER)

    q = headwise(xc, wq)
    k = headwise(xc, wk)
    v = headwise(xm, wv)
    gates = jnp.concatenate([q, k, v], axis=-1) @ w_if
    i_pre = gates[..., :C_HEADS] + b_i
    f_pre = gates[..., C_HEADS:] + b_f
    hh = lambda t: t.reshape(B_, S_, C_HEADS, C_HEAD_DIM)
    y = mlstm_chunkwise(hh(q), hh(k) * (C_HEAD_DIM ** -0.5), hh(v), i_pre, f_pre)
    y = head_norm(y, C_GN_EPS).reshape(B_, S_, C_INNER) * mh_g
    y = (y + skip * xc) * jax.nn.silu(z)
    return y.astype(h.dtype) @ w_down


def swiglu_ffn(x, g, w_gate, w_up, w_down):
    h = rms_norm(x, g)
    return (jax.nn.silu(h @ w_gate) * (h @ w_up)) @ w_down


def per_layer_embedding(x, p_i, w_ple, g, w_gate):
    gate = jax.nn.sigmoid(rms_norm(x, g) @ w_gate)
    return (p_i @ w_ple) * gate


def setup_inputs(seed: int = 0) -> dict:
    key = jax.random.key(seed)
    ks = iter(jax.random.split(key, 40))
    nrm = lambda shape, scale: jax.random.normal(next(ks), shape, jnp.float32) * scale
    gain = lambda shape: 1.0 + 0.02 * jax.random.normal(next(ks), shape, jnp.float32)
    D = D_MODEL
    return {
        'x': nrm((BATCH, SEQ, D), 1.0),
        'p': nrm((DEPTH, BATCH, SEQ, PLE_DIM), 1.0),
        'mix_norm': gain((DEPTH, D)),
        'a_q_gain': gain((N_EVEN, A_HEAD_DIM)),
        'a_k_gain': gain((N_EVEN, A_KV_DIM)),
        'w_in_e': nrm((N_EVEN, D, A_TOTAL + B_TOTAL), D ** -0.5),
        'b_mu': jax.random.uniform(next(ks), (N_EVEN, B_TOTAL), jnp.float32),
        'b_w0': jax.random.uniform(next(ks), (N_EVEN, B_WIDTH), jnp.float32, minval=-5.0, maxval=0.5),
        'b_w2': nrm((N_EVEN, DECAY_LORA, B_WIDTH), 0.1),
        'b_a0': nrm((N_EVEN, B_WIDTH), 0.1),
        'b_a2': nrm((N_EVEN, AAA_LORA, B_WIDTH), 0.5 * AAA_LORA ** -0.5),
        'b_g2': nrm((N_EVEN, GATE_LORA, B_WIDTH), GATE_LORA ** -0.5),
        'b_k_k': 0.85 + 0.02 * jax.random.normal(next(ks), (N_EVEN, B_WIDTH), jnp.float32),
        'b_k_a': gain((N_EVEN, B_WIDTH)),
        'b_r_k': nrm((N_EVEN, B_HEADS, B_HEAD_DIM), 0.1),
        'b_gn_g': gain((N_EVEN, B_WIDTH)),
        'b_gn_b': nrm((N_EVEN, B_WIDTH), 0.01),
        'w_out_e': nrm((N_EVEN, MIX_WIDTH, D), MIX_WIDTH ** -0.5),
        'c_w_up': nrm((N_ODD, D, 2 * C_INNER), D ** -0.5),
        'c_conv_w': nrm((N_ODD, C_CONV, 1, C_INNER), C_CONV ** -0.5),
        'c_conv_b': nrm((N_ODD, C_INNER), 0.01),
        'c_wq': nrm((N_ODD, C_INNER // C_QKV_BLOCK, C_QKV_BLOCK, C_QKV_BLOCK), C_QKV_BLOCK ** -0.5),
        'c_wk': nrm((N_ODD, C_INNER // C_QKV_BLOCK, C_QKV_BLOCK, C_QKV_BLOCK), C_QKV_BLOCK ** -0.5),
        'c_wv': nrm((N_ODD, C_INNER // C_QKV_BLOCK, C_QKV_BLOCK, C_QKV_BLOCK), C_QKV_BLOCK ** -0.5),
        'c_w_if': nrm((N_ODD, 3 * C_INNER, 2 * C_HEADS), 0.5 * (3 * C_INNER) ** -0.5),
        'c_b_i': nrm((N_ODD, C_HEADS), 0.1),
        'c_b_f': jnp.linspace(3.0, 6.0, C_HEADS)[None, :] + nrm((N_ODD, C_HEADS), 0.1),
        'c_mh_g': gain((N_ODD, C_INNER)),
        'c_skip': gain((N_ODD, C_INNER)),
        'c_w_down': nrm((N_ODD, C_INNER, D), C_INNER ** -0.5),
        'ffn_norm': gain((DEPTH, D)),
        'ffn_w_gate': nrm((DEPTH, D, FFN_HIDDEN), D ** -0.5),
        'ffn_w_up': nrm((DEPTH, D, FFN_HIDDEN), D ** -0.5),
        'ffn_w_down': nrm((DEPTH, FFN_HIDDEN, D), FFN_HIDDEN ** -0.5),
        'ple_w': nrm((DEPTH, PLE_DIM, D), PLE_DIM ** -0.5),
        'ple_norm': gain((DEPTH, D)),
        'ple_w_gate': nrm((DEPTH, D, D), D ** -0.5),
    }


def reference(x, p, mix_norm, a_q_gain, a_k_gain, w_in_e, b_mu, b_w0, b_w2, b_a0, b_a2, b_g2,
              b_k_k, b_k_a, b_r_k, b_gn_g, b_gn_b, w_out_e, c_w_up, c_conv_w, c_conv_b, c_wq,
              c_wk, c_wv, c_w_if, c_b_i, c_b_f, c_mh_g, c_skip, c_w_down, ffn_norm, ffn_w_gate,
              ffn_w_up, ffn_w_down, ple_w, ple_norm, ple_w_gate):
    for i in range(DEPTH):
        j = i // 2
        h = rms_norm(x, mix_norm[i])
        if i % 2 == 0:
            x = x + even_mixer(h, w_in_e[j], a_q_gain[j], a_k_gain[j], b_mu[j], b_w0[j], b_w2[j],
                               b_a0[j], b_a2[j], b_g2[j], b_k_k[j], b_k_a[j], b_r_k[j],
                               b_gn_g[j], b_gn_b[j], w_out_e[j])
        else:
            x = x + odd_mixer(h, c_w_up[j], c_conv_w[j], c_conv_b[j], c_wq[j], c_wk[j], c_wv[j],
                              c_w_if[j], c_b_i[j], c_b_f[j], c_mh_g[j], c_skip[j], c_w_down[j])
        x = x + swiglu_ffn(x, ffn_norm[i], ffn_w_gate[i], ffn_w_up[i], ffn_w_down[i])
        x = x + per_layer_embedding(x, p[i], ple_w[i], ple_norm[i], ple_w_gate[i])
    return x
```

```python
import contextlib
import numpy as np
import ml_dtypes
import concourse.bass as bass
import concourse.mybir as mybir
from concourse.bass_utils import run_bass_kernel_spmd

F32 = mybir.dt.float32
BF16 = mybir.dt.bfloat16
AF = mybir.ActivationFunctionType
ALU = mybir.AluOpType
AX = mybir.AxisListType

D = 1024
NCH = D // 128
SEQ = 2048
BATCH = 32
NCORE = 8
CHUNK = 64
PLE_DIM = 256
FFN_H = 2816
NHC = FFN_H // 128
NORM_EPS = 1e-6


class Sem:
    def __init__(self, handle, name, is_dma=False):
        self.h = handle
        self.name = name
        self.is_dma = is_dma


class Buf:
    def __init__(self, t, name):
        self.t = t
        self.name = name
        self.w = {}
        self.r = {}

    def __getitem__(self, key):
        return self.t[key]


class Eng:
    def __init__(self, name, h, sem):
        self.name = name
        self.h = h
        self.sem = sem
        self.cnt = 0
        self.seen = {}


class K:
    def __init__(self, nc, n_dma_sems=32):
        self.nc = nc
        self.es = contextlib.ExitStack()
        self.eng = {}
        for name, h in [("pe", nc.tensor), ("act", nc.scalar), ("dve", nc.vector),
                        ("pool", nc.gpsimd), ("sp", nc.sync)]:
            self.eng[name] = Eng(name, h, self.new_sem("e_" + name))
        self.dsems = [[self.new_sem(f"d{i}", True), 0] for i in range(n_dma_sems)]
        self.dnext = 0
        self.nbuf = 0

    def new_sem(self, name, is_dma=False):
        return Sem(self.es.enter_context(self.nc.semaphore(name)), name, is_dma)

    def sbuf(self, shape, dtype, name=None, scope=None):
        self.nbuf += 1
        name = f"{name or 'sb'}_{self.nbuf}"
        t = (scope or self.es).enter_context(self.nc.sbuf_tensor(name, list(shape), dtype))
        return Buf(t, name)

    def psum(self, shape, dtype, name=None, scope=None):
        self.nbuf += 1
        name = f"{name or 'ps'}_{self.nbuf}"
        t = (scope or self.es).enter_context(self.nc.psum_tensor(name, list(shape), dtype))
        return Buf(t, name)

    def dram(self, shape, dtype, name, kind="Internal"):
        t = self.nc.dram_tensor(name, list(shape), dtype, kind=kind)
        return Buf(t.ap(), name)

    def _deps(self, e, R, W, skip_self):
        need = {}

        def add(s, v):
            if need.get(s, 0) < v:
                need[s] = v

        for b in R:
            for s, v in b.w.items():
                add(s, v)
        for b in W:
            for s, v in b.w.items():
                add(s, v)
            for s, v in b.r.items():
                add(s, v)
        for s, v in need.items():
            if skip_self and s is e.sem:
                continue
            if e.seen.get(s, 0) >= v:
                continue
            e.h.wait_ge(s.h, v)
            e.seen[s] = v

    def _mark(self, ev, R, W, dma=False):
        s, v = ev
        for b in R:
            if b.r.get(s, 0) < v:
                b.r[s] = v
        for b in W:
            if dma:
                b.w = {ss: vv for ss, vv in b.w.items() if ss.is_dma}
                b.w[s] = v
            else:
                b.w = {s: v}
            b.r = {}

    def barrier(self):
        for e in self.eng.values():
            for o in self.eng.values():
                if o is e or o.cnt == 0:
                    continue
                if e.seen.get(o.sem, 0) < o.cnt:
                    e.h.wait_ge(o.sem.h, o.cnt)
                    e.seen[o.sem] = o.cnt
            for s, v in self.dsems:
                if v > 0 and e.seen.get(s, 0) < v:
                    e.h.wait_ge(s.h, v)
                    e.seen[s] = v

    def op(self, en, fn, R=(), W=()):
        e = self.eng[en]
        self._deps(e, R, W, skip_self=(en == "pe"))
        ins = fn(e.h)
        e.cnt += 1
        ins.then_inc(e.sem.h, 1)
        self._mark((e.sem, e.cnt), R, W)

    def dma(self, en, out, in_, R=(), W=(), **kw):
        e = self.eng[en]
        self._deps(e, R, W, skip_self=False)
        slot = self.dsems[self.dnext]
        self.dnext = (self.dnext + 1) % len(self.dsems)
        s, v = slot
        if v > 0 and e.seen.get(s, 0) < v:
            e.h.wait_ge(s.h, v)
            e.seen[s] = v
        e.h.dma_start(out=out, in_=in_, **kw).then_inc(s.h, 16)
        slot[1] = v + 16
        self._mark((s, v + 16), R, W, dma=True)

    def wait_all(self, en, bufs):
        e = self.eng[en]
        self._deps(e, bufs, (), skip_self=False)

    def close(self):
        self.es.close()


class Prog:
    def __init__(self, ntok, layers=(0, 1), stages=("mix", "ffn"), dbg=False):
        self.ntok = ntok
        self.layers = layers
        self.stages = stages
        nc = bass.Bass("TRN2", target_bir_lowering=False)
        self.nc = nc
        self.k = K(nc)
        self.inputs = {}
        self.dbg = dbg
        self.dumps = {}
        import os
        self.upto = os.environ.get('KUPTO', '')
        self.skip = os.environ.get('KSKIP', '')

    def dump(self, name, buf, ap, shape, dtype=F32):
        if not self.dbg or name in self.dumps:
            return
        t = self.nc.dram_tensor("dbg_" + name, list(shape), dtype, kind="ExternalOutput")
        b = Buf(t.ap(), "dbg_" + name)
        self.dumps[name] = b
        self.k.dma("sp", b[:], ap, R=[buf], W=[b])

    def inp(self, name, shape, dtype=F32):
        if name in self.inputs:
            return self.inputs[name]
        t = self.nc.dram_tensor(name, list(shape), dtype, kind="ExternalInput")
        b = Buf(t.ap(), name)
        self.inputs[name] = b
        return b

    def setup_consts(self):
        k = self.k
        self.c_ident_f = k.sbuf([128, 128], F32, "identf")
        self.c_ident_b = k.sbuf([128, 128], BF16, "identb")
        self.c_mean = k.sbuf([128, 128], BF16, "meanmat")
        cid = self.inp("c_ident", [128, 128])
        k.dma("sp", self.c_ident_f[:], cid[:], R=[cid], W=[self.c_ident_f])
        k.op("dve", lambda e: e.tensor_copy(out=self.c_ident_b[:], in_=self.c_ident_f[:]),
             R=[self.c_ident_f], W=[self.c_ident_b])
        k.op("dve", lambda e: e.memset(self.c_mean[:], 1.0 / D), W=[self.c_mean])
        self.c_eps = k.sbuf([128, 4], F32, "ceps")
        k.op("dve", lambda e: e.memset(self.c_eps[:, 0:1], NORM_EPS), W=[self.c_eps])
        self.ps = [k.psum([128, 512], F32, f"bank{i}") for i in range(8)]
        self.psi = 0
        self.ps_rot = list(range(8))

    def bank(self):
        b = self.ps[self.ps_rot[self.psi % len(self.ps_rot)]]
        self.psi += 1
        return b

    def conv_weight(self, w_ap, Kdim, N, name, stage_bufs):
        k = self.k
        kc = Kdim // 128
        dst = k.dram([128, kc, N], BF16, name)
        src = w_ap.rearrange("(c p) n -> p c n", p=128)
        step = max(1, 4096 // kc)
        i = 0
        for n0 in range(0, N, step):
            n1 = min(N, n0 + step)
            st = stage_bufs[i % 2]
            i += 1
            k.dma("pool", st[:, 0:kc, 0:n1 - n0], src[:, :, n0:n1], W=[st])
            k.dma("sp", dst[:, :, n0:n1], st[:, 0:kc, 0:n1 - n0], R=[st], W=[dst])
        return dst

    def rmsnorm(self, xs, gcol, outs, n, tmp_sq, tmp_rstd, eps=NORM_EPS):
        k = self.k
        ps = self.bank()
        for c in range(NCH):
            xb, xa = xs[c]
            k.op("act", lambda e, xa=xa, c=c: e.activation(out=tmp_sq[:, c, 0:n], in_=xa, func=AF.Square),
                 R=[xb], W=[tmp_sq])
        for c in range(NCH):
            k.op("pe", lambda e, c=c: e.matmul(ps[:, 0:n], lhsT=self.c_mean[:], rhs=tmp_sq[:, c, 0:n],
                                               start=(c == 0), stop=(c == NCH - 1)),
                 R=[self.c_mean, tmp_sq], W=[ps])
        k.op("act", lambda e: e.activation(out=tmp_rstd[:, 0:n], in_=ps[:, 0:n], func=AF.Sqrt, bias=self.c_eps[:, 0:1],
                                           scale=1.0), R=[ps, self.c_eps], W=[tmp_rstd])
        k.op("dve", lambda e: e.reciprocal(out=tmp_rstd[:, 0:n], in_=tmp_rstd[:, 0:n]), R=[tmp_rstd], W=[tmp_rstd])
        self.dump("sq", tmp_sq, tmp_sq[:], [128, NCH, 512], BF16)
        self.dump("rstd", tmp_rstd, tmp_rstd[:], [128, 512], F32)
        gb, ga = gcol
        self.dump("gcol", gb, ga, [128, NCH], F32)
        for c in range(NCH):
            xb, xa = xs[c]
            ob, oa = outs[c]
            k.op("dve", lambda e, xa=xa, oa=oa, c=c: e.scalar_tensor_tensor(
                out=oa, in0=xa, scalar=ga[:, c:c + 1], in1=tmp_rstd[:, 0:n], op0=ALU.mult, op1=ALU.mult),
                R=[xb, gb, tmp_rstd], W=[ob])

    def phase_in(self, x_in):
        k = self.k
        ntok = self.ntok
        self.xT = k.dram([128, NCH, ntok], F32, "xT_scr")
        with contextlib.ExitStack() as sc:
            xin = [k.sbuf([128, 4, D], F32, "xin", sc) for _ in range(2)]
            xo = [k.sbuf([128, NCH, 512], F32, "xo", sc) for _ in range(2)]
            xv = x_in.t.rearrange("(g j p) d -> g p j d", p=128, j=4)
            for g in range(ntok // 512):
                a = xin[g % 2]
                o = xo[g % 2]
                k.dma("sp", a[:], xv[g], R=[x_in], W=[a])
                for c in range(NCH):
                    ps = self.bank()
                    for j in range(4):
                        k.op("pe", lambda e, c=c, j=j, ps=ps: e.transpose(
                            ps[:, j * 128:(j + 1) * 128], a[:, j, c * 128:(c + 1) * 128], self.c_ident_f[:]),
                            R=[a, self.c_ident_f], W=[ps])
                    en = "act" if c % 2 else "dve"
                    if en == "act":
                        k.op("act", lambda e, c=c, ps=ps: e.copy(out=o[:, c, :], in_=ps[:]), R=[ps], W=[o])
                    else:
                        k.op("dve", lambda e, c=c, ps=ps: e.tensor_copy(out=o[:, c, :], in_=ps[:]), R=[ps], W=[o])
                k.dma("sp", self.xT[:, :, g * 512:(g + 1) * 512], o[:], R=[o], W=[self.xT])
        k.barrier()

    def phase_out(self, y_out):
        k = self.k
        ntok = self.ntok
        with contextlib.ExitStack() as sc:
            xi = [k.sbuf([128, NCH, 512], F32, "oxi", sc) for _ in range(2)]
            xo = [k.sbuf([128, 4, D], F32, "oxo", sc) for _ in range(2)]
            yv = y_out.t.rearrange("(g j p) d -> g p j d", p=128, j=4)
            for g in range(ntok // 512):
                a = xi[g % 2]
                o = xo[g % 2]
                k.dma("sp", a[:], self.xT[:, :, g * 512:(g + 1) * 512], R=[self.xT], W=[a])
                for j in range(4):
                    for ch in range(2):
                        ps = self.bank()
                        for cc in range(4):
                            c = ch * 4 + cc
                            k.op("pe", lambda e, c=c, cc=cc, j=j, ps=ps: e.transpose(
                                ps[:, cc * 128:(cc + 1) * 128], a[:, c, j * 128:(j + 1) * 128], self.c_ident_f[:]),
                                R=[a, self.c_ident_f], W=[ps])
                        if ch:
                            k.op("act", lambda e, j=j, ch=ch, ps=ps: e.copy(
                                out=o[:, j, ch * 512:(ch + 1) * 512], in_=ps[:]), R=[ps], W=[o])
                        else:
                            k.op("dve", lambda e, j=j, ch=ch, ps=ps: e.tensor_copy(
                                out=o[:, j, ch * 512:(ch + 1) * 512], in_=ps[:]), R=[ps], W=[o])
                k.dma("sp", yv[g], o[:], R=[o], W=[y_out])
        k.barrier()

    def phase_ffn(self, li, W):
        k = self.k
        ntok = self.ntok
        TB = 512
        with contextlib.ExitStack() as sc:
            stage = [k.sbuf([128, 8, 512], BF16, "cst", sc) for _ in range(2)]
            wg_s = self.conv_weight(W["ffn_w_gate"].t[li], D, FFN_H, f"wg_s{li}", stage)
            wu_s = self.conv_weight(W["ffn_w_up"].t[li], D, FFN_H, f"wu_s{li}", stage)
            wd = k.sbuf([128, NHC, D], BF16, "wd", sc)
            wple = k.sbuf([128, 2, D], BF16, "wple", sc)
            wpg = k.sbuf([128, NCH, D], BF16, "wpg", sc)
            k.dma("pool", wd[:], W["ffn_w_down"].t[li].rearrange("(c p) n -> p c n", p=128),
                  R=[W["ffn_w_down"]], W=[wd])
            k.dma("pool", wple[:], W["ple_w"].t[li].rearrange("(c p) n -> p c n", p=128),
                  R=[W["ple_w"]], W=[wple])
            k.dma("pool", wpg[:], W["ple_w_gate"].t[li].rearrange("(c p) n -> p c n", p=128),
                  R=[W["ple_w_gate"]], W=[wpg])
            gcol = k.sbuf([128, 2, NCH], F32, "gcol", sc)
            k.dma("sp", gcol[:, 0, :], W["ffn_norm"].t[li].rearrange("(c p) -> p c", p=128),
                  R=[W["ffn_norm"]], W=[gcol], allow_slow_non_contiguous=True)
            k.dma("sp", gcol[:, 1, :], W["ple_norm"].t[li].rearrange("(c p) -> p c", p=128),
                  R=[W["ple_norm"]], W=[gcol], allow_slow_non_contiguous=True)
            xb = [k.sbuf([128, NCH, TB], F32, "fx", sc) for _ in range(2)]
            hT = k.sbuf([128, NCH, TB], BF16, "fh", sc)
            sq = k.sbuf([128, NCH, TB], BF16, "fsq", sc)
            rstd = k.sbuf([128, TB], F32, "frstd", sc)
            act = k.sbuf([128, NHC, TB], BF16, "fact", sc)
            wgb = [k.sbuf([128, NCH, 512], BF16, "wgb", sc) for _ in range(2)]
            wub = [k.sbuf([128, NCH, 512], BF16, "wub", sc) for _ in range(2)]
            sg = [k.sbuf([128, TB], F32, "fsg", sc) for _ in range(2)]
            pin = k.sbuf([128, 4, PLE_DIM], F32, "pin", sc)
            pT = k.sbuf([128, 2, TB], BF16, "pT", sc)
            pe_sb = k.sbuf([128, TB], F32, "pesb", sc)
            p_in = W["p"]
            pv = p_in.t[li].rearrange("(g j p) d -> g p j d", p=128, j=4)
            ngrp = (NHC + 3) // 4
            it = 0
            for tb in range(ntok // TB):
                x = xb[tb % 2]
                k.dma("sp", x[:], self.xT[:, :, tb * TB:(tb + 1) * TB], R=[self.xT], W=[x])
                k.dma("sp", pin[:], pv[tb], R=[p_in], W=[pin])
                self.dump(f"xl{li}", x, x[:], [128, NCH, TB], F32)
                self.rmsnorm([(x, x[:, c, :]) for c in range(NCH)], (gcol, gcol[:, 0, :]),
                             [(hT, hT[:, c, :]) for c in range(NCH)], TB, sq, rstd)
                self.dump(f"hT{li}", hT, hT[:], [128, NCH, TB], BF16)
                for jg in range(ngrp):
                    ncol = min(512, FFN_H - jg * 512)
                    wgt, wut = wgb[it % 2], wub[it % 2]
                    it += 1
                    k.dma("sp", wgt[:, :, 0:ncol], wg_s[:, :, jg * 512:jg * 512 + ncol], R=[wg_s], W=[wgt])
                    k.dma("sp", wut[:, :, 0:ncol], wu_s[:, :, jg * 512:jg * 512 + ncol], R=[wu_s], W=[wut])
                    for jj in range(ncol // 128):
                        j = jg * 4 + jj
                        pg, pu = self.bank(), self.bank()
                        for c in range(NCH):
                            k.op("pe", lambda e, c=c, jj=jj, pg=pg, wgt=wgt: e.matmul(
                                pg[:], lhsT=wgt[:, c, jj * 128:(jj + 1) * 128], rhs=hT[:, c, :],
                                start=(c == 0), stop=(c == NCH - 1)), R=[wgt, hT], W=[pg])
                        for c in range(NCH):
                            k.op("pe", lambda e, c=c, jj=jj, pu=pu, wut=wut: e.matmul(
                                pu[:], lhsT=wut[:, c, jj * 128:(jj + 1) * 128], rhs=hT[:, c, :],
                                start=(c == 0), stop=(c == NCH - 1)), R=[wut, hT], W=[pu])
                        s = sg[j % 2]
                        k.op("act", lambda e, s=s, pg=pg: e.activation(out=s[:], in_=pg[:], func=AF.Silu),
                             R=[pg], W=[s])
                        k.op("dve", lambda e, s=s, pu=pu, j=j: e.tensor_tensor(
                            out=act[:, j, :], in0=s[:], in1=pu[:], op=ALU.mult), R=[s, pu], W=[act])
                self.dump(f"act{li}", act, act[:], [128, NHC, TB], BF16)
                for n in range(NCH):
                    po = self.bank()
                    for j in range(NHC):
                        k.op("pe", lambda e, n=n, j=j, po=po: e.matmul(
                            po[:], lhsT=wd[:, j, n * 128:(n + 1) * 128], rhs=act[:, j, :],
                            start=(j == 0), stop=(j == NHC - 1)), R=[wd, act], W=[po])
                    k.op("dve", lambda e, n=n, po=po, x=x: e.tensor_tensor(
                        out=x[:, n, :], in0=x[:, n, :], in1=po[:], op=ALU.add), R=[po, x], W=[x])
                self.dump(f"xf{li}", x, x[:], [128, NCH, TB], F32)
                for j in range(4):
                    ps = self.bank()
                    for c2 in range(2):
                        k.op("pe", lambda e, j=j, c2=c2, ps=ps: e.transpose(
                            ps[:, c2 * 128:(c2 + 1) * 128], pin[:, j, c2 * 128:(c2 + 1) * 128], self.c_ident_f[:]),
                            R=[pin, self.c_ident_f], W=[ps])
                    k.op("act", lambda e, j=j, ps=ps: e.copy(
                        out=pT[:, :, j * 128:(j + 1) * 128],
                        in_=ps[:, 0:256].rearrange("p (c t) -> p c t", c=2)), R=[ps], W=[pT])
                self.rmsnorm([(x, x[:, c, :]) for c in range(NCH)], (gcol, gcol[:, 1, :]),
                             [(hT, hT[:, c, :]) for c in range(NCH)], TB, sq, rstd)
                for n in range(NCH):
                    pg, pp = self.bank(), self.bank()
                    for c in range(NCH):
                        k.op("pe", lambda e, c=c, n=n, pg=pg: e.matmul(
                            pg[:], lhsT=wpg[:, c, n * 128:(n + 1) * 128], rhs=hT[:, c, :],
                            start=(c == 0), stop=(c == NCH - 1)), R=[wpg, hT], W=[pg])
                    for c in range(2):
                        k.op("pe", lambda e, c=c, n=n, pp=pp: e.matmul(
                            pp[:], lhsT=wple[:, c, n * 128:(n + 1) * 128], rhs=pT[:, c, :],
                            start=(c == 0), stop=(c == 1)), R=[wple, pT], W=[pp])
                    s = sg[n % 2]
                    k.op("act", lambda e, s=s, pg=pg: e.activation(out=s[:], in_=pg[:], func=AF.Sigmoid),
                         R=[pg], W=[s])
                    k.op("dve", lambda e, s=s, pp=pp: e.tensor_tensor(
                        out=pe_sb[:], in0=s[:], in1=pp[:], op=ALU.mult), R=[s, pp], W=[pe_sb])
                    k.op("dve", lambda e, n=n, x=x: e.tensor_tensor(
                        out=x[:, n, :], in0=x[:, n, :], in1=pe_sb[:], op=ALU.add), R=[pe_sb, x], W=[x])
                k.dma("sp", self.xT[:, :, tb * TB:(tb + 1) * TB], x[:], R=[x], W=[self.xT])
        k.barrier()


    def bank_rot(self, idxs):
        self.ps_rot = list(idxs)
        self.psi = 0

    def phase_mix0(self, W):
        k = self.k
        ntok = self.ntok
        T = min(SEQ, ntok)
        nseq = ntok // T
        NQT = T // 128
        NB = 14
        BBASE = 964
        qn_scr = k.dram([128, 4, ntok], BF16, "qn_scr")
        kn_scr = k.dram([128, ntok], BF16, "kn_scr")
        iq_scr = k.dram([128, 2, ntok], BF16, "iq_scr")
        ik_scr = k.dram([128, ntok], BF16, "ik_scr")
        va_scr = k.dram([ntok, 65], BF16, "va_scr")
        iw_scr = k.dram([ntok, 4], F32, "iw_scr")
        ub_scr = k.dram([128, NB, nseq, T + 4], F32, "ub_scr")
        ya_scr = k.dram([128, 4, ntok], BF16, "ya_scr")
        yb_scr = k.dram([128, 4, ntok], BF16, "yb_scr")
        self.yb_scr = yb_scr
        with contextlib.ExitStack() as sc:
            stage = [k.sbuf([128, 8, 512], BF16, "cst", sc) for _ in range(2)]
            win_s = self.conv_weight(W["w_in_e"].t[0], D, 2756, "win_s", stage)
            win = k.sbuf([128, NCH, 2756], BF16, "win", sc)
            k.dma("sp", win[:], win_s[:], R=[win_s], W=[win])
            gcol = k.sbuf([128, NCH], F32, "gcol", sc)
            k.dma("sp", gcol[:], W["mix_norm"].t[0].rearrange("(c p) -> p c", p=128),
                  R=[W["mix_norm"]], W=[gcol], allow_slow_non_contiguous=True)
            gqk = k.sbuf([128, 2], F32, "gqk", sc)
            for half in range(2):
                k.dma("sp", gqk[half * 64:(half + 1) * 64, 0:1], W["a_q_gain"].t[0].rearrange("(p o) -> p o", o=1),
                      R=[W["a_q_gain"]], W=[gqk])
                k.dma("sp", gqk[half * 64:(half + 1) * 64, 1:2], W["a_k_gain"].t[0].rearrange("(p o) -> p o", o=1),
                      R=[W["a_k_gain"]], W=[gqk])
            k.op("dve", lambda e: e.tensor_scalar(out=gqk[:, 0:1], in0=gqk[:, 0:1], scalar1=0.125, scalar2=None,
                                                  op0=ALU.mult), R=[gqk], W=[gqk])
            bd64f = k.sbuf([128, 128], F32, "bd64f", sc)
            bd64 = k.sbuf([128, 128], BF16, "bd64", sc)
            cb64 = self.inp("c_bd64", [128, 128])
            k.dma("sp", bd64f[:], cb64[:], R=[cb64], W=[bd64f])
            k.op("dve", lambda e: e.tensor_copy(out=bd64[:], in_=bd64f[:]), R=[bd64f], W=[bd64])
            zt = k.sbuf([128, NB, 4], F32, "zt", sc)
            k.op("dve", lambda e: e.memset(zt[:], 0.0), W=[zt])
            for s_ in range(nseq):
                k.dma("sp", ub_scr[:, :, s_, 0:4], zt[:], R=[zt], W=[ub_scr])
            xb = [k.sbuf([128, NCH, 512], F32, "a1x", sc) for _ in range(2)]
            hT = k.sbuf([128, NCH, 512], BF16, "a1h", sc)
            sq = k.sbuf([128, NCH, 512], BF16, "a1sq", sc)
            rstd = k.sbuf([128, 512], F32, "a1rstd", sc)
            qsq = k.sbuf([128, 512], BF16, "qsq", sc)
            qr = k.sbuf([128, 512], F32, "qr", sc)
            qno = k.sbuf([128, 4, 512], BF16, "qno", sc)
            kno = k.sbuf([128, 512], BF16, "kno", sc)
            iqo = k.sbuf([128, 2, 512], BF16, "iqo", sc)
            iko = k.sbuf([128, 512], BF16, "iko", sc)
            ubo = k.sbuf([128, NB, 512], F32, "ubo", sc)
            vao = k.sbuf([128, 4, 65], BF16, "vao", sc)
            iwo = k.sbuf([128, 4, 4], F32, "iwo", sc)
            k.op("dve", lambda e: e.memset(vao[:], 1.0), W=[vao])

            def proj(ps, col0, ncol, po=0):
                for c in range(NCH):
                    k.op("pe", lambda e, c=c: e.matmul(ps[po:po + ncol, :], lhsT=win[:, c, col0:col0 + ncol],
                                                      rhs=hT[:, c, :], start=(c == 0), stop=(c == NCH - 1)),
                         R=[win, hT], W=[ps])

            def qknorm(ps, gi, out_ap, out_buf):
                k.op("act", lambda e: e.activation(out=qsq[:], in_=ps[:], func=AF.Square), R=[ps], W=[qsq])
                ps2 = self.bank()
                k.op("pe", lambda e: e.matmul(ps2[:], lhsT=bd64[:], rhs=qsq[:], start=True, stop=True),
                     R=[bd64, qsq], W=[ps2])
                k.op("act", lambda e: e.activation(out=qr[:], in_=ps2[:], func=AF.Sqrt, bias=self.c_eps[:, 0:1],
                                                   scale=1.0), R=[ps2, self.c_eps], W=[qr])
                k.op("dve", lambda e: e.reciprocal(out=qr[:], in_=qr[:]), R=[qr], W=[qr])
                k.op("dve", lambda e: e.scalar_tensor_tensor(out=out_ap, in0=ps[:], scalar=gqk[:, gi:gi + 1],
                                                             in1=qr[:], op0=ALU.mult, op1=ALU.mult),
                     R=[ps, gqk, qr], W=[out_buf])

            for tb in range(ntok // 512):
                s_, off = (tb * 512) // T, (tb * 512) % T
                x = xb[tb % 2]
                k.dma("sp", x[:], self.xT[:, :, tb * 512:(tb + 1) * 512], R=[self.xT], W=[x])
                self.rmsnorm([(x, x[:, c, :]) for c in range(NCH)], (gcol, gcol[:, :]),
                             [(hT, hT[:, c, :]) for c in range(NCH)], 512, sq, rstd)
                for j in range(4):
                    ps = self.bank()
                    proj(ps, j * 128, 128)
                    qknorm(ps, 0, qno[:, j, :], qno)
                ps = self.bank()
                proj(ps, 512, 64, 0)
                proj(ps, 512, 64, 64)
                qknorm(ps, 1, kno[:], kno)
                for j in range(2):
                    ps = self.bank()
                    proj(ps, 640 + j * 128, 128)
                    k.op("act", lambda e, j=j, ps=ps: e.copy(out=iqo[:, j, :], in_=ps[:]), R=[ps], W=[iqo])
                ps = self.bank()
                proj(ps, 896, 64, 0)
                proj(ps, 896, 64, 64)
                k.op("act", lambda e, ps=ps: e.copy(out=iko[:], in_=ps[:]), R=[ps], W=[iko])
                for g in range(NB):
                    ps = self.bank()
                    proj(ps, BBASE + g * 128, 128)
                    if g % 2:
                        k.op("act", lambda e, g=g, ps=ps: e.copy(out=ubo[:, g, :], in_=ps[:]), R=[ps], W=[ubo])
                    else:
                        k.op("dve", lambda e, g=g, ps=ps: e.tensor_copy(out=ubo[:, g, :], in_=ps[:]), R=[ps], W=[ubo])
                for tt in range(4):
                    ps = self.bank()
                    for c in range(NCH):
                        k.op("pe", lambda e, c=c, tt=tt, ps=ps: e.matmul(
                            ps[:, 0:64], lhsT=hT[:, c, tt * 128:(tt + 1) * 128], rhs=win[:, c, 576:640],
                            start=(c == 0), stop=(c == NCH - 1)), R=[hT, win], W=[ps])
                    for c in range(NCH):
                        k.op("pe", lambda e, c=c, tt=tt, ps=ps: e.matmul(
                            ps[:, 64:68], lhsT=hT[:, c, tt * 128:(tt + 1) * 128], rhs=win[:, c, 960:964],
                            start=(c == 0), stop=(c == NCH - 1)), R=[hT, win], W=[ps])
                    k.op("act", lambda e, tt=tt, ps=ps: e.copy(out=vao[:, tt, 0:64], in_=ps[:, 0:64]), R=[ps], W=[vao])
                    k.op("act", lambda e, tt=tt, ps=ps: e.copy(out=iwo[:, tt, :], in_=ps[:, 64:68]), R=[ps], W=[iwo])
                tsl = slice(tb * 512, (tb + 1) * 512)
                k.dma("sp", qn_scr[:, :, tsl], qno[:], R=[qno], W=[qn_scr])
                k.dma("sp", kn_scr[:, tsl], kno[:], R=[kno], W=[kn_scr])
                k.dma("sp", iq_scr[:, :, tsl], iqo[:], R=[iqo], W=[iq_scr])
                k.dma("sp", ik_scr[:, tsl], iko[:], R=[iko], W=[ik_scr])
                k.dma("sp", ub_scr[:, :, s_, 4 + off:4 + off + 512], ubo[:], R=[ubo], W=[ub_scr])
                k.dma("sp", va_scr[tsl, :].rearrange("(j p) n -> p j n", p=128), vao[:], R=[vao], W=[va_scr])
                k.dma("sp", iw_scr[tsl, :].rearrange("(j p) n -> p j n", p=128), iwo[:], R=[iwo], W=[iw_scr])
        k.barrier()
        if self.upto == "A1":
            return
        if "rwkv" not in self.skip:
            self.phase_rwkv(W, ub_scr, yb_scr)
        with contextlib.ExitStack() as sc:
            NBK = T // 128
            KSEL = min(256, T // 4)
            qn = k.sbuf([128, 4, T], BF16, "qn", sc)
            kn = k.sbuf([128, T], BF16, "kn", sc)
            iq = k.sbuf([128, 2, T], BF16, "iq", sc)
            ik = k.sbuf([128, T], BF16, "ik", sc)
            va = k.sbuf([128, NBK, 66], BF16, "va", sc)
            iw = k.sbuf([128, NBK, 4], F32, "iw", sc)
            score = k.sbuf([128, T], F32, "score", sc)
            tmp = [k.sbuf([128, 512], F32, "stmp", sc) for _ in range(2)]
            junk = k.sbuf([128, T], BF16, "sjunk", sc)
            maskf = k.sbuf([128, T], F32, "maskf", sc)
            maskT = k.sbuf([128, NBK, 128], BF16, "maskT", sc)
            mneg2 = k.sbuf([128, 128], F32, "mneg2", sc)
            cm2 = self.inp("c_mneg2", [128, 128])
            k.dma("sp", mneg2[:], cm2[:], R=[cm2], W=[mneg2])
            bs = k.sbuf([128, 8], F32, "bs", sc)
            psb = [k.sbuf([128, 512], BF16, "psb", sc) for _ in range(2)]
            pm = [k.sbuf([128, 4, 128], BF16, "pm", sc) for _ in range(2)]
            ya = k.sbuf([128, 512], F32, "ya", sc)
            yaT = k.sbuf([128, 4, 128], BF16, "yaT", sc)
            rec = k.sbuf([128, 8], F32, "rec", sc)
            po = [self.ps[0], self.ps[1]]
            self.bank_rot(range(2, 8))
            k.op("dve", lambda e: e.memset(va[:], 0.0), W=[va])
            for s_ in range(nseq):
                ssl = slice(s_ * T, (s_ + 1) * T)
                k.dma("sp", qn[:], qn_scr[:, :, ssl], R=[qn_scr], W=[qn])
                k.dma("sp", kn[:], kn_scr[:, ssl], R=[kn_scr], W=[kn])
                k.dma("sp", iq[:], iq_scr[:, :, ssl], R=[iq_scr], W=[iq])
                k.dma("sp", ik[:], ik_scr[:, ssl], R=[ik_scr], W=[ik])
                k.dma("sp", va[:, :, 0:65], va_scr[ssl, :].rearrange("(j p) n -> p j n", p=128), R=[va_scr], W=[va])
                k.dma("sp", iw[:], iw_scr[ssl, :].rearrange("(j p) n -> p j n", p=128), R=[iw_scr], W=[iw])
                for qt in range(NQT):
                    nb = qt + 1
                    S = nb * 128
                    qsl = slice(qt * 128, (qt + 1) * 128)
                    for kb in range(0, S, 512):
                        n = min(512, S - kb)
                        for h in range(4):
                            hp, j = h % 2, h // 2
                            ps = self.bank()
                            k.op("pe", lambda e, ps=ps, hp=hp, j=j, kb=kb, n=n: e.matmul(
                                ps[:, 0:n], lhsT=iq[hp * 64:(hp + 1) * 64, j, qsl], rhs=ik[hp * 64:(hp + 1) * 64, kb:kb + n],
                                start=True, stop=True), R=[iq, ik], W=[ps])
                            if h == 0:
                                k.op("dve", lambda e, ps=ps, kb=kb, n=n, h=h: e.tensor_scalar(
                                    out=score[:, kb:kb + n], in0=ps[:, 0:n], scalar1=0.0, scalar2=iw[:, qt, h:h + 1],
                                    op0=ALU.max, op1=ALU.mult), R=[ps, iw], W=[score])
                            else:
                                t_ = tmp[h % 2]
                                k.op("dve", lambda e, ps=ps, n=n, h=h, t_=t_: e.tensor_scalar(
                                    out=t_[:, 0:n], in0=ps[:, 0:n], scalar1=0.0, scalar2=iw[:, qt, h:h + 1],
                                    op0=ALU.max, op1=ALU.mult), R=[ps, iw], W=[t_])
                                k.op("pool", lambda e, kb=kb, n=n, t_=t_: e.tensor_tensor(
                                    out=score[:, kb:kb + n], in0=score[:, kb:kb + n], in1=t_[:, 0:n], op=ALU.add),
                                    R=[score, t_], W=[score])
                    if 'a4s1' in self.skip:
                        continue
                    k.op("dve", lambda e, S=S: e.tensor_tensor(out=score[:, S - 128:S], in0=score[:, S - 128:S],
                                                               in1=mneg2[:], op=ALU.add), R=[score, mneg2], W=[score])
                    if S > KSEL:
                        k.op("dve", lambda e, S=S: e.reduce_max(out=bs[:, 1:2], in_=score[:, 0:S], axis=AX.X),
                             R=[score], W=[bs])
                        k.op("dve", lambda e: e.tensor_scalar(out=bs[:, 0:1], in0=bs[:, 1:2], scalar1=-2048.0,
                                                              scalar2=None, op0=ALU.add), R=[bs], W=[bs])
                        k.op("dve", lambda e: e.tensor_scalar(out=bs[:, 1:2], in0=bs[:, 1:2], scalar1=1.0,
                                                              scalar2=None, op0=ALU.add), R=[bs], W=[bs])
                        for it_ in range(28):
                            k.op("dve", lambda e: e.tensor_scalar(out=bs[:, 2:3], in0=bs[:, 0:1], scalar1=bs[:, 1:2],
                                                                  scalar2=0.5, op0=ALU.add, op1=ALU.mult), R=[bs], W=[bs])
                            k.op("dve", lambda e, S=S: e.tensor_scalar(out=junk[:, 0:S], in0=score[:, 0:S],
                                                                       scalar1=bs[:, 2:3], scalar2=None, op0=ALU.is_ge),
                                 R=[score, bs], W=[junk])
                            k.op("dve", lambda e, S=S: e.reduce_sum(out=bs[:, 3:4], in_=junk[:, 0:S], axis=AX.X),
                                 R=[junk], W=[bs])
                            k.op("dve", lambda e: e.tensor_scalar(out=bs[:, 4:5], in0=bs[:, 3:4], scalar1=KSEL - 0.5,
                                                                  scalar2=None, op0=ALU.is_ge), R=[bs], W=[bs])
                            k.op("dve", lambda e: e.tensor_tensor(out=bs[:, 5:6], in0=bs[:, 2:3], in1=bs[:, 0:1],
                                                                  op=ALU.subtract), R=[bs], W=[bs])
                            k.op("dve", lambda e: e.scalar_tensor_tensor(out=bs[:, 0:1], in0=bs[:, 5:6], scalar=bs[:, 4:5],
                                                                         in1=bs[:, 0:1], op0=ALU.mult, op1=ALU.add),
                                 R=[bs], W=[bs])
                            k.op("dve", lambda e: e.tensor_tensor(out=bs[:, 5:6], in0=bs[:, 1:2], in1=bs[:, 2:3],
                                                                  op=ALU.subtract), R=[bs], W=[bs])
                            k.op("dve", lambda e: e.scalar_tensor_tensor(out=bs[:, 1:2], in0=bs[:, 5:6], scalar=bs[:, 4:5],
                                                                         in1=bs[:, 2:3], op0=ALU.mult, op1=ALU.add),
                                 R=[bs], W=[bs])
                    else:
                        k.op("dve", lambda e: e.memset(bs[:, 0:1], -1e29), W=[bs])
                    if 'a4s2' in self.skip:
                        continue
                    k.op("dve", lambda e, S=S: e.tensor_scalar(out=maskf[:, 0:S], in0=score[:, 0:S], scalar1=bs[:, 0:1],
                                                               scalar2=None, op0=ALU.is_ge), R=[score, bs], W=[maskf])
                    for b4 in range(0, nb, 4):
                        nn = min(4, nb - b4)
                        ps = self.bank()
                        for bb in range(nn):
                            b = b4 + bb
                            k.op("pe", lambda e, ps=ps, bb=bb, b=b: e.transpose(
                                ps[:, bb * 128:(bb + 1) * 128], maskf[:, b * 128:(b + 1) * 128], self.c_ident_f[:]),
                                R=[maskf, self.c_ident_f], W=[ps])
                        k.op("act", lambda e, ps=ps, b4=b4, nn=nn: e.copy(
                            out=maskT[:, b4:b4 + nn, :], in_=ps[:, 0:nn * 128].rearrange("p (b t) -> p b t", t=128)),
                            R=[ps], W=[maskT])
                    if 'a4s3' in self.skip:
                        continue
                    for hg in range(2):
                        k.op("dve", lambda e, hg=hg: e.memset(po[hg][:], 0.0), W=[po[hg]])
                    for b in range(nb):
                        bsl = slice(b * 128, (b + 1) * 128)
                        for hg in range(2):
                            ps = self.bank()
                            for hh in range(4):
                                hp, j = hg, hh
                                k.op("pe", lambda e, ps=ps, hh=hh, hp=hp, j=j, bsl=bsl: e.matmul(
                                    ps[:, hh * 128:(hh + 1) * 128], lhsT=kn[hp * 64:(hp + 1) * 64, bsl],
                                    rhs=qn[hp * 64:(hp + 1) * 64, j, qsl], start=True, stop=True), R=[kn, qn], W=[ps])
                            pb = psb[hg]
                            pmm = pm[hg]
                            k.op("act", lambda e, ps=ps, pb=pb: e.activation(out=pb[:], in_=ps[:], func=AF.Exp),
                                 R=[ps], W=[pb])
                            if 'a4qk' in self.skip:
                                continue
                            for hh in range(4):
                                k.op("pool" if hh % 2 else "dve", lambda e, pb=pb, pmm=pmm, b=b, hh=hh: e.tensor_tensor(
                                    out=pmm[:, hh, :], in0=pb[:, hh * 128:(hh + 1) * 128],
                                    in1=maskT[:, b, :], op=ALU.mult), R=[pb, maskT], W=[pmm])
                            if 'a4pm' in self.skip:
                                continue
                            for hh in range(4):
                                k.op("pe", lambda e, hg=hg, hh=hh, pmm=pmm, b=b, nb=nb: e.matmul(
                                    po[hg][:, hh * 128:hh * 128 + 66], lhsT=pmm[:, hh, :], rhs=va[:, b, 0:66],
                                    start=False, stop=(b == nb - 1), skip_group_check=True), R=[pmm, va], W=[po[hg]])
                    if 'a4s4' in self.skip:
                        continue
                    for hg in range(2):
                        for hh in range(4):
                            h = 2 * hh + hg
                            k.op("dve", lambda e, hg=hg, hh=hh, h=h: e.reciprocal(
                                out=rec[:, h:h + 1], in_=po[hg][:, hh * 128 + 64:hh * 128 + 65]), R=[po[hg]], W=[rec])
                            k.op("dve", lambda e, hg=hg, hh=hh, h=h: e.tensor_scalar(
                                out=ya[:, h * 64:(h + 1) * 64], in0=po[hg][:, hh * 128:hh * 128 + 64],
                                scalar1=rec[:, h:h + 1], scalar2=None, op0=ALU.mult), R=[po[hg], rec], W=[ya])
                    ps = self.bank()
                    for j in range(4):
                        k.op("pe", lambda e, ps=ps, j=j: e.transpose(
                            ps[:, j * 128:(j + 1) * 128], ya[:, j * 128:(j + 1) * 128], self.c_ident_f[:]),
                            R=[ya, self.c_ident_f], W=[ps])
                    k.op("act", lambda e, ps=ps: e.copy(out=yaT[:], in_=ps[:].rearrange("p (j t) -> p j t", j=4)),
                         R=[ps], W=[yaT])
                    t0 = s_ * T + qt * 128
                    k.dma("sp", ya_scr[:, :, t0:t0 + 128], yaT[:], R=[yaT], W=[ya_scr])
            self.bank_rot(range(8))
        k.barrier()
        with contextlib.ExitStack() as sc:
            wo = k.sbuf([128, NCH, D], BF16, "wo", sc)
            k.dma("pool", wo[:], W["w_out_e"].t[0].rearrange("(c p) n -> p c n", p=128), R=[W["w_out_e"]], W=[wo])
            yab = k.sbuf([128, NCH, 512], BF16, "yab", sc)
            x = k.sbuf([128, NCH, 512], F32, "a5x", sc)
            if "rwkv" in self.skip:
                k.op("dve", lambda e: e.memset(yab[:], 0.0), W=[yab])
            for tb in range(ntok // 512):
                tsl = slice(tb * 512, (tb + 1) * 512)
                k.dma("sp", yab[:, 0:4, :], ya_scr[:, :, tsl], R=[ya_scr], W=[yab])
                if "rwkv" not in self.skip:
                    k.dma("sp", yab[:, 4:8, :], yb_scr[:, :, tsl], R=[yb_scr], W=[yab])
                k.dma("sp", x[:], self.xT[:, :, tsl], R=[self.xT], W=[x])
                for n in range(NCH):
                    pso = self.bank()
                    for c in range(NCH):
                        k.op("pe", lambda e, n=n, c=c, pso=pso: e.matmul(
                            pso[:], lhsT=wo[:, c, n * 128:(n + 1) * 128], rhs=yab[:, c, :],
                            start=(c == 0), stop=(c == NCH - 1)), R=[wo, yab], W=[pso])
                    k.op("dve", lambda e, n=n, pso=pso: e.tensor_tensor(
                        out=x[:, n, :], in0=x[:, n, :], in1=pso[:], op=ALU.add), R=[pso, x], W=[x])
                k.dma("sp", self.xT[:, :, tsl], x[:], R=[x], W=[self.xT])
        k.barrier()

    def phase_rwkv(self, W, ub_scr, yb_scr):
        k = self.k
        ntok = self.ntok
        T = min(SEQ, ntok)
        nseq = ntok // T
        NB = 14
        names = ["At", "rt", "Bt", "kt", "v", "W", "bon", "g"]
        scr = {n_: k.dram([128, 4, ntok], F32, "rw_" + n_) for n_ in names}
        with contextlib.ExitStack() as sc:
            w2a2 = k.sbuf([128, 512], BF16, "w2a2", sc)
            g2 = k.sbuf([128, 512], BF16, "g2", sc)
            k.dma("pool", w2a2[0:64, :], W["b_w2"].t[0], R=[W["b_w2"]], W=[w2a2])
            k.dma("pool", w2a2[64:128, :], W["b_a2"].t[0], R=[W["b_a2"]], W=[w2a2])
            k.dma("pool", g2[:], W["b_g2"].t[0], R=[W["b_g2"]], W=[g2])
            cols = k.sbuf([128, 8, 4], F32, "rwcols", sc)
            for i_, nm in enumerate(["b_w0", "b_a0", "b_k_k", "b_k_a", "b_r_k"]):
                src = W[nm].t[0]
                if nm == "b_r_k":
                    src = src.rearrange("h d -> (h d)")
                k.dma("sp", cols[:, i_, :], src.rearrange("(c p) -> p c", p=128), R=[W[nm]], W=[cols],
                      allow_slow_non_contiguous=True)
            k.op("dve", lambda e: e.tensor_scalar(out=cols[:, 5, :], in0=cols[:, 0, :], scalar1=-1.0, scalar2=None,
                                                  op0=ALU.mult), R=[cols], W=[cols])
            k.op("dve", lambda e: e.memset(cols[:, 6, :], -0.5), W=[cols])
            k.op("dve", lambda e: e.memset(cols[:, 7, :], 1.0), W=[cols])
            mu = k.sbuf([128, NB], F32, "mu", sc)
            k.dma("sp", mu[:], W["b_mu"].t[0].rearrange("(c p) -> p c", p=128), R=[W["b_mu"]], W=[mu],
                  allow_slow_non_contiguous=True)
            bones = k.sbuf([128, 128], F32, "bones", sc)
            cbo = self.inp("c_bones", [128, 128])
            k.dma("sp", bones[:], cbo[:], R=[cbo], W=[bones])
            ubh = k.sbuf([128, NB, 516], F32, "ubh", sc)
            xs = k.sbuf([128, NB, 512], F32, "xs", sc)
            twa = k.sbuf([128, 512], BF16, "twa", sc)
            sxg = k.sbuf([128, 512], BF16, "sxg", sc)
            o = {n_: k.sbuf([128, 4, 512], F32, "o_" + n_, sc) for n_ in names}
            tt_ = {n_: k.sbuf([128, 512], F32, "t_" + n_, sc) for n_ in
                   ("t1", "e2", "a", "kk", "sq", "t2", "t3", "k", "cwA", "cwB", "Wex", "Winv")}
            for tb in range(ntok // 512):
                s_, off = (tb * 512) // T, (tb * 512) % T
                k.dma("sp", ubh[:], ub_scr[:, :, s_, off:off + 516], R=[ub_scr], W=[ubh])
                for g in range(NB):
                    k.op("pool", lambda e, g=g: e.tensor_tensor(out=xs[:, g, :], in0=ubh[:, g, 3:515], in1=ubh[:, g, 4:516],
                                                                op=ALU.subtract), R=[ubh], W=[xs])
                    k.op("dve", lambda e, g=g: e.scalar_tensor_tensor(out=xs[:, g, :], in0=xs[:, g, :], scalar=mu[:, g:g + 1],
                                                                      in1=ubh[:, g, 4:516], op0=ALU.mult, op1=ALU.add),
                         R=[xs, mu, ubh], W=[xs])
                k.op("act", lambda e: e.activation(out=twa[0:64, :], in_=xs[0:64, 12, :], func=AF.Tanh), R=[xs], W=[twa])
                k.op("act", lambda e: e.copy(out=twa[64:128, :], in_=xs[64:128, 12, :]), R=[xs], W=[twa])
                k.op("act", lambda e: e.activation(out=sxg[:], in_=xs[:, 13, :], func=AF.Sigmoid), R=[xs], W=[sxg])
                for p in range(4):
                    r_, k0_, v_ = xs[:, p, :], xs[:, 4 + p, :], xs[:, 8 + p, :]
                    psl = slice(p * 128, (p + 1) * 128)
                    ps1 = self.bank()
                    k.op("pe", lambda e, ps1=ps1, psl=psl: e.matmul(ps1[:], lhsT=w2a2[0:64, psl], rhs=twa[0:64, :],
                                                                    start=True, stop=True), R=[w2a2, twa], W=[ps1])
                    ps2 = self.bank()
                    k.op("pe", lambda e, ps2=ps2, psl=psl: e.matmul(ps2[:], lhsT=w2a2[64:128, psl], rhs=twa[64:128, :],
                                                                    start=True, stop=True), R=[w2a2, twa], W=[ps2])
                    ps3 = self.bank()
                    k.op("pe", lambda e, ps3=ps3, psl=psl: e.matmul(ps3[:], lhsT=g2[:, psl], rhs=sxg[:],
                                                                    start=True, stop=True), R=[g2, sxg], W=[ps3])
                    t1, e2, a_, kk_, sq_, t2, t3, k_ = (tt_[n_] for n_ in ("t1", "e2", "a", "kk", "sq", "t2", "t3", "k"))
                    cwA, cwB, Wex, Winv = (tt_[n_] for n_ in ("cwA", "cwB", "Wex", "Winv"))
                    k.op("act", lambda e, ps1=ps1, p=p: e.activation(out=t1[:], in_=ps1[:], func=AF.Exp, scale=-1.0,
                                                                     bias=cols[:, 5, p:p + 1]), R=[ps1, cols], W=[t1])
                    k.op("act", lambda e: e.activation(out=t1[:], in_=t1[:], func=AF.Ln, scale=1.0, bias=cols[:, 7, 0:1]),
                         R=[t1, cols], W=[t1])
                    k.op("act", lambda e: e.activation(out=e2[:], in_=t1[:], func=AF.Exp, scale=-1.0, bias=cols[:, 6, 0:1]),
                         R=[t1, cols], W=[e2])
                    k.op("act", lambda e, ps2=ps2, p=p: e.activation(out=a_[:], in_=ps2[:], func=AF.Sigmoid, scale=1.0,
                                                                     bias=cols[:, 1, p:p + 1]), R=[ps2, cols], W=[a_])
                    k.op("act", lambda e, ps3=ps3, p=p: e.copy(out=o["g"][:, p, :], in_=ps3[:]), R=[ps3], W=[o["g"]])
                    k.op("dve", lambda e, p=p, k0_=k0_: e.tensor_scalar(out=kk_[:], in0=k0_, scalar1=cols[:, 2, p:p + 1],
                                                                       scalar2=None, op0=ALU.mult), R=[xs, cols], W=[kk_])
                    k.op("pool", lambda e: e.tensor_tensor(out=sq_[:], in0=kk_[:], in1=kk_[:], op=ALU.mult), R=[kk_], W=[sq_])
                    ps4 = self.bank()
                    k.op("pe", lambda e, ps4=ps4: e.matmul(ps4[:], lhsT=bones[:], rhs=sq_[:], start=True, stop=True),
                         R=[bones, sq_], W=[ps4])
                    k.op("act", lambda e, ps4=ps4: e.activation(out=t2[:], in_=ps4[:], func=AF.Sqrt), R=[ps4], W=[t2])
                    k.op("dve", lambda e: e.tensor_scalar(out=t2[:], in0=t2[:], scalar1=1e-12, scalar2=None, op0=ALU.max),
                         R=[t2], W=[t2])
                    k.op("dve", lambda e: e.reciprocal(out=t2[:], in_=t2[:]), R=[t2], W=[t2])
                    k.op("dve", lambda e: e.tensor_tensor(out=kk_[:], in0=kk_[:], in1=t2[:], op=ALU.mult), R=[kk_, t2], W=[kk_])
                    k.op("dve", lambda e, p=p: e.tensor_scalar(out=t3[:], in0=a_[:], scalar1=-1.0, scalar2=cols[:, 3, p:p + 1],
                                                              op0=ALU.add, op1=ALU.mult), R=[a_, cols], W=[t3])
                    k.op("dve", lambda e, k0_=k0_: e.scalar_tensor_tensor(out=k_[:], in0=t3[:], scalar=1.0, in1=k0_,
                                                                         op0=ALU.add, op1=ALU.mult), R=[t3, xs], W=[k_])
                    k.op("dve", lambda e, p=p, r_=r_: e.scalar_tensor_tensor(out=t3[:], in0=r_, scalar=cols[:, 4, p:p + 1],
                                                                            in1=k_[:], op0=ALU.mult, op1=ALU.mult),
                         R=[xs, cols, k_], W=[t3])
                    ps5 = self.bank()
                    k.op("pe", lambda e, ps5=ps5: e.matmul(ps5[:], lhsT=bones[:], rhs=t3[:], start=True, stop=True),
                         R=[bones, t3], W=[ps5])
                    k.op("dve", lambda e, ps5=ps5, p=p, v_=v_: e.tensor_tensor(out=o["bon"][:, p, :], in0=v_, in1=ps5[:],
                                                                              op=ALU.mult), R=[xs, ps5], W=[o["bon"]])
                    k.op("dve", lambda e: e.tensor_scalar(out=cwA[:], in0=e2[:], scalar1=-1.0, scalar2=None, op0=ALU.mult),
                         R=[e2], W=[cwA])
                    src_, dst_ = cwA, cwB
                    for j in (1, 2, 4, 8, 16, 32):
                        sv = src_[:].rearrange("p (c l) -> p c l", l=64)
                        dv = dst_[:].rearrange("p (c l) -> p c l", l=64)
                        k.op("pool", lambda e, sv=sv, dv=dv, j=j: e.tensor_copy(out=dv[:, :, 0:j], in_=sv[:, :, 0:j]),
                             R=[src_], W=[dst_])
                        k.op("dve", lambda e, sv=sv, dv=dv, j=j: e.tensor_tensor(out=dv[:, :, j:64], in0=sv[:, :, j:64],
                                                                                 in1=sv[:, :, 0:64 - j], op=ALU.add),
                             R=[src_], W=[dst_])
                        src_, dst_ = dst_, src_
                    cw = src_
                    k.op("act", lambda e, p=p, cw=cw: e.activation(out=o["W"][:, p, :], in_=cw[:], func=AF.Exp), R=[cw], W=[o["W"]])
                    k.op("act", lambda e, cw=cw: e.activation(out=Winv[:], in_=cw[:], func=AF.Exp, scale=-1.0), R=[cw], W=[Winv])
                    k.op("dve", lambda e, cw=cw: e.tensor_tensor(out=Wex[:], in0=cw[:], in1=e2[:], op=ALU.add), R=[cw, e2], W=[Wex])
                    k.op("act", lambda e: e.activation(out=Wex[:], in_=Wex[:], func=AF.Exp), R=[Wex], W=[Wex])
                    k.op("dve", lambda e, p=p: e.scalar_tensor_tensor(out=o["At"][:, p, :], in0=kk_[:], scalar=-1.0, in1=Wex[:],
                                                                     op0=ALU.mult, op1=ALU.mult), R=[kk_, Wex], W=[o["At"]])
                    k.op("dve", lambda e, p=p, r_=r_: e.tensor_tensor(out=o["rt"][:, p, :], in0=r_, in1=o["W"][:, p, :],
                                                                     op=ALU.mult), R=[xs, o["W"]], W=[o["rt"]])
                    k.op("pool", lambda e: e.tensor_tensor(out=t3[:], in0=kk_[:], in1=a_[:], op=ALU.mult), R=[kk_, a_], W=[t3])
                    k.op("dve", lambda e, p=p: e.tensor_tensor(out=o["Bt"][:, p, :], in0=t3[:], in1=Winv[:], op=ALU.mult),
                         R=[t3, Winv], W=[o["Bt"]])
                    k.op("pool", lambda e, p=p: e.tensor_tensor(out=o["kt"][:, p, :], in0=k_[:], in1=Winv[:], op=ALU.mult),
                         R=[k_, Winv], W=[o["kt"]])
                    k.op("pool", lambda e, p=p, v_=v_: e.tensor_copy(out=o["v"][:, p, :], in_=v_), R=[xs], W=[o["v"]])
                for n_ in names:
                    k.dma("sp", scr[n_][:, :, tb * 512:(tb + 1) * 512], o[n_][:], R=[o[n_]], W=[scr[n_]])
        k.barrier()
        if self.upto == "A2":
            return
        with contextlib.ExitStack() as sc:
            L = CHUNK
            ST = k.sbuf([128, 4, 64], F32, "ST", sc)
            stmp = k.sbuf([128, 4, 64], F32, "STtmp", sc)
            ups = k.sbuf([64, 64], F32, "ups", sc)
            upi = k.sbuf([64, 64], F32, "upi", sc)
            los = k.sbuf([64, 64], F32, "los", sc)
            for nm, t_ in (("c_ups", ups), ("c_tri", upi), ("c_los", los)):
                ci_ = self.inputs.get(nm) or self.inp(nm, [64, 64])
                k.dma("sp", t_[:], ci_[:], R=[ci_], W=[t_])
            gn = k.sbuf([128, 2, 4], F32, "gn", sc)
            k.dma("sp", gn[:, 0, :], W["b_gn_g"].t[0].rearrange("(c p) -> p c", p=128), R=[W["b_gn_g"]], W=[gn],
                  allow_slow_non_contiguous=True)
            k.dma("sp", gn[:, 1, :], W["b_gn_b"].t[0].rearrange("(c p) -> p c", p=128), R=[W["b_gn_b"]], W=[gn],
                  allow_slow_non_contiguous=True)
            blk = {n_: k.sbuf([128, 4, 512], F32, "b_" + n_, sc) for n_ in names}
            ybo = k.sbuf([128, 4, 512], BF16, "ybo", sc)
            def tm(nm):
                return k.sbuf([64, 4, 2, 64], F32, nm, sc)
            Pm = [tm("Pm0"), tm("Pm1")]
            Qm = [tm("Qm0"), tm("Qm1")]
            MAK, MRB, MRK = tm("MAK"), tm("MRB"), tm("MRK")
            Vt, Btk, ktk = tm("Vt"), tm("Btk"), tm("ktk")
            X, Xb, Y, Yc, Ysq = tm("X"), tm("Xb"), tm("Y"), tm("Yc"), tm("Ysq")
            st8 = k.sbuf([64, 4, 8], F32, "st8", sc)
            yT = k.sbuf([128, 4, 64], F32, "yT", sc)

            def fm_mm(dst_bank, hp, lhs_blk, rhs_blk, csl):
                hs = slice(hp * 64, (hp + 1) * 64)
                for p in range(4):
                    k.op("pe", lambda e, p=p: e.matmul(dst_bank[0:64, p * 64:(p + 1) * 64], lhsT=lhs_blk[hs, p, csl],
                                                       rhs=rhs_blk[hs, p, csl], start=True, stop=True),
                         R=[lhs_blk, rhs_blk], W=[dst_bank])

            def masked(dst, lhs_blk, rhs_blk, mask, csl):
                for hp in range(2):
                    ps = self.bank()
                    fm_mm(ps, hp, lhs_blk, rhs_blk, csl)
                    k.op("dve", lambda e, ps=ps, hp=hp: e.tensor_tensor(
                        out=dst[:, :, hp, :], in0=ps[0:64, 0:256].rearrange("t (p s) -> t p s", p=4),
                        in1=mask[:, :].unsqueeze(1).to_broadcast([64, 4, 64]), op=ALU.mult), R=[ps, mask], W=[dst])

            def tok_T(dst, src_blk, csl):
                ps = self.bank()
                for p in range(4):
                    k.op("pe", lambda e, p=p, ps=ps: e.transpose(ps[0:64, p * 128:(p + 1) * 128], src_blk[:, p, csl],
                                                                 self.c_ident_f[:]), R=[src_blk, self.c_ident_f], W=[ps])
                k.op("act", lambda e, ps=ps: e.copy(out=dst[:].rearrange("t p h v -> t (p h v)"), in_=ps[0:64, :]),
                     R=[ps], W=[dst])

            def tm_mm(ps, terms):
                for p in range(4):
                    for hp in range(2):
                        c0 = (p * 2 + hp) * 64
                        for i_, (lt, rt_) in enumerate(terms):
                            k.op("pe", lambda e, p=p, hp=hp, c0=c0, i_=i_, lt=lt, rt_=rt_: e.matmul(
                                ps[0:64, c0:c0 + 64], lhsT=lt[:, p, hp, :], rhs=rt_[:, p, hp, :],
                                start=(i_ == 0), stop=(i_ == len(terms) - 1)), R=[lt, rt_], W=[ps])

            for s_ in range(nseq):
                k.op("dve", lambda e: e.memset(ST[:], 0.0), W=[ST])
                for ch in range(T // L):
                    t0 = s_ * T + ch * L
                    if (ch * L) % 512 == 0:
                        for n_ in names:
                            k.dma("sp", blk[n_][:], scr[n_][:, :, t0:t0 + 512], R=[scr[n_]], W=[blk[n_]])
                    c0_ = (ch * L) % 512
                    csl = slice(c0_, c0_ + L)
                    At, rt, Bt, kt, vv, Wb = (blk[n_] for n_ in ("At", "rt", "Bt", "kt", "v", "W"))
                    P, Q = Pm[0], Qm[0]
                    masked(P, Bt, At, ups, csl)
                    masked(Q, At, Bt, los, csl)
                    masked(MAK, kt, At, ups, csl)
                    masked(MRB, Bt, rt, upi, csl)
                    masked(MRK, kt, rt, upi, csl)
                    tok_T(Vt, vv, csl)
                    tok_T(Btk, Bt, csl)
                    tok_T(ktk, kt, csl)
                    psb_ = self.bank()
                    tm_mm(psb_, [(MAK, Vt)])
                    k.op("act", lambda e, psb_=psb_: e.copy(out=Xb[:].rearrange("t p h v -> t (p h v)"), in_=psb_[0:64, :]),
                         R=[psb_], W=[Xb])
                    for hp in range(2):
                        hs = slice(hp * 64, (hp + 1) * 64)
                        ps = self.bank()
                        for p in range(4):
                            k.op("pe", lambda e, p=p, ps=ps, hs=hs: e.matmul(
                                ps[0:64, p * 64:(p + 1) * 64], lhsT=At[hs, p, csl], rhs=ST[hs, p, :], start=True, stop=True),
                                R=[At, ST], W=[ps])
                        k.op("dve", lambda e, ps=ps, hp=hp: e.tensor_tensor(
                            out=X[:, :, hp, :], in0=ps[0:64, 0:256].rearrange("t (p s) -> t p s", p=4),
                            in1=Xb[:, :, hp, :], op=ALU.add), R=[ps, Xb], W=[X])
                    for j in range(6):
                        ps = self.bank()
                        tm_mm(ps, [(P, X)])
                        if j < 5:
                            psP = self.bank()
                            tm_mm(psP, [(Q, P)])
                            psQ = self.bank()
                            tm_mm(psQ, [(P, Q)])
                        k.op("dve", lambda e, ps=ps: e.tensor_tensor(
                            out=X[:].rearrange("t p h v -> t (p h v)"), in0=X[:].rearrange("t p h v -> t (p h v)"),
                            in1=ps[0:64, :], op=ALU.add), R=[ps, X], W=[X])
                        if j < 5:
                            P2, Q2 = Pm[(j + 1) % 2], Qm[(j + 1) % 2]
                            k.op("act", lambda e, psP=psP, P2=P2: e.copy(out=P2[:].rearrange("t p h v -> t (p h v)"),
                                                                         in_=psP[0:64, :]), R=[psP], W=[P2])
                            k.op("dve", lambda e, psQ=psQ, Q2=Q2: e.tensor_copy(out=Q2[:].rearrange("t p h v -> t (p h v)"),
                                                                                in_=psQ[0:64, :]), R=[psQ], W=[Q2])
                            P, Q = P2, Q2
                    SA = X
                    psb_ = self.bank()
                    tm_mm(psb_, [(MRB, SA), (MRK, Vt)])
                    k.op("act", lambda e, psb_=psb_: e.copy(out=Xb[:].rearrange("t p h v -> t (p h v)"), in_=psb_[0:64, :]),
                         R=[psb_], W=[Xb])
                    for hp in range(2):
                        hs = slice(hp * 64, (hp + 1) * 64)
                        ps = self.bank()
                        for p in range(4):
                            k.op("pe", lambda e, p=p, ps=ps, hs=hs: e.matmul(
                                ps[0:64, p * 64:(p + 1) * 64], lhsT=rt[hs, p, csl], rhs=ST[hs, p, :], start=True, stop=True),
                                R=[rt, ST], W=[ps])
                        k.op("dve", lambda e, ps=ps, hp=hp: e.tensor_tensor(
                            out=Y[:, :, hp, :], in0=ps[0:64, 0:256].rearrange("t (p s) -> t p s", p=4),
                            in1=Xb[:, :, hp, :], op=ALU.add), R=[ps, Xb], W=[Y])
                    psS = self.bank()
                    for hp in range(2):
                        for p in range(4):
                            for i_, (lt, rt_) in enumerate(((Btk, SA), (ktk, Vt))):
                                k.op("pe", lambda e, p=p, hp=hp, i_=i_, lt=lt, rt_=rt_: e.matmul(
                                    psS[hp * 64:(hp + 1) * 64, p * 64:(p + 1) * 64], lhsT=lt[:, p, hp, :], rhs=rt_[:, p, hp, :],
                                    start=(i_ == 0), stop=(i_ == 1)), R=[lt, rt_], W=[psS])
                    k.op("dve", lambda e, psS=psS: e.tensor_tensor(
                        out=stmp[:], in0=ST[:], in1=psS[:, 0:256].rearrange("k (p v) -> k p v", p=4), op=ALU.add),
                        R=[ST, psS], W=[stmp])
                    for p in range(4):
                        k.op("dve", lambda e, p=p: e.tensor_scalar(
                            out=ST[:, p, :], in0=stmp[:, p, :], scalar1=Wb[:, p, c0_ + L - 1:c0_ + L], scalar2=None,
                            op0=ALU.mult), R=[stmp, Wb], W=[ST])
                    Y8 = Y[:].rearrange("t p h v -> t (p h) v")
                    k.op("dve", lambda e: e.reduce_sum(out=st8[:, 0, :], in_=Y8, axis=AX.X), R=[Y], W=[st8])
                    k.op("dve", lambda e: e.tensor_scalar(out=st8[:, 0, :], in0=st8[:, 0, :], scalar1=1.0 / 64, scalar2=None,
                                                          op0=ALU.mult), R=[st8], W=[st8])
                    k.op("dve", lambda e: e.tensor_tensor(
                        out=Yc[:].rearrange("t p h v -> t (p h) v"), in0=Y8,
                        in1=st8[:, 0, :].unsqueeze(2).to_broadcast([64, 8, 64]), op=ALU.subtract), R=[Y, st8], W=[Yc])
                    k.op("pool", lambda e: e.tensor_tensor(out=Ysq[:], in0=Yc[:], in1=Yc[:], op=ALU.mult), R=[Yc], W=[Ysq])
                    k.op("dve", lambda e: e.reduce_sum(out=st8[:, 1, :], in_=Ysq[:].rearrange("t p h v -> t (p h) v"),
                                                       axis=AX.X), R=[Ysq], W=[st8])
                    k.op("dve", lambda e: e.tensor_scalar(out=st8[:, 1, :], in0=st8[:, 1, :], scalar1=1.0 / 64, scalar2=64e-5,
                                                          op0=ALU.mult, op1=ALU.add), R=[st8], W=[st8])
                    k.op("act", lambda e: e.activation(out=st8[:, 2, :], in_=st8[:, 1, :], func=AF.Sqrt), R=[st8], W=[st8])
                    k.op("dve", lambda e: e.reciprocal(out=st8[:, 3, :], in_=st8[:, 2, :]), R=[st8], W=[st8])
                    k.op("dve", lambda e: e.tensor_tensor(
                        out=Yc[:].rearrange("t p h v -> t (p h) v"), in0=Yc[:].rearrange("t p h v -> t (p h) v"),
                        in1=st8[:, 3, :].unsqueeze(2).to_broadcast([64, 8, 64]), op=ALU.mult), R=[Yc, st8], W=[Yc])
                    ps = self.bank()
                    for p in range(4):
                        k.op("pe", lambda e, p=p, ps=ps: e.transpose(
                            ps[:, p * 64:(p + 1) * 64], Yc[:, p, :, :].rearrange("t h v -> t (h v)"),
                            self.c_ident_f[0:64, 0:64]), R=[Yc, self.c_ident_f], W=[ps])
                    for p in range(4):
                        k.op("dve", lambda e, p=p, ps=ps: e.tensor_scalar(
                            out=yT[:, p, :], in0=ps[:, p * 64:(p + 1) * 64], scalar1=gn[:, 0, p:p + 1],
                            scalar2=gn[:, 1, p:p + 1], op0=ALU.mult, op1=ALU.add), R=[ps, gn], W=[yT])
                    k.op("pool", lambda e: e.tensor_tensor(out=yT[:], in0=yT[:], in1=blk["bon"][:, :, csl], op=ALU.add),
                         R=[yT, blk["bon"]], W=[yT])
                    k.op("pool", lambda e: e.tensor_tensor(out=ybo[:, :, csl], in0=yT[:], in1=blk["g"][:, :, csl], op=ALU.mult),
                         R=[yT, blk["g"]], W=[ybo])
                    if c0_ + L == 512 or (ch + 1) * L == T:
                        b0 = t0 + L - (c0_ + L)
                        k.dma("sp", yb_scr[:, :, b0:b0 + c0_ + L], ybo[:, :, 0:c0_ + L], R=[ybo], W=[yb_scr])
        k.barrier()

    def phase_mix1(self, W):
        k = self.k
        ntok = self.ntok
        T = min(SEQ, ntok)
        nseq = ntok // T
        NI = 16
        xm_scr = k.dram([128, NI, nseq, T + 4], BF16, "xm_scr")
        zs_scr = k.dram([128, NI, ntok], BF16, "zs_scr")
        xc_scr = k.dram([128, NI, ntok], BF16, "xc_scr")
        q_scr = k.dram([128, NI, ntok], BF16, "q_scr")
        k_scr = k.dram([128, NI, ntok], BF16, "k_scr")
        Kt_scr = k.dram([ntok, 2048], BF16, "Kt_scr")
        Vt_scr = k.dram([ntok, 2048], BF16, "Vt_scr")
        g_scr = k.dram([ntok, 8], F32, "g_scr")
        y_scr = k.dram([ntok, 2048], F32, "y_scr")
        QS = 512.0 ** -0.5
        with contextlib.ExitStack() as sc:
            stage = [k.sbuf([128, 8, 512], BF16, "cst", sc) for _ in range(2)]
            wup_s = self.conv_weight(W["c_w_up"].t[0], D, 4096, "wup_s", stage)
            gcol = k.sbuf([128, NCH], F32, "gcol", sc)
            k.dma("sp", gcol[:], W["mix_norm"].t[1].rearrange("(c p) -> p c", p=128),
                  R=[W["mix_norm"]], W=[gcol], allow_slow_non_contiguous=True)
            zt = k.sbuf([128, NI, 4], BF16, "zt", sc)
            k.op("dve", lambda e: e.memset(zt[:], 0.0), W=[zt])
            for s_ in range(nseq):
                k.dma("sp", xm_scr[:, :, s_, 0:4], zt[:], R=[zt], W=[xm_scr])
            xb = [k.sbuf([128, NCH, 512], F32, "m1x", sc) for _ in range(2)]
            hT = k.sbuf([128, NCH, 512], BF16, "m1h", sc)
            sq = k.sbuf([128, NCH, 512], BF16, "m1sq", sc)
            rstd = k.sbuf([128, 512], F32, "m1rstd", sc)
            wt = [k.sbuf([128, NCH, 512], BF16, "m1w", sc) for _ in range(2)]
            xmo = k.sbuf([128, NI, 512], BF16, "m1xm", sc)
            zo = k.sbuf([128, NI, 512], BF16, "m1z", sc)
            it = 0
            for tb in range(ntok // 512):
                s_, off = (tb * 512) // T, (tb * 512) % T
                x = xb[tb % 2]
                k.dma("sp", x[:], self.xT[:, :, tb * 512:(tb + 1) * 512], R=[self.xT], W=[x])
                self.rmsnorm([(x, x[:, c, :]) for c in range(NCH)], (gcol, gcol[:, :]),
                             [(hT, hT[:, c, :]) for c in range(NCH)], 512, sq, rstd)
                for og in range(8):
                    w = wt[it % 2]
                    it += 1
                    k.dma("sp", w[:], wup_s[:, :, og * 512:(og + 1) * 512], R=[wup_s], W=[w])
                    for jj in range(4):
                        ps = self.bank()
                        for c in range(NCH):
                            k.op("pe", lambda e, c=c, jj=jj, ps=ps, w=w: e.matmul(
                                ps[:], lhsT=w[:, c, jj * 128:(jj + 1) * 128], rhs=hT[:, c, :],
                                start=(c == 0), stop=(c == NCH - 1)), R=[w, hT], W=[ps])
                        if og < 4:
                            k.op("act", lambda e, ps=ps, i=og * 4 + jj: e.copy(out=xmo[:, i, :], in_=ps[:]),
                                 R=[ps], W=[xmo])
                        else:
                            k.op("act", lambda e, ps=ps, i=(og - 4) * 4 + jj: e.activation(
                                out=zo[:, i, :], in_=ps[:], func=AF.Silu), R=[ps], W=[zo])
                k.dma("sp", xm_scr[:, :, s_, 4 + off:4 + off + 512], xmo[:], R=[xmo], W=[xm_scr])
                k.dma("sp", zs_scr[:, :, tb * 512:(tb + 1) * 512], zo[:], R=[zo], W=[zs_scr])
        k.barrier()
        if self.upto == "M1":
            return
        with contextlib.ExitStack() as sc:
            bd = k.sbuf([128, 48, 128], BF16, "bd", sc)
            wqt = k.sbuf([128, 48, 4], F32, "wqt", sc)
            bdm = k.sbuf([128, 32], F32, "bdm", sc)
            cbm = self.inp("c_bdmask", [128, 32])
            k.dma("sp", bdm[:], cbm[:], R=[cbm], W=[bdm])
            for wi, wn in enumerate(["c_wq", "c_wk", "c_wv"]):
                for c_ in range(16):
                    k.dma("sp", wqt[:, wi * 16 + c_, :],
                          W[wn].t[0, c_ * 32:(c_ + 1) * 32].rearrange("g i j -> (g i) j"), R=[W[wn]], W=[wqt])
            k.op("dve", lambda e: e.memset(bd[:], 0.0), W=[bd])
            for ci in range(48):
                for j in range(4):
                    k.op("dve", lambda e, ci=ci, j=j: e.tensor_scalar(
                        out=bd[:, ci, :].rearrange("p (g j) -> p g j", j=4)[:, :, j], in0=bdm[:],
                        scalar1=wqt[:, ci, j:j + 1], scalar2=None, op0=ALU.mult), R=[bdm, wqt], W=[bd])
            wif = k.sbuf([128, 48, 8], BF16, "wif", sc)
            for i3_ in range(3):
                k.dma("pool", wif[:, i3_ * 16:(i3_ + 1) * 16, :],
                      W["c_w_if"].t[0, i3_ * 2048:(i3_ + 1) * 2048].rearrange("(c p) n -> p c n", p=128),
                      R=[W["c_w_if"]], W=[wif])
            brow = k.sbuf([1, 8], F32, "brow", sc)
            k.dma("sp", brow[0:1, 0:4], W["c_b_i"].t[0:1, :], R=[W["c_b_i"]], W=[brow])
            k.dma("sp", brow[0:1, 4:8], W["c_b_f"].t[0:1, :], R=[W["c_b_f"]], W=[brow])
            onesf = k.sbuf([1, 128], F32, "onesf", sc)
            k.op("dve", lambda e: e.memset(onesf[:], 1.0), W=[onesf])
            cw = k.sbuf([128, NI, 4], F32, "cw", sc)
            for j_ in range(4):
                k.dma("sp", cw[:, :, j_], W["c_conv_w"].t[0, j_, 0].rearrange("(c p) -> p c", p=128),
                      R=[W["c_conv_w"]], W=[cw], allow_slow_non_contiguous=True)
            cb = k.sbuf([128, NI], F32, "cb", sc)
            k.dma("sp", cb[:], W["c_conv_b"].t[0].rearrange("(c p) -> p c", p=128),
                  R=[W["c_conv_b"]], W=[cb], allow_slow_non_contiguous=True)
            xmh = k.sbuf([128, NI, 516], BF16, "xmh", sc)
            acc = [k.sbuf([128, 512], F32, "acc", sc) for _ in range(2)]
            xc = k.sbuf([128, NI, 512], BF16, "xc", sc)
            qT = k.sbuf([128, NI, 512], BF16, "qT", sc)
            qs = k.sbuf([128, NI, 512], BF16, "qs", sc)
            kT = k.sbuf([128, NI, 512], BF16, "kT", sc)
            vT = k.sbuf([128, NI, 512], BF16, "vT", sc)
            Kt = k.sbuf([128, 2048], BF16, "Kt", sc)
            Vt = k.sbuf([128, 2048], BF16, "Vt", sc)
            gt = k.sbuf([128, 8], F32, "gt", sc)
            for tb in range(ntok // 512 if 'm2loop' not in self.skip else 0):
                s_, off = (tb * 512) // T, (tb * 512) % T
                k.dma("sp", xmh[:], xm_scr[:, :, s_, off:off + 516], R=[xm_scr], W=[xmh])
                for c in range(NI):
                    a = acc[c % 2]
                    k.op("dve", lambda e, c=c, a=a: e.tensor_scalar(
                        out=a[:], in0=xmh[:, c, 1:513], scalar1=cw[:, c, 0:1], scalar2=None, op0=ALU.mult),
                        R=[xmh, cw], W=[a])
                    for j in range(1, 4):
                        k.op("dve", lambda e, c=c, a=a, j=j: e.scalar_tensor_tensor(
                            out=a[:], in0=xmh[:, c, 1 + j:1 + j + 512], scalar=cw[:, c, j:j + 1], in1=a[:],
                            op0=ALU.mult, op1=ALU.add), R=[xmh, cw, a], W=[a])
                    k.op("act", lambda e, c=c, a=a: e.activation(
                        out=xc[:, c, :], in_=a[:], func=AF.Silu, bias=cb[:, c:c + 1], scale=1.0),
                        R=[a, cb], W=[xc])
                k.dma("sp", xc_scr[:, :, tb * 512:(tb + 1) * 512], xc[:], R=[xc], W=[xc_scr])
                if 'm2qkv' in self.skip:
                    continue
                for c in range(NI):
                    ps = self.bank()
                    k.op("pe", lambda e, c=c, ps=ps: e.matmul(ps[:], lhsT=bd[:, c, :], rhs=xc[:, c, :],
                                                             start=True, stop=True), R=[bd, xc], W=[ps])
                    k.op("act", lambda e, c=c, ps=ps: e.copy(out=qT[:, c, :], in_=ps[:]), R=[ps], W=[qT])
                    if 'm2qs' not in self.skip:
                        k.op("pool", lambda e, c=c: e.tensor_scalar(
                            out=qs[:, c, :], in0=qT[:, c, :], scalar1=QS, scalar2=None, op0=ALU.mult), R=[qT], W=[qs])
                    ps = self.bank()
                    k.op("pe", lambda e, c=c, ps=ps: e.matmul(ps[:], lhsT=bd[:, 16 + c, :], rhs=xc[:, c, :],
                                                             start=True, stop=True), R=[bd, xc], W=[ps])
                    k.op("act", lambda e, c=c, ps=ps: e.copy(out=kT[:, c, :], in_=ps[:]), R=[ps], W=[kT])
                    ps = self.bank()
                    k.op("pe", lambda e, c=c, ps=ps: e.matmul(ps[:], lhsT=bd[:, 32 + c, :],
                                                             rhs=(xc[:, c, :] if 'm2v' in self.skip else xmh[:, c, 4:516]),
                                                             start=True, stop=True), R=[bd, xmh], W=[ps])
                    k.op("dve", lambda e, c=c, ps=ps: e.tensor_copy(out=vT[:, c, :], in_=ps[:]), R=[ps], W=[vT])
                k.dma("sp", q_scr[:, :, tb * 512:(tb + 1) * 512], qs[:], R=[qs], W=[q_scr])
                k.dma("sp", k_scr[:, :, tb * 512:(tb + 1) * 512], kT[:], R=[kT], W=[k_scr])
                if 'm2tok' in self.skip:
                    continue
                for tt in range(4):
                    tsl = slice(tt * 128, (tt + 1) * 128)
                    for which, dst in ((0, Kt), (1, Vt)):
                        for b4 in range(4):
                            ps = self.bank()
                            for cc in range(4):
                                c = b4 * 4 + cc
                                if which == 0:
                                    k.op("pe", lambda e, c=c, cc=cc, ps=ps: e.matmul(
                                        ps[:, cc * 128:(cc + 1) * 128], lhsT=xc[:, c, tsl], rhs=bd[:, 16 + c, :],
                                        start=True, stop=True), R=[xc, bd], W=[ps])
                                else:
                                    k.op("pe", lambda e, c=c, cc=cc, ps=ps: e.matmul(
                                        ps[:, cc * 128:(cc + 1) * 128],
                                        lhsT=xmh[:, c, 4 + tt * 128:4 + (tt + 1) * 128], rhs=bd[:, 32 + c, :],
                                        start=True, stop=True), R=[xmh, bd], W=[ps])
                            en = "act" if b4 % 2 else "dve"
                            if en == "act":
                                k.op("act", lambda e, ps=ps, b4=b4, dst=dst: e.copy(
                                    out=dst[:, b4 * 512:(b4 + 1) * 512], in_=ps[:]), R=[ps], W=[dst])
                            else:
                                k.op("dve", lambda e, ps=ps, b4=b4, dst=dst: e.tensor_copy(
                                    out=dst[:, b4 * 512:(b4 + 1) * 512], in_=ps[:]), R=[ps], W=[dst])
                    ps = self.bank()
                    for i3, src in enumerate((qT, kT, vT)):
                        for c in range(NI):
                            k.op("pe", lambda e, c=c, i3=i3, src=src, ps=ps: e.matmul(
                                ps[:, 0:8], lhsT=src[:, c, tsl], rhs=wif[:, i3 * 16 + c, :],
                                start=(i3 == 0 and c == 0), stop=False), R=[src, wif], W=[ps])
                    if 'bias' not in self.skip:
                        k.op("pe", lambda e, ps=ps: e.matmul(ps[:, 0:8], lhsT=onesf[0:1, :], rhs=brow[0:1, :],
                                                            start=False, stop=True), R=[onesf, brow], W=[ps])
                    k.op("act", lambda e, ps=ps: e.copy(out=gt[:], in_=ps[:, 0:8]), R=[ps], W=[gt])
                    r0 = tb * 512 + tt * 128
                    k.dma("sp", Kt_scr[r0:r0 + 128, :], Kt[:], R=[Kt], W=[Kt_scr])
                    k.dma("sp", Vt_scr[r0:r0 + 128, :], Vt[:], R=[Vt], W=[Vt_scr])
                    k.dma("sp", g_scr[r0:r0 + 128, :], gt[:], R=[gt], W=[g_scr])
        k.barrier()
        if self.upto == "M2":
            return
        with contextlib.ExitStack() as sc:
            L = CHUNK
            C = k.sbuf([128, NI, 512], F32, "C", sc)
            Cb = k.sbuf([128, NI, 512], BF16, "Cb", sc)
            nv = k.sbuf([128, NI], F32, "nv", sc)
            nvb = k.sbuf([128, NI], BF16, "nvb", sc)
            mbc = k.sbuf([128, 4], F32, "mbc", sc)
            tri = k.sbuf([64, 64], F32, "tri", sc)
            sel = k.sbuf([64, 128], F32, "sel", sc)
            one64 = k.sbuf([64, 64], F32, "one64", sc)
            mneg = k.sbuf([64, 64], F32, "mneg", sc)
            oneb = k.sbuf([64, 1], BF16, "oneb", sc)
            cone = k.sbuf([128, 1], F32, "cone", sc)
            for nm, t_, shp in (("c_tri", tri, [64, 64]), ("c_sel", sel, [64, 128]), ("c_mneg", mneg, [64, 64])):
                ci_ = self.inp(nm, shp)
                k.dma("sp", t_[:], ci_[:], R=[ci_], W=[t_])
            k.op("dve", lambda e: e.memset(one64[:], 1.0), W=[one64])
            k.op("dve", lambda e: e.memset(oneb[:], 1.0), W=[oneb])
            k.op("dve", lambda e: e.memset(cone[:], 1.0), W=[cone])
            qsb = k.sbuf([128, NI, 512], BF16, "qsb", sc)
            kTb = k.sbuf([128, NI, 512], BF16, "kTb", sc)
            Ktc = [k.sbuf([64, 2048], BF16, "Ktc", sc) for _ in range(2)]
            Vtc = [k.sbuf([64, 2048], BF16, "Vtc", sc) for _ in range(2)]
            gtc = [k.sbuf([64, 8], F32, "gtc", sc) for _ in range(2)]
            sm = {n_: k.sbuf([128, 4], F32, n_, sc) for n_ in
                  ("l", "b", "bL", "bm", "cv", "mt", "negm", "scv", "emt", "mnew", "ws", "dc", "tmp")}
            dmat = [k.sbuf([64, 64], F32, f"dmat{h}", sc) for h in range(4)]
            diagc = k.sbuf([64, 64], F32, "diagc", sc)
            wts = k.sbuf([64, 64], F32, "wts", sc)
            qkw = k.sbuf([64, 64], F32, "qkw", sc)
            qkwT = k.sbuf([64, 64], BF16, "qkwT", sc)
            Asb = k.sbuf([64, 512], F32, "Asb", sc)
            hh = k.sbuf([64, 512], F32, "hh", sc)
            junk = k.sbuf([64, 512], F32, "junk", sc)
            Kw = k.sbuf([64, 512], BF16, "Kw", sc)
            st4 = k.sbuf([64, 8], F32, "st4", sc)
            yt = [k.sbuf([64, 2048], F32, "yt", sc) for _ in range(2)]
            for s_ in range(nseq):
                k.op("dve", lambda e: e.memset(C[:], 0.0), W=[C])
                k.op("dve", lambda e: e.memset(Cb[:], 0.0), W=[Cb])
                k.op("dve", lambda e: e.memset(nv[:], 0.0), W=[nv])
                k.op("dve", lambda e: e.memset(nvb[:], 0.0), W=[nvb])
                k.op("dve", lambda e: e.memset(mbc[:], 0.0), W=[mbc])
                for ch in range(T // L):
                    t0 = s_ * T + ch * L
                    if (ch * L) % 512 == 0:
                        k.dma("sp", qsb[:], q_scr[:, :, t0:t0 + 512], R=[q_scr], W=[qsb])
                        k.dma("sp", kTb[:], k_scr[:, :, t0:t0 + 512], R=[k_scr], W=[kTb])
                    csl = slice((ch * L) % 512, (ch * L) % 512 + L)
                    Kc, Vc, gc, yo = Ktc[ch % 2], Vtc[ch % 2], gtc[ch % 2], yt[ch % 2]
                    k.dma("sp", Kc[:], Kt_scr[t0:t0 + L, :], R=[Kt_scr], W=[Kc])
                    k.dma("sp", Vc[:], Vt_scr[t0:t0 + L, :], R=[Vt_scr], W=[Vc])
                    k.dma("sp", gc[:], g_scr[t0:t0 + L, :], R=[g_scr], W=[gc])
                    S = sm
                    k.op("act", lambda e: e.activation(out=S["tmp"][0:64, :], in_=gc[:, 4:8], func=AF.Exp, scale=-1.0),
                         R=[gc], W=[S["tmp"]])
                    k.op("act", lambda e: e.activation(out=S["l"][0:64, :], in_=S["tmp"][0:64, :], func=AF.Ln,
                                                       bias=cone[0:64, 0:1], scale=1.0), R=[S["tmp"], cone], W=[S["l"]])
                    ps = self.bank()
                    k.op("pe", lambda e, ps=ps: e.matmul(ps[0:64, 0:4], lhsT=tri[:], rhs=S["l"][0:64, :],
                                                        start=True, stop=True), R=[tri, S["l"]], W=[ps])
                    k.op("dve", lambda e, ps=ps: e.tensor_scalar(out=S["b"][0:64, :], in0=ps[0:64, 0:4], scalar1=-1.0,
                                                                scalar2=None, op0=ALU.mult), R=[ps], W=[S["b"]])
                    ps = self.bank()
                    k.op("pe", lambda e, ps=ps: e.matmul(ps[:, 0:4], lhsT=sel[:], rhs=S["b"][0:64, :],
                                                        start=True, stop=True), R=[sel, S["b"]], W=[ps])
                    k.op("dve", lambda e, ps=ps: e.tensor_copy(out=S["bL"][:], in_=ps[:, 0:4]), R=[ps], W=[S["bL"]])
                    k.op("dve", lambda e: e.tensor_tensor(out=S["bm"][0:64, :], in0=S["b"][0:64, :], in1=mbc[0:64, :],
                                                          op=ALU.add), R=[S["b"], mbc], W=[S["bm"]])
                    k.op("dve", lambda e: e.tensor_tensor(out=S["cv"][0:64, :], in0=gc[:, 0:4], in1=S["b"][0:64, :],
                                                          op=ALU.subtract), R=[gc, S["b"]], W=[S["cv"]])
                    for h in range(4):
                        k.op("dve", lambda e, h=h: e.tensor_scalar(
                            out=diagc[:], in0=self.c_ident_f[0:64, 0:64], scalar1=S["cv"][0:64, h:h + 1], scalar2=None,
                            op0=ALU.mult), R=[self.c_ident_f, S["cv"]], W=[diagc])
                        ps = self.bank()
                        k.op("pe", lambda e, ps=ps: e.matmul(ps[0:64, 0:64], lhsT=one64[:], rhs=diagc[:],
                                                            start=True, stop=True), R=[one64, diagc], W=[ps])
                        k.op("dve", lambda e, ps=ps, h=h: e.scalar_tensor_tensor(
                            out=dmat[h][:], in0=ps[0:64, 0:64], scalar=S["b"][0:64, h:h + 1], in1=mneg[:],
                            op0=ALU.add, op1=ALU.add), R=[ps, S["b"], mneg], W=[dmat[h]])
                        k.op("dve", lambda e, h=h: e.reduce_max(out=S["tmp"][0:64, h:h + 1], in_=dmat[h][:], axis=AX.X),
                             R=[dmat[h]], W=[S["tmp"]])
                    k.op("dve", lambda e: e.tensor_tensor(out=S["mt"][0:64, :], in0=S["tmp"][0:64, :], in1=S["bm"][0:64, :],
                                                          op=ALU.max), R=[S["tmp"], S["bm"]], W=[S["mt"]])
                    k.op("dve", lambda e: e.tensor_scalar(out=S["negm"][0:64, :], in0=S["mt"][0:64, :], scalar1=-1.0,
                                                          scalar2=None, op0=ALU.mult), R=[S["mt"]], W=[S["negm"]])
                    k.op("dve", lambda e: e.tensor_tensor(out=S["scv"][0:64, :], in0=S["bm"][0:64, :], in1=S["mt"][0:64, :],
                                                          op=ALU.subtract), R=[S["bm"], S["mt"]], W=[S["scv"]])
                    k.op("act", lambda e: e.activation(out=S["scv"][0:64, :], in_=S["scv"][0:64, :], func=AF.Exp),
                         R=[S["scv"]], W=[S["scv"]])
                    k.op("act", lambda e: e.activation(out=S["emt"][0:64, :], in_=S["negm"][0:64, :], func=AF.Exp),
                         R=[S["negm"]], W=[S["emt"]])
                    ps = self.bank()
                    k.op("pe", lambda e, ps=ps: e.matmul(ps[:, 0:4], lhsT=sel[:], rhs=S["mt"][0:64, :],
                                                        start=True, stop=True), R=[sel, S["mt"]], W=[ps])
                    k.op("dve", lambda e, ps=ps: e.tensor_copy(out=S["mnew"][:], in_=ps[:, 0:4]), R=[ps], W=[S["mnew"]])
                    k.op("dve", lambda e: e.tensor_tensor(out=S["ws"][0:64, :], in0=S["bL"][0:64, :], in1=S["cv"][0:64, :],
                                                          op=ALU.add), R=[S["bL"], S["cv"]], W=[S["ws"]])
                    k.op("dve", lambda e: e.tensor_tensor(out=S["ws"][0:64, :], in0=S["ws"][0:64, :], in1=S["mnew"][0:64, :],
                                                          op=ALU.subtract), R=[S["ws"], S["mnew"]], W=[S["ws"]])
                    k.op("act", lambda e: e.activation(out=S["ws"][0:64, :], in_=S["ws"][0:64, :], func=AF.Exp),
                         R=[S["ws"]], W=[S["ws"]])
                    k.op("dve", lambda e: e.tensor_tensor(out=S["dc"][:], in0=S["bL"][:], in1=mbc[:], op=ALU.add),
                         R=[S["bL"], mbc], W=[S["dc"]])
                    k.op("dve", lambda e: e.tensor_tensor(out=S["dc"][:], in0=S["dc"][:], in1=S["mnew"][:],
                                                          op=ALU.subtract), R=[S["dc"], S["mnew"]], W=[S["dc"]])
                    k.op("act", lambda e: e.activation(out=S["dc"][:], in_=S["dc"][:], func=AF.Exp),
                         R=[S["dc"]], W=[S["dc"]])
                    k.op("dve", lambda e: e.tensor_copy(out=mbc[:], in_=S["mnew"][:]), R=[S["mnew"]], W=[mbc])
                    for h in range(4):
                        hs = slice(h * 512, (h + 1) * 512)
                        k.op("act", lambda e, h=h: e.activation(out=wts[:], in_=dmat[h][:], func=AF.Exp,
                                                                bias=S["negm"][0:64, h:h + 1], scale=1.0),
                             R=[dmat[h], S["negm"]], W=[wts])
                        ps = self.bank()
                        for dcc in range(4):
                            k.op("pe", lambda e, ps=ps, dcc=dcc, h=h: e.matmul(
                                ps[0:64, 0:64], lhsT=qsb[:, h * 4 + dcc, csl], rhs=kTb[:, h * 4 + dcc, csl],
                                start=(dcc == 0), stop=(dcc == 3)), R=[qsb, kTb], W=[ps])
                        k.op("dve", lambda e, ps=ps: e.tensor_tensor(out=qkw[:], in0=wts[:], in1=ps[0:64, 0:64],
                                                                     op=ALU.mult), R=[wts, ps], W=[qkw])
                        k.op("dve", lambda e: e.reduce_sum(out=st4[:, 0:1], in_=qkw[:], axis=AX.X), R=[qkw], W=[st4])
                        ps = self.bank()
                        k.op("pe", lambda e, ps=ps: e.transpose(ps[0:64, 0:64], qkw[:], self.c_ident_f[0:64, 0:64]),
                             R=[qkw, self.c_ident_f], W=[ps])
                        k.op("act", lambda e, ps=ps: e.copy(out=qkwT[:], in_=ps[0:64, 0:64]), R=[ps], W=[qkwT])
                        psA = self.bank()
                        k.op("pe", lambda e, psA=psA, hs=hs: e.matmul(psA[0:64, :], lhsT=qkwT[:], rhs=Vc[:, hs],
                                                                      start=True, stop=True), R=[qkwT, Vc], W=[psA])
                        psB = self.bank()
                        for dcc in range(4):
                            k.op("pe", lambda e, psB=psB, dcc=dcc, h=h: e.matmul(
                                psB[0:64, :], lhsT=qsb[:, h * 4 + dcc, csl], rhs=Cb[:, h * 4 + dcc, :],
                                start=(dcc == 0), stop=(dcc == 3)), R=[qsb, Cb], W=[psB])
                        psn = self.bank()
                        for dcc in range(4):
                            k.op("pe", lambda e, psn=psn, dcc=dcc, h=h: e.matmul(
                                psn[0:64, 0:1], lhsT=qsb[:, h * 4 + dcc, csl], rhs=nvb[:, h * 4 + dcc:h * 4 + dcc + 1],
                                start=(dcc == 0), stop=(dcc == 3)), R=[qsb, nvb], W=[psn])
                        k.op("act", lambda e, psA=psA: e.copy(out=Asb[:], in_=psA[0:64, :]), R=[psA], W=[Asb])
                        k.op("dve", lambda e, psB=psB, h=h: e.scalar_tensor_tensor(
                            out=hh[:], in0=psB[0:64, :], scalar=S["scv"][0:64, h:h + 1], in1=Asb[:],
                            op0=ALU.mult, op1=ALU.add), R=[psB, S["scv"], Asb], W=[hh])
                        k.op("dve", lambda e, psn=psn, h=h: e.scalar_tensor_tensor(
                            out=st4[:, 1:2], in0=psn[0:64, 0:1], scalar=S["scv"][0:64, h:h + 1], in1=st4[:, 0:1],
                            op0=ALU.mult, op1=ALU.add), R=[psn, S["scv"], st4], W=[st4])
                        k.op("dve", lambda e, h=h: e.tensor_scalar(
                            out=st4[:, 2:3], in0=st4[:, 1:2], scalar1=-1.0, scalar2=S["emt"][0:64, h:h + 1],
                            op0=ALU.mult, op1=ALU.max), R=[st4, S["emt"]], W=[st4])
                        k.op("dve", lambda e, h=h: e.tensor_tensor(
                            out=st4[:, 2:3], in0=st4[:, 2:3], in1=st4[:, 1:2], op=ALU.max), R=[st4], W=[st4])
                        k.op("dve", lambda e: e.reciprocal(out=st4[:, 3:4], in_=st4[:, 2:3]), R=[st4], W=[st4])
                        k.op("dve", lambda e: e.tensor_scalar(out=hh[:], in0=hh[:], scalar1=st4[:, 3:4], scalar2=None,
                                                              op0=ALU.mult), R=[hh, st4], W=[hh])
                        k.op("dve", lambda e: e.reduce_sum(out=st4[:, 4:5], in_=hh[:], axis=AX.X), R=[hh], W=[st4])
                        k.op("dve", lambda e: e.tensor_scalar(out=st4[:, 4:5], in0=st4[:, 4:5], scalar1=-1.0 / 512,
                                                              scalar2=None, op0=ALU.mult), R=[st4], W=[st4])
                        k.op("dve", lambda e: e.tensor_scalar(out=hh[:], in0=hh[:], scalar1=st4[:, 4:5], scalar2=None,
                                                              op0=ALU.add), R=[hh, st4], W=[hh])
                        k.op("dve", lambda e: e.tensor_tensor(out=junk[:], in0=hh[:], in1=hh[:], op=ALU.mult),
                             R=[hh], W=[junk])
                        k.op("dve", lambda e: e.reduce_sum(out=st4[:, 5:6], in_=junk[:], axis=AX.X), R=[junk], W=[st4])
                        k.op("dve", lambda e: e.tensor_scalar(out=st4[:, 5:6], in0=st4[:, 5:6], scalar1=1.0 / 512,
                                                              scalar2=1e-5, op0=ALU.mult, op1=ALU.add), R=[st4], W=[st4])
                        k.op("act", lambda e: e.activation(out=st4[:, 6:7], in_=st4[:, 5:6], func=AF.Sqrt), R=[st4], W=[st4])
                        k.op("dve", lambda e: e.reciprocal(out=st4[:, 7:8], in_=st4[:, 6:7]), R=[st4], W=[st4])
                        k.op("dve", lambda e, hs=hs, yo=yo: e.tensor_scalar(
                            out=yo[:, hs], in0=hh[:], scalar1=st4[:, 7:8], scalar2=None, op0=ALU.mult),
                            R=[hh, st4], W=[yo])
                        k.op("dve", lambda e, h=h, hs=hs: e.tensor_scalar(
                            out=Kw[:], in0=Kc[:, hs], scalar1=S["ws"][0:64, h:h + 1], scalar2=None, op0=ALU.mult),
                            R=[Kc, S["ws"]], W=[Kw])
                        for dcc in range(4):
                            psU = self.bank()
                            k.op("pe", lambda e, psU=psU, dcc=dcc, hs=hs: e.matmul(
                                psU[:], lhsT=Kw[:, dcc * 128:(dcc + 1) * 128], rhs=Vc[:, hs], start=True, stop=True),
                                R=[Kw, Vc], W=[psU])
                            i_ = h * 4 + dcc
                            k.op("dve", lambda e, psU=psU, i_=i_, h=h: e.scalar_tensor_tensor(
                                out=C[:, i_, :], in0=C[:, i_, :], scalar=S["dc"][:, h:h + 1], in1=psU[:],
                                op0=ALU.mult, op1=ALU.add), R=[C, S["dc"], psU], W=[C])
                            k.op("act", lambda e, i_=i_: e.copy(out=Cb[:, i_, :], in_=C[:, i_, :]), R=[C], W=[Cb])
                        psn2 = self.bank()
                        for dcc in range(4):
                            k.op("pe", lambda e, psn2=psn2, dcc=dcc: e.matmul(
                                psn2[:, dcc:dcc + 1], lhsT=Kw[:, dcc * 128:(dcc + 1) * 128], rhs=oneb[:],
                                start=True, stop=True), R=[Kw, oneb], W=[psn2])
                        k.op("dve", lambda e, psn2=psn2, h=h: e.scalar_tensor_tensor(
                            out=nv[:, h * 4:h * 4 + 4], in0=nv[:, h * 4:h * 4 + 4], scalar=S["dc"][:, h:h + 1],
                            in1=psn2[:, 0:4], op0=ALU.mult, op1=ALU.add), R=[nv, S["dc"], psn2], W=[nv])
                        k.op("act", lambda e, h=h: e.copy(out=nvb[:, h * 4:h * 4 + 4], in_=nv[:, h * 4:h * 4 + 4]),
                             R=[nv], W=[nvb])
                    k.dma("sp", y_scr[t0:t0 + L, :], yo[:], R=[yo], W=[y_scr])
        k.barrier()
        if self.upto == "M3":
            return
        with contextlib.ExitStack() as sc:
            wdn = k.sbuf([128, NI, D], BF16, "wdn", sc)
            k.dma("pool", wdn[:], W["c_w_down"].t[0].rearrange("(c p) n -> p c n", p=128), R=[W["c_w_down"]], W=[wdn])
            mhg = k.sbuf([128, NI], F32, "mhg", sc)
            skp = k.sbuf([128, NI], F32, "skp", sc)
            k.dma("sp", mhg[:], W["c_mh_g"].t[0].rearrange("(c p) -> p c", p=128), R=[W["c_mh_g"]], W=[mhg],
                  allow_slow_non_contiguous=True)
            k.dma("sp", skp[:], W["c_skip"].t[0].rearrange("(c p) -> p c", p=128), R=[W["c_skip"]], W=[skp],
                  allow_slow_non_contiguous=True)
            ytk = k.sbuf([128, 4, 2048], F32, "ytk", sc)
            xcb = k.sbuf([128, NI, 512], BF16, "xcb", sc)
            zsb = k.sbuf([128, NI, 512], BF16, "zsb", sc)
            yz = k.sbuf([128, NI, 512], BF16, "yz", sc)
            t1 = [k.sbuf([128, 512], F32, "t1", sc) for _ in range(2)]
            x = k.sbuf([128, NCH, 512], F32, "m4x", sc)
            for tb in range(ntok // 512):
                k.dma("sp", ytk[:], y_scr[tb * 512:(tb + 1) * 512, :].rearrange("(j p) n -> p j n", p=128),
                      R=[y_scr], W=[ytk])
                k.dma("sp", xcb[:], xc_scr[:, :, tb * 512:(tb + 1) * 512], R=[xc_scr], W=[xcb])
                k.dma("sp", zsb[:], zs_scr[:, :, tb * 512:(tb + 1) * 512], R=[zs_scr], W=[zsb])
                k.dma("sp", x[:], self.xT[:, :, tb * 512:(tb + 1) * 512], R=[self.xT], W=[x])
                for c in range(NI):
                    ps = self.bank()
                    for j in range(4):
                        k.op("pe", lambda e, c=c, j=j, ps=ps: e.transpose(
                            ps[:, j * 128:(j + 1) * 128], ytk[:, j, c * 128:(c + 1) * 128], self.c_ident_f[:]),
                            R=[ytk, self.c_ident_f], W=[ps])
                    t = t1[c % 2]
                    k.op("dve", lambda e, c=c, t=t: e.tensor_scalar(
                        out=t[:], in0=xcb[:, c, :], scalar1=skp[:, c:c + 1], scalar2=None, op0=ALU.mult),
                        R=[xcb, skp], W=[t])
                    k.op("dve", lambda e, c=c, t=t, ps=ps: e.scalar_tensor_tensor(
                        out=t[:], in0=ps[:], scalar=mhg[:, c:c + 1], in1=t[:], op0=ALU.mult, op1=ALU.add),
                        R=[ps, mhg, t], W=[t])
                    k.op("dve", lambda e, c=c, t=t: e.tensor_tensor(out=yz[:, c, :], in0=t[:], in1=zsb[:, c, :],
                                                                   op=ALU.mult), R=[t, zsb], W=[yz])
                for n in range(NCH):
                    po = self.bank()
                    for c in range(NI):
                        k.op("pe", lambda e, n=n, c=c, po=po: e.matmul(
                            po[:], lhsT=wdn[:, c, n * 128:(n + 1) * 128], rhs=yz[:, c, :],
                            start=(c == 0), stop=(c == NI - 1)), R=[wdn, yz], W=[po])
                    k.op("dve", lambda e, n=n, po=po: e.tensor_tensor(
                        out=x[:, n, :], in0=x[:, n, :], in1=po[:], op=ALU.add), R=[po, x], W=[x])
                k.dma("sp", self.xT[:, :, tb * 512:(tb + 1) * 512], x[:], R=[x], W=[self.xT])
        k.barrier()

    def build(self):
        ntok = self.ntok
        W = {}
        x_in = self.inp("x", [ntok, D])
        W["p"] = self.inp("p", [2, ntok, PLE_DIM])
        for name, shape in WEIGHT_SHAPES.items():
            W[name] = self.inp(name, shape)
        yt = self.nc.dram_tensor("y", [ntok, D], F32, kind="ExternalOutput")
        y_out = Buf(yt.ap(), "y")
        self.setup_consts()
        self.phase_in(x_in)
        for li in self.layers:
            if "mix" in self.stages and li == 1:
                self.phase_mix1(W)
            if "mix" in self.stages and li == 0:
                self.phase_mix0(W)
            if "ffn" in self.stages:
                self.phase_ffn(li, W)
        self.phase_out(y_out)
        self.k.wait_all("sp", [y_out] + list(self.dumps.values()))
        self.k.close()
        return self.nc


WEIGHT_SHAPES = {
    "mix_norm": [2, 1024], "a_q_gain": [1, 64], "a_k_gain": [1, 64], "w_in_e": [1, 1024, 2756],
    "b_mu": [1, 1792], "b_w0": [1, 512], "b_w2": [1, 64, 512], "b_a0": [1, 512], "b_a2": [1, 64, 512],
    "b_g2": [1, 128, 512], "b_k_k": [1, 512], "b_k_a": [1, 512], "b_r_k": [1, 8, 64], "b_gn_g": [1, 512],
    "b_gn_b": [1, 512], "w_out_e": [1, 1024, 1024], "c_w_up": [1, 1024, 4096], "c_conv_w": [1, 4, 1, 2048],
    "c_conv_b": [1, 2048], "c_wq": [1, 512, 4, 4], "c_wk": [1, 512, 4, 4], "c_wv": [1, 512, 4, 4],
    "c_w_if": [1, 6144, 8], "c_b_i": [1, 4], "c_b_f": [1, 4], "c_mh_g": [1, 2048], "c_skip": [1, 2048],
    "c_w_down": [1, 2048, 1024], "ffn_norm": [2, 1024], "ffn_w_gate": [2, 1024, 2816],
    "ffn_w_up": [2, 1024, 2816], "ffn_w_down": [2, 2816, 1024], "ple_w": [2, 256, 1024],
    "ple_norm": [2, 1024], "ple_w_gate": [2, 1024, 1024],
}


def host_consts():
    i64 = np.arange(64)
    sel = np.zeros((64, 128), np.float32)
    sel[63, :] = 1.0
    return {"c_ident": np.eye(128, dtype=np.float32),
            "c_bdmask": (np.arange(128)[:, None] // 4 == np.arange(32)[None, :]).astype(np.float32),
            "c_tri": (i64[:, None] <= i64[None, :]).astype(np.float32),
            "c_ups": (i64[:, None] < i64[None, :]).astype(np.float32),
            "c_los": (i64[:, None] > i64[None, :]).astype(np.float32),
            "c_bones": (np.arange(128)[:, None] // 64 == np.arange(128)[None, :] // 64).astype(np.float32),
            "c_bd64": ((np.arange(128)[:, None] // 64 == np.arange(128)[None, :] // 64) / 64.0).astype(np.float32),
            "c_mneg2": np.where((np.arange(128)[:, None] < 64) & (np.arange(128)[None, :] >= 64), -1e30, 0.0).astype(np.float32),
            "c_sel": sel,
            "c_mneg": np.where(i64[None, :] <= i64[:, None], 0.0, -1e30).astype(np.float32)}


def run(inputs, ntok_core, ncore=NCORE, layers=(0, 1), stages=("mix", "ffn"), trace=False, dbg=False):
    prog = Prog(ntok_core, layers=layers, stages=stages, dbg=dbg)
    nc = prog.build()
    x = np.ascontiguousarray(inputs["x"]).reshape(-1, D)
    p = np.ascontiguousarray(inputs["p"]).reshape(2, -1, PLE_DIM)
    consts = host_consts()
    in_maps = []
    for c in range(ncore):
        m = {"x": x[c * ntok_core:(c + 1) * ntok_core],
             "p": np.ascontiguousarray(p[:, c * ntok_core:(c + 1) * ntok_core])}
        for name in WEIGHT_SHAPES:
            m[name] = np.ascontiguousarray(inputs[name], dtype=np.float32)
        m.update({kk_: vv_ for kk_, vv_ in consts.items() if kk_ in prog.inputs})
        in_maps.append(m)
    res = run_bass_kernel_spmd(nc, in_maps, core_ids=list(range(ncore)), trace=trace)
    y = np.concatenate([r["y"] for r in res.results], axis=0)
    if dbg:
        res.dumps = {n: res.results[0]["dbg_" + n] for n in prog.dumps}
    return y, res


def kernel(**inputs):
    ntok_core = BATCH * SEQ // NCORE
    y, _ = run(inputs, ntok_core)
    return y.reshape(BATCH, SEQ, D).astype(np.float32)
```

```python
import contextlib
import numpy as np
import ml_dtypes
import concourse.bass as bass
import concourse.mybir as mybir
from concourse.bass_utils import run_bass_kernel_spmd

F32 = mybir.dt.float32
BF16 = mybir.dt.bfloat16
AF = mybir.ActivationFunctionType
ALU = mybir.AluOpType
AX = mybir.AxisListType

D = 1024
NCH = D // 128
SEQ = 2048
BATCH = 32
NCORE = 8
CHUNK = 64
PLE_DIM = 256
FFN_H = 2816
NHC = FFN_H // 128
NORM_EPS = 1e-6


class Sem:
    def __init__(self, handle, name, is_dma=False):
        self.h = handle
        self.name = name
        self.is_dma = is_dma


class Buf:
    def __init__(self, t, name):
        self.t = t
        self.name = name
        self.w = {}
        self.r = {}

    def __getitem__(self, key):
        return self.t[key]


class Eng:
    def __init__(self, name, h, sem):
        self.name = name
        self.h = h
        self.sem = sem
        self.cnt = 0
        self.seen = {}


class K:
    def __init__(self, nc, n_dma_sems=32):
        self.nc = nc
        self.es = contextlib.ExitStack()
        self.eng = {}
        for name, h in [("pe", nc.tensor), ("act", nc.scalar), ("dve", nc.vector),
                        ("pool", nc.gpsimd), ("sp", nc.sync)]:
            self.eng[name] = Eng(name, h, self.new_sem("e_" + name))
        self.dsems = [[self.new_sem(f"d{i}", True), 0] for i in range(n_dma_sems)]
        self.dnext = 0
        self.nbuf = 0

    def new_sem(self, name, is_dma=False):
        return Sem(self.es.enter_context(self.nc.semaphore(name)), name, is_dma)

    def sbuf(self, shape, dtype, name=None, scope=None):
        self.nbuf += 1
        name = f"{name or 'sb'}_{self.nbuf}"
        t = (scope or self.es).enter_context(self.nc.sbuf_tensor(name, list(shape), dtype))
        return Buf(t, name)

    def psum(self, shape, dtype, name=None, scope=None):
        self.nbuf += 1
        name = f"{name or 'ps'}_{self.nbuf}"
        t = (scope or self.es).enter_context(self.nc.psum_tensor(name, list(shape), dtype))
        return Buf(t, name)

    def dram(self, shape, dtype, name, kind="Internal"):
        t = self.nc.dram_tensor(name, list(shape), dtype, kind=kind)
        return Buf(t.ap(), name)

    def _deps(self, e, R, W, skip_self):
        need = {}

        def add(s, v):
            if need.get(s, 0) < v:
                need[s] = v

        for b in R:
            for s, v in b.w.items():
                add(s, v)
        for b in W:
            for s, v in b.w.items():
                add(s, v)
            for s, v in b.r.items():
                add(s, v)
        for s, v in need.items():
            if skip_self and s is e.sem:
                continue
            if e.seen.get(s, 0) >= v:
                continue
            e.h.wait_ge(s.h, v)
            e.seen[s] = v

    def _mark(self, ev, R, W, dma=False):
        s, v = ev
        for b in R:
            if b.r.get(s, 0) < v:
                b.r[s] = v
        for b in W:
            if dma:
                b.w = {ss: vv for ss, vv in b.w.items() if ss.is_dma}
                b.w[s] = v
            else:
                b.w = {s: v}
            b.r = {}

    def barrier(self):
        for e in self.eng.values():
            for o in self.eng.values():
                if o is e or o.cnt == 0:
                    continue
                if e.seen.get(o.sem, 0) < o.cnt:
                    e.h.wait_ge(o.sem.h, o.cnt)
                    e.seen[o.sem] = o.cnt
            for s, v in self.dsems:
                if v > 0 and e.seen.get(s, 0) < v:
                    e.h.wait_ge(s.h, v)
                    e.seen[s] = v

    def op(self, en, fn, R=(), W=()):
        e = self.eng[en]
        self._deps(e, R, W, skip_self=(en == "pe"))
        ins = fn(e.h)
        e.cnt += 1
        ins.then_inc(e.sem.h, 1)
        self._mark((e.sem, e.cnt), R, W)

    def dma(self, en, out, in_, R=(), W=(), **kw):
        e = self.eng[en]
        self._deps(e, R, W, skip_self=False)
        slot = self.dsems[self.dnext]
        self.dnext = (self.dnext + 1) % len(self.dsems)
        s, v = slot
        if v > 0 and e.seen.get(s, 0) < v:
            e.h.wait_ge(s.h, v)
            e.seen[s] = v
        e.h.dma_start(out=out, in_=in_, **kw).then_inc(s.h, 16)
        slot[1] = v + 16
        self._mark((s, v + 16), R, W, dma=True)

    def wait_all(self, en, bufs):
        e = self.eng[en]
        self._deps(e, bufs, (), skip_self=False)

    def close(self):
        self.es.close()


class Prog:
    def __init__(self, ntok, layers=(0, 1), stages=("mix", "ffn"), dbg=False):
        self.ntok = ntok
        self.layers = layers
        self.stages = stages
        nc = bass.Bass("TRN2", target_bir_lowering=False)
        self.nc = nc
        self.k = K(nc)
        self.inputs = {}
        self.dbg = dbg
        self.dumps = {}
        import os
        self.upto = os.environ.get('KUPTO', '')
        self.skip = os.environ.get('KSKIP', '')

    def dump(self, name, buf, ap, shape, dtype=F32):
        if not self.dbg or name in self.dumps:
            return
        t = self.nc.dram_tensor("dbg_" + name, list(shape), dtype, kind="ExternalOutput")
        b = Buf(t.ap(), "dbg_" + name)
        self.dumps[name] = b
        self.k.dma("sp", b[:], ap, R=[buf], W=[b])

    def inp(self, name, shape, dtype=F32):
        if name in self.inputs:
            return self.inputs[name]
        t = self.nc.dram_tensor(name, list(shape), dtype, kind="ExternalInput")
        b = Buf(t.ap(), name)
        self.inputs[name] = b
        return b

    def setup_consts(self):
        k = self.k
        self.c_ident_f = k.sbuf([128, 128], F32, "identf")
        self.c_ident_b = k.sbuf([128, 128], BF16, "identb")
        self.c_mean = k.sbuf([128, 128], BF16, "meanmat")
        cid = self.inp("c_ident", [128, 128])
        k.dma("sp", self.c_ident_f[:], cid[:], R=[cid], W=[self.c_ident_f])
        k.op("dve", lambda e: e.tensor_copy(out=self.c_ident_b[:], in_=self.c_ident_f[:]),
             R=[self.c_ident_f], W=[self.c_ident_b])
        k.op("dve", lambda e: e.memset(self.c_mean[:], 1.0 / D), W=[self.c_mean])
        self.c_eps = k.sbuf([128, 4], F32, "ceps")
        k.op("dve", lambda e: e.memset(self.c_eps[:, 0:1], NORM_EPS), W=[self.c_eps])
        self.ps = [k.psum([128, 512], F32, f"bank{i}") for i in range(8)]
        self.psi = 0
        self.ps_rot = list(range(8))

    def bank(self):
        b = self.ps[self.ps_rot[self.psi % len(self.ps_rot)]]
        self.psi += 1
        return b

    def conv_weight(self, w_ap, Kdim, N, name, stage_bufs):
        k = self.k
        kc = Kdim // 128
        assert kc == 8
        ng = (N + 511) // 512
        dst = k.dram([ng, 128, kc, 512], BF16, name)
        src = w_ap.rearrange("(c p) n -> p c n", p=128)
        for g in range(ng):
            n0, n1 = g * 512, min(N, (g + 1) * 512)
            st = stage_bufs[g % 2]
            k.dma("pool", st[:, 0:kc, 0:n1 - n0], src[:, :, n0:n1], W=[st])
            if n1 - n0 == 512:
                k.dma("sp", dst[g], st[:, 0:kc, :], R=[st], W=[dst])
            else:
                k.dma("sp", dst[g, :, :, 0:n1 - n0], st[:, 0:kc, 0:n1 - n0], R=[st], W=[dst])
        return dst

    def rmsnorm(self, xs, gcol, outs, n, tmp_sq, tmp_rstd, eps=NORM_EPS):
        k = self.k
        ps = self.bank()
        for c in range(NCH):
            xb, xa = xs[c]
            k.op("act", lambda e, xa=xa, c=c: e.activation(out=tmp_sq[:, c, 0:n], in_=xa, func=AF.Square),
                 R=[xb], W=[tmp_sq])
        for c in range(NCH):
            k.op("pe", lambda e, c=c: e.matmul(ps[:, 0:n], lhsT=self.c_mean[:], rhs=tmp_sq[:, c, 0:n],
                                               start=(c == 0), stop=(c == NCH - 1)),
                 R=[self.c_mean, tmp_sq], W=[ps])
        k.op("act", lambda e: e.activation(out=tmp_rstd[:, 0:n], in_=ps[:, 0:n], func=AF.Sqrt, bias=self.c_eps[:, 0:1],
                                           scale=1.0), R=[ps, self.c_eps], W=[tmp_rstd])
        k.op("dve", lambda e: e.reciprocal(out=tmp_rstd[:, 0:n], in_=tmp_rstd[:, 0:n]), R=[tmp_rstd], W=[tmp_rstd])
        self.dump("sq", tmp_sq, tmp_sq[:], [128, NCH, 512], BF16)
        self.dump("rstd", tmp_rstd, tmp_rstd[:], [128, 512], F32)
        gb, ga = gcol
        self.dump("gcol", gb, ga, [128, NCH], F32)
        for c in range(NCH):
            xb, xa = xs[c]
            ob, oa = outs[c]
            k.op("dve", lambda e, xa=xa, oa=oa, c=c: e.scalar_tensor_tensor(
                out=oa, in0=xa, scalar=ga[:, c:c + 1], in1=tmp_rstd[:, 0:n], op0=ALU.mult, op1=ALU.mult),
                R=[xb, gb, tmp_rstd], W=[ob])

    def phase_in(self, x_in):
        k = self.k
        ntok = self.ntok
        self.xT = k.dram([128, NCH, ntok], F32, "xT_scr")
        with contextlib.ExitStack() as sc:
            xin = [k.sbuf([128, 4, D], F32, "xin", sc) for _ in range(2)]
            xo = [k.sbuf([128, NCH, 512], F32, "xo", sc) for _ in range(2)]
            xv = x_in.t.rearrange("(g j p) d -> g p j d", p=128, j=4)
            for g in range(ntok // 512):
                a = xin[g % 2]
                o = xo[g % 2]
                k.dma("sp", a[:], xv[g], R=[x_in], W=[a])
                for c in range(NCH):
                    ps = self.bank()
                    for j in range(4):
                        k.op("pe", lambda e, c=c, j=j, ps=ps: e.transpose(
                            ps[:, j * 128:(j + 1) * 128], a[:, j, c * 128:(c + 1) * 128], self.c_ident_f[:]),
                            R=[a, self.c_ident_f], W=[ps])
                    en = "act" if c % 2 else "dve"
                    if en == "act":
                        k.op("act", lambda e, c=c, ps=ps: e.copy(out=o[:, c, :], in_=ps[:]), R=[ps], W=[o])
                    else:
                        k.op("dve", lambda e, c=c, ps=ps: e.tensor_copy(out=o[:, c, :], in_=ps[:]), R=[ps], W=[o])
                k.dma("sp", self.xT[:, :, g * 512:(g + 1) * 512], o[:], R=[o], W=[self.xT])
        k.barrier()

    def phase_out(self, y_out):
        k = self.k
        ntok = self.ntok
        with contextlib.ExitStack() as sc:
            xi = [k.sbuf([128, NCH, 512], F32, "oxi", sc) for _ in range(2)]
            xo = [k.sbuf([128, 4, D], F32, "oxo", sc) for _ in range(2)]
            yv = y_out.t.rearrange("(g j p) d -> g p j d", p=128, j=4)
            for g in range(ntok // 512):
                a = xi[g % 2]
                o = xo[g % 2]
                k.dma("sp", a[:], self.xT[:, :, g * 512:(g + 1) * 512], R=[self.xT], W=[a])
                for j in range(4):
                    for ch in range(2):
                        ps = self.bank()
                        for cc in range(4):
                            c = ch * 4 + cc
                            k.op("pe", lambda e, c=c, cc=cc, j=j, ps=ps: e.transpose(
                                ps[:, cc * 128:(cc + 1) * 128], a[:, c, j * 128:(j + 1) * 128], self.c_ident_f[:]),
                                R=[a, self.c_ident_f], W=[ps])
                        if ch:
                            k.op("act", lambda e, j=j, ch=ch, ps=ps: e.copy(
                                out=o[:, j, ch * 512:(ch + 1) * 512], in_=ps[:]), R=[ps], W=[o])
                        else:
                            k.op("dve", lambda e, j=j, ch=ch, ps=ps: e.tensor_copy(
                                out=o[:, j, ch * 512:(ch + 1) * 512], in_=ps[:]), R=[ps], W=[o])
                k.dma("sp", yv[g], o[:], R=[o], W=[y_out])
        k.barrier()

    def phase_ffn(self, li, W):
        k = self.k
        ntok = self.ntok
        TB = 512
        with contextlib.ExitStack() as sc:
            stage = [k.sbuf([128, 8, 512], BF16, "cst", sc) for _ in range(2)]
            wg_s = self.conv_weight(W["ffn_w_gate"].t[li], D, FFN_H, f"wg_s{li}", stage)
            wu_s = self.conv_weight(W["ffn_w_up"].t[li], D, FFN_H, f"wu_s{li}", stage)
            wd = k.sbuf([128, NHC, D], BF16, "wd", sc)
            wple = k.sbuf([128, 2, D], BF16, "wple", sc)
            wpg = k.sbuf([128, NCH, D], BF16, "wpg", sc)
            k.dma("pool", wd[:], W["ffn_w_down"].t[li].rearrange("(c p) n -> p c n", p=128),
                  R=[W["ffn_w_down"]], W=[wd])
            k.dma("pool", wple[:], W["ple_w"].t[li].rearrange("(c p) n -> p c n", p=128),
                  R=[W["ple_w"]], W=[wple])
            k.dma("pool", wpg[:], W["ple_w_gate"].t[li].rearrange("(c p) n -> p c n", p=128),
                  R=[W["ple_w_gate"]], W=[wpg])
            gcol = k.sbuf([128, 2, NCH], F32, "gcol", sc)
            k.dma("sp", gcol[:, 0, :], W["ffn_norm"].t[li].rearrange("(c p) -> p c", p=128),
                  R=[W["ffn_norm"]], W=[gcol], allow_slow_non_contiguous=True)
            k.dma("sp", gcol[:, 1, :], W["ple_norm"].t[li].rearrange("(c p) -> p c", p=128),
                  R=[W["ple_norm"]], W=[gcol], allow_slow_non_contiguous=True)
            xb = [k.sbuf([128, NCH, TB], F32, "fx", sc) for _ in range(2)]
            hT = k.sbuf([128, NCH, TB], BF16, "fh", sc)
            sq = k.sbuf([128, NCH, TB], BF16, "fsq", sc)
            rstd = k.sbuf([128, TB], F32, "frstd", sc)
            act = k.sbuf([128, NHC, TB], BF16, "fact", sc)
            wgb = [k.sbuf([128, NCH, 512], BF16, "wgb", sc) for _ in range(2)]
            wub = [k.sbuf([128, NCH, 512], BF16, "wub", sc) for _ in range(2)]
            sg = [k.sbuf([128, TB], F32, "fsg", sc) for _ in range(2)]
            pin = k.sbuf([128, 4, PLE_DIM], F32, "pin", sc)
            pT = k.sbuf([128, 2, TB], BF16, "pT", sc)
            pe_sb = k.sbuf([128, TB], F32, "pesb", sc)
            p_in = W["p"]
            pv = p_in.t[li].rearrange("(g j p) d -> g p j d", p=128, j=4)
            ngrp = (NHC + 3) // 4
            it = 0
            for tb in range(ntok // TB):
                x = xb[tb % 2]
                k.dma("sp", x[:], self.xT[:, :, tb * TB:(tb + 1) * TB], R=[self.xT], W=[x])
                k.dma("sp", pin[:], pv[tb], R=[p_in], W=[pin])
                self.dump(f"xl{li}", x, x[:], [128, NCH, TB], F32)
                self.rmsnorm([(x, x[:, c, :]) for c in range(NCH)], (gcol, gcol[:, 0, :]),
                             [(hT, hT[:, c, :]) for c in range(NCH)], TB, sq, rstd)
                self.dump(f"hT{li}", hT, hT[:], [128, NCH, TB], BF16)
                for jg in range(ngrp):
                    ncol = min(512, FFN_H - jg * 512)
                    wgt, wut = wgb[it % 2], wub[it % 2]
                    it += 1
                    k.dma("sp", wgt[:, :, 0:ncol], wg_s[jg, :, :, 0:ncol], R=[wg_s], W=[wgt])
                    k.dma("sp", wut[:, :, 0:ncol], wu_s[jg, :, :, 0:ncol], R=[wu_s], W=[wut])
                    for jj in range(ncol // 128):
                        j = jg * 4 + jj
                        pg, pu = self.bank(), self.bank()
                        for c in range(NCH):
                            k.op("pe", lambda e, c=c, jj=jj, pg=pg, wgt=wgt: e.matmul(
                                pg[:], lhsT=wgt[:, c, jj * 128:(jj + 1) * 128], rhs=hT[:, c, :],
                                start=(c == 0), stop=(c == NCH - 1)), R=[wgt, hT], W=[pg])
                        for c in range(NCH):
                            k.op("pe", lambda e, c=c, jj=jj, pu=pu, wut=wut: e.matmul(
                                pu[:], lhsT=wut[:, c, jj * 128:(jj + 1) * 128], rhs=hT[:, c, :],
                                start=(c == 0), stop=(c == NCH - 1)), R=[wut, hT], W=[pu])
                        s = sg[j % 2]
                        k.op("act", lambda e, s=s, pg=pg: e.activation(out=s[:], in_=pg[:], func=AF.Silu),
                             R=[pg], W=[s])
                        k.op("dve", lambda e, s=s, pu=pu, j=j: e.tensor_tensor(
                            out=act[:, j, :], in0=s[:], in1=pu[:], op=ALU.mult), R=[s, pu], W=[act])
                self.dump(f"act{li}", act, act[:], [128, NHC, TB], BF16)
                for n in range(NCH):
                    po = self.bank()
                    for j in range(NHC):
                        k.op("pe", lambda e, n=n, j=j, po=po: e.matmul(
                            po[:], lhsT=wd[:, j, n * 128:(n + 1) * 128], rhs=act[:, j, :],
                            start=(j == 0), stop=(j == NHC - 1)), R=[wd, act], W=[po])
                    k.op("dve", lambda e, n=n, po=po, x=x: e.tensor_tensor(
                        out=x[:, n, :], in0=x[:, n, :], in1=po[:], op=ALU.add), R=[po, x], W=[x])
                self.dump(f"xf{li}", x, x[:], [128, NCH, TB], F32)
                for j in range(4):
                    ps = self.bank()
                    for c2 in range(2):
                        k.op("pe", lambda e, j=j, c2=c2, ps=ps: e.transpose(
                            ps[:, c2 * 128:(c2 + 1) * 128], pin[:, j, c2 * 128:(c2 + 1) * 128], self.c_ident_f[:]),
                            R=[pin, self.c_ident_f], W=[ps])
                    k.op("act", lambda e, j=j, ps=ps: e.copy(
                        out=pT[:, :, j * 128:(j + 1) * 128],
                        in_=ps[:, 0:256].rearrange("p (c t) -> p c t", c=2)), R=[ps], W=[pT])
                self.rmsnorm([(x, x[:, c, :]) for c in range(NCH)], (gcol, gcol[:, 1, :]),
                             [(hT, hT[:, c, :]) for c in range(NCH)], TB, sq, rstd)
                for n in range(NCH):
                    pg, pp = self.bank(), self.bank()
                    for c in range(NCH):
                        k.op("pe", lambda e, c=c, n=n, pg=pg: e.matmul(
                            pg[:], lhsT=wpg[:, c, n * 128:(n + 1) * 128], rhs=hT[:, c, :],
                            start=(c == 0), stop=(c == NCH - 1)), R=[wpg, hT], W=[pg])
                    for c in range(2):
                        k.op("pe", lambda e, c=c, n=n, pp=pp: e.matmul(
                            pp[:], lhsT=wple[:, c, n * 128:(n + 1) * 128], rhs=pT[:, c, :],
                            start=(c == 0), stop=(c == 1)), R=[wple, pT], W=[pp])
                    s = sg[n % 2]
                    k.op("act", lambda e, s=s, pg=pg: e.activation(out=s[:], in_=pg[:], func=AF.Sigmoid),
                         R=[pg], W=[s])
                    k.op("dve", lambda e, s=s, pp=pp: e.tensor_tensor(
                        out=pe_sb[:], in0=s[:], in1=pp[:], op=ALU.mult), R=[s, pp], W=[pe_sb])
                    k.op("dve", lambda e, n=n, x=x: e.tensor_tensor(
                        out=x[:, n, :], in0=x[:, n, :], in1=pe_sb[:], op=ALU.add), R=[pe_sb, x], W=[x])
                k.dma("sp", self.xT[:, :, tb * TB:(tb + 1) * TB], x[:], R=[x], W=[self.xT])
        k.barrier()


    def bank_rot(self, idxs):
        self.ps_rot = list(idxs)
        self.psi = 0

    def phase_mix0(self, W):
        k = self.k
        ntok = self.ntok
        T = min(SEQ, ntok)
        nseq = ntok // T
        NQT = T // 128
        NB = 14
        BBASE = 964
        qn_scr = k.dram([128, 4, ntok], BF16, "qn_scr")
        kn_scr = k.dram([128, ntok], BF16, "kn_scr")
        iq_scr = k.dram([128, 2, ntok], BF16, "iq_scr")
        ik_scr = k.dram([128, ntok], BF16, "ik_scr")
        va_scr = k.dram([ntok, 65], BF16, "va_scr")
        iw_scr = k.dram([ntok, 4], F32, "iw_scr")
        ub_scr = k.dram([128, NB, nseq, T + 4], F32, "ub_scr")
        ya_scr = k.dram([128, 4, ntok], BF16, "ya_scr")
        yb_scr = k.dram([128, 4, ntok], BF16, "yb_scr")
        self.yb_scr = yb_scr
        with contextlib.ExitStack() as sc:
            stage = [k.sbuf([128, 8, 512], BF16, "cst", sc) for _ in range(2)]
            win_s = self.conv_weight(W["w_in_e"].t[0], D, 2756, "win_s", stage)
            win = k.sbuf([128, NCH, 2756], BF16, "win", sc)
            for g_ in range(6):
                nc_ = min(512, 2756 - g_ * 512)
                k.dma("sp", win[:, :, g_ * 512:g_ * 512 + nc_], win_s[g_, :, :, 0:nc_], R=[win_s], W=[win])
            gcol = k.sbuf([128, NCH], F32, "gcol", sc)
            k.dma("sp", gcol[:], W["mix_norm"].t[0].rearrange("(c p) -> p c", p=128),
                  R=[W["mix_norm"]], W=[gcol], allow_slow_non_contiguous=True)
            gqk = k.sbuf([128, 2], F32, "gqk", sc)
            for half in range(2):
                k.dma("sp", gqk[half * 64:(half + 1) * 64, 0:1], W["a_q_gain"].t[0].rearrange("(p o) -> p o", o=1),
                      R=[W["a_q_gain"]], W=[gqk])
                k.dma("sp", gqk[half * 64:(half + 1) * 64, 1:2], W["a_k_gain"].t[0].rearrange("(p o) -> p o", o=1),
                      R=[W["a_k_gain"]], W=[gqk])
            k.op("dve", lambda e: e.tensor_scalar(out=gqk[:, 0:1], in0=gqk[:, 0:1], scalar1=0.125, scalar2=None,
                                                  op0=ALU.mult), R=[gqk], W=[gqk])
            bd64f = k.sbuf([128, 128], F32, "bd64f", sc)
            bd64 = k.sbuf([128, 128], BF16, "bd64", sc)
            cb64 = self.inp("c_bd64", [128, 128])
            k.dma("sp", bd64f[:], cb64[:], R=[cb64], W=[bd64f])
            k.op("dve", lambda e: e.tensor_copy(out=bd64[:], in_=bd64f[:]), R=[bd64f], W=[bd64])
            zt = k.sbuf([128, NB, 4], F32, "zt", sc)
            k.op("dve", lambda e: e.memset(zt[:], 0.0), W=[zt])
            for s_ in range(nseq):
                k.dma("sp", ub_scr[:, :, s_, 0:4], zt[:], R=[zt], W=[ub_scr])
            xb = [k.sbuf([128, NCH, 512], F32, "a1x", sc) for _ in range(2)]
            hT = k.sbuf([128, NCH, 512], BF16, "a1h", sc)
            sq = k.sbuf([128, NCH, 512], BF16, "a1sq", sc)
            rstd = k.sbuf([128, 512], F32, "a1rstd", sc)
            qsq = k.sbuf([128, 512], BF16, "qsq", sc)
            qr = k.sbuf([128, 512], F32, "qr", sc)
            qno = k.sbuf([128, 4, 512], BF16, "qno", sc)
            kno = k.sbuf([128, 512], BF16, "kno", sc)
            iqo = k.sbuf([128, 2, 512], BF16, "iqo", sc)
            iko = k.sbuf([128, 512], BF16, "iko", sc)
            ubo = k.sbuf([128, NB, 512], F32, "ubo", sc)
            vao = k.sbuf([128, 4, 65], BF16, "vao", sc)
            iwo = k.sbuf([128, 4, 4], F32, "iwo", sc)
            k.op("dve", lambda e: e.memset(vao[:], 1.0), W=[vao])

            def proj(ps, col0, ncol, po=0):
                for c in range(NCH):
                    k.op("pe", lambda e, c=c: e.matmul(ps[po:po + ncol, :], lhsT=win[:, c, col0:col0 + ncol],
                                                      rhs=hT[:, c, :], start=(c == 0), stop=(c == NCH - 1)),
                         R=[win, hT], W=[ps])

            def qknorm(ps, gi, out_ap, out_buf):
                k.op("act", lambda e: e.activation(out=qsq[:], in_=ps[:], func=AF.Square), R=[ps], W=[qsq])
                ps2 = self.bank()
                k.op("pe", lambda e: e.matmul(ps2[:], lhsT=bd64[:], rhs=qsq[:], start=True, stop=True),
                     R=[bd64, qsq], W=[ps2])
                k.op("act", lambda e: e.activation(out=qr[:], in_=ps2[:], func=AF.Sqrt, bias=self.c_eps[:, 0:1],
                                                   scale=1.0), R=[ps2, self.c_eps], W=[qr])
                k.op("dve", lambda e: e.reciprocal(out=qr[:], in_=qr[:]), R=[qr], W=[qr])
                k.op("dve", lambda e: e.scalar_tensor_tensor(out=out_ap, in0=ps[:], scalar=gqk[:, gi:gi + 1],
                                                             in1=qr[:], op0=ALU.mult, op1=ALU.mult),
                     R=[ps, gqk, qr], W=[out_buf])

            for tb in range(ntok // 512):
                s_, off = (tb * 512) // T, (tb * 512) % T
                x = xb[tb % 2]
                k.dma("sp", x[:], self.xT[:, :, tb * 512:(tb + 1) * 512], R=[self.xT], W=[x])
                self.rmsnorm([(x, x[:, c, :]) for c in range(NCH)], (gcol, gcol[:, :]),
                             [(hT, hT[:, c, :]) for c in range(NCH)], 512, sq, rstd)
                for j in range(4):
                    ps = self.bank()
                    proj(ps, j * 128, 128)
                    qknorm(ps, 0, qno[:, j, :], qno)
                ps = self.bank()
                proj(ps, 512, 64, 0)
                proj(ps, 512, 64, 64)
                qknorm(ps, 1, kno[:], kno)
                for j in range(2):
                    ps = self.bank()
                    proj(ps, 640 + j * 128, 128)
                    k.op("act", lambda e, j=j, ps=ps: e.copy(out=iqo[:, j, :], in_=ps[:]), R=[ps], W=[iqo])
                ps = self.bank()
                proj(ps, 896, 64, 0)
                proj(ps, 896, 64, 64)
                k.op("act", lambda e, ps=ps: e.copy(out=iko[:], in_=ps[:]), R=[ps], W=[iko])
                for g in range(NB):
                    ps = self.bank()
                    proj(ps, BBASE + g * 128, 128)
                    if g % 2:
                        k.op("act", lambda e, g=g, ps=ps: e.copy(out=ubo[:, g, :], in_=ps[:]), R=[ps], W=[ubo])
                    else:
                        k.op("dve", lambda e, g=g, ps=ps: e.tensor_copy(out=ubo[:, g, :], in_=ps[:]), R=[ps], W=[ubo])
                for tt in range(4):
                    ps = self.bank()
                    for c in range(NCH):
                        k.op("pe", lambda e, c=c, tt=tt, ps=ps: e.matmul(
                            ps[:, 0:64], lhsT=hT[:, c, tt * 128:(tt + 1) * 128], rhs=win[:, c, 576:640],
                            start=(c == 0), stop=(c == NCH - 1)), R=[hT, win], W=[ps])
                    for c in range(NCH):
                        k.op("pe", lambda e, c=c, tt=tt, ps=ps: e.matmul(
                            ps[:, 64:68], lhsT=hT[:, c, tt * 128:(tt + 1) * 128], rhs=win[:, c, 960:964],
                            start=(c == 0), stop=(c == NCH - 1)), R=[hT, win], W=[ps])
                    k.op("act", lambda e, tt=tt, ps=ps: e.copy(out=vao[:, tt, 0:64], in_=ps[:, 0:64]), R=[ps], W=[vao])
                    k.op("act", lambda e, tt=tt, ps=ps: e.copy(out=iwo[:, tt, :], in_=ps[:, 64:68]), R=[ps], W=[iwo])
                tsl = slice(tb * 512, (tb + 1) * 512)
                k.dma("sp", qn_scr[:, :, tsl], qno[:], R=[qno], W=[qn_scr])
                k.dma("sp", kn_scr[:, tsl], kno[:], R=[kno], W=[kn_scr])
                k.dma("sp", iq_scr[:, :, tsl], iqo[:], R=[iqo], W=[iq_scr])
                k.dma("sp", ik_scr[:, tsl], iko[:], R=[iko], W=[ik_scr])
                k.dma("sp", ub_scr[:, :, s_, 4 + off:4 + off + 512], ubo[:], R=[ubo], W=[ub_scr])
                k.dma("sp", va_scr[tsl, :].rearrange("(j p) n -> p j n", p=128), vao[:], R=[vao], W=[va_scr])
                k.dma("sp", iw_scr[tsl, :].rearrange("(j p) n -> p j n", p=128), iwo[:], R=[iwo], W=[iw_scr])
        k.barrier()
        if self.upto == "A1":
            return
        if "rwkv" not in self.skip:
            self.phase_rwkv(W, ub_scr, yb_scr)
        with contextlib.ExitStack() as sc:
            NBK = T // 128
            KSEL = min(256, T // 4)
            qn = k.sbuf([128, 4, T], BF16, "qn", sc)
            kn = k.sbuf([128, T], BF16, "kn", sc)
            iq = k.sbuf([128, 2, T], BF16, "iq", sc)
            ik = k.sbuf([128, T], BF16, "ik", sc)
            va = k.sbuf([128, NBK, 66], BF16, "va", sc)
            iw = k.sbuf([128, NBK, 4], F32, "iw", sc)
            score = k.sbuf([128, T], F32, "score", sc)
            tmp = [k.sbuf([128, 512], F32, "stmp", sc) for _ in range(2)]
            junk = k.sbuf([128, T], BF16, "sjunk", sc)
            maskf = k.sbuf([128, T], F32, "maskf", sc)
            maskT = k.sbuf([128, NBK, 128], BF16, "maskT", sc)
            mneg2 = k.sbuf([128, 128], F32, "mneg2", sc)
            cm2 = self.inp("c_mneg2", [128, 128])
            k.dma("sp", mneg2[:], cm2[:], R=[cm2], W=[mneg2])
            bs = k.sbuf([128, 8], F32, "bs", sc)
            psb = [k.sbuf([128, 512], BF16, "psb", sc) for _ in range(2)]
            pm = [k.sbuf([128, 4, 128], BF16, "pm", sc) for _ in range(2)]
            ya = k.sbuf([128, 512], F32, "ya", sc)
            yaT = k.sbuf([128, 4, 128], BF16, "yaT", sc)
            rec = k.sbuf([128, 8], F32, "rec", sc)
            po = [self.ps[0], self.ps[1]]
            self.bank_rot(range(2, 8))
            k.op("dve", lambda e: e.memset(va[:], 0.0), W=[va])
            for s_ in range(nseq):
                ssl = slice(s_ * T, (s_ + 1) * T)
                k.dma("sp", qn[:], qn_scr[:, :, ssl], R=[qn_scr], W=[qn])
                k.dma("sp", kn[:], kn_scr[:, ssl], R=[kn_scr], W=[kn])
                k.dma("sp", iq[:], iq_scr[:, :, ssl], R=[iq_scr], W=[iq])
                k.dma("sp", ik[:], ik_scr[:, ssl], R=[ik_scr], W=[ik])
                k.dma("sp", va[:, :, 0:65], va_scr[ssl, :].rearrange("(j p) n -> p j n", p=128), R=[va_scr], W=[va])
                k.dma("sp", iw[:], iw_scr[ssl, :].rearrange("(j p) n -> p j n", p=128), R=[iw_scr], W=[iw])
                for qt in range(NQT):
                    nb = qt + 1
                    S = nb * 128
                    qsl = slice(qt * 128, (qt + 1) * 128)
                    for kb in range(0, S, 512):
                        n = min(512, S - kb)
                        for h in range(4):
                            hp, j = h % 2, h // 2
                            ps = self.bank()
                            k.op("pe", lambda e, ps=ps, hp=hp, j=j, kb=kb, n=n: e.matmul(
                                ps[:, 0:n], lhsT=iq[hp * 64:(hp + 1) * 64, j, qsl], rhs=ik[hp * 64:(hp + 1) * 64, kb:kb + n],
                                start=True, stop=True), R=[iq, ik], W=[ps])
                            if h == 0:
                                k.op("dve", lambda e, ps=ps, kb=kb, n=n, h=h: e.tensor_scalar(
                                    out=score[:, kb:kb + n], in0=ps[:, 0:n], scalar1=0.0, scalar2=iw[:, qt, h:h + 1],
                                    op0=ALU.max, op1=ALU.mult), R=[ps, iw], W=[score])
                            else:
                                t_ = tmp[h % 2]
                                k.op("dve", lambda e, ps=ps, n=n, h=h, t_=t_: e.tensor_scalar(
                                    out=t_[:, 0:n], in0=ps[:, 0:n], scalar1=0.0, scalar2=iw[:, qt, h:h + 1],
                                    op0=ALU.max, op1=ALU.mult), R=[ps, iw], W=[t_])
                                k.op("pool", lambda e, kb=kb, n=n, t_=t_: e.tensor_tensor(
                                    out=score[:, kb:kb + n], in0=score[:, kb:kb + n], in1=t_[:, 0:n], op=ALU.add),
                                    R=[score, t_], W=[score])
                    if 'a4s1' in self.skip:
                        continue
                    k.op("dve", lambda e, S=S: e.tensor_tensor(out=score[:, S - 128:S], in0=score[:, S - 128:S],
                                                               in1=mneg2[:], op=ALU.add), R=[score, mneg2], W=[score])
                    if S > KSEL:
                        k.op("dve", lambda e, S=S: e.reduce_max(out=bs[:, 1:2], in_=score[:, 0:S], axis=AX.X),
                             R=[score], W=[bs])
                        RNG = 512.0
                        k.op("dve", lambda e: e.tensor_scalar(out=bs[:, 0:1], in0=bs[:, 1:2], scalar1=1.0 - RNG,
                                                              scalar2=None, op0=ALU.add), R=[bs], W=[bs])
                        wd_ = RNG
                        for it_ in range(20):
                            wd_ = wd_ * 0.5
                            k.op("dve", lambda e, S=S, wd_=wd_: e.tensor_scalar(
                                out=junk[:, 0:S], in0=score[:, 0:S], scalar1=bs[:, 0:1], scalar2=wd_,
                                op0=ALU.subtract, op1=ALU.is_ge), R=[score, bs], W=[junk])
                            k.op("dve", lambda e, S=S: e.reduce_sum(out=bs[:, 3:4], in_=junk[:, 0:S], axis=AX.X),
                                 R=[junk], W=[bs])
                            k.op("dve", lambda e, wd_=wd_: e.tensor_scalar(out=bs[:, 4:5], in0=bs[:, 3:4], scalar1=KSEL - 0.5,
                                                                           scalar2=wd_, op0=ALU.is_ge, op1=ALU.mult),
                                 R=[bs], W=[bs])
                            k.op("dve", lambda e: e.tensor_tensor(out=bs[:, 0:1], in0=bs[:, 0:1], in1=bs[:, 4:5],
                                                                  op=ALU.add), R=[bs], W=[bs])
                    else:
                        k.op("dve", lambda e: e.memset(bs[:, 0:1], -1e29), W=[bs])
                    if 'a4s2' in self.skip:
                        continue
                    k.op("dve", lambda e, S=S: e.tensor_scalar(out=maskf[:, 0:S], in0=score[:, 0:S], scalar1=bs[:, 0:1],
                                                               scalar2=None, op0=ALU.is_ge), R=[score, bs], W=[maskf])
                    for b4 in range(0, nb, 4):
                        nn = min(4, nb - b4)
                        ps = self.bank()
                        for bb in range(nn):
                            b = b4 + bb
                            k.op("pe", lambda e, ps=ps, bb=bb, b=b: e.transpose(
                                ps[:, bb * 128:(bb + 1) * 128], maskf[:, b * 128:(b + 1) * 128], self.c_ident_f[:]),
                                R=[maskf, self.c_ident_f], W=[ps])
                        k.op("act", lambda e, ps=ps, b4=b4, nn=nn: e.copy(
                            out=maskT[:, b4:b4 + nn, :], in_=ps[:, 0:nn * 128].rearrange("p (b t) -> p b t", t=128)),
                            R=[ps], W=[maskT])
                    if 'a4s3' in self.skip:
                        continue
                    for hg in range(2):
                        k.op("dve", lambda e, hg=hg: e.memset(po[hg][:], 0.0), W=[po[hg]])
                    for b in range(nb):
                        bsl = slice(b * 128, (b + 1) * 128)
                        for hg in range(2):
                            ps = self.bank()
                            for hh in range(4):
                                hp, j = hg, hh
                                k.op("pe", lambda e, ps=ps, hh=hh, hp=hp, j=j, bsl=bsl: e.matmul(
                                    ps[:, hh * 128:(hh + 1) * 128], lhsT=kn[hp * 64:(hp + 1) * 64, bsl],
                                    rhs=qn[hp * 64:(hp + 1) * 64, j, qsl], start=True, stop=True), R=[kn, qn], W=[ps])
                            pb = psb[hg]
                            pmm = pm[hg]
                            k.op("act", lambda e, ps=ps, pb=pb: e.activation(out=pb[:], in_=ps[:], func=AF.Exp),
                                 R=[ps], W=[pb])
                            if 'a4qk' in self.skip:
                                continue
                            for hh in range(4):
                                k.op("pool" if hh % 2 else "dve", lambda e, pb=pb, pmm=pmm, b=b, hh=hh: e.tensor_tensor(
                                    out=pmm[:, hh, :], in0=pb[:, hh * 128:(hh + 1) * 128],
                                    in1=maskT[:, b, :], op=ALU.mult), R=[pb, maskT], W=[pmm])
                            if 'a4pm' in self.skip:
                                continue
                            for hh in range(4):
                                k.op("pe", lambda e, hg=hg, hh=hh, pmm=pmm, b=b, nb=nb: e.matmul(
                                    po[hg][:, hh * 128:hh * 128 + 66], lhsT=pmm[:, hh, :], rhs=va[:, b, 0:66],
                                    start=False, stop=(b == nb - 1), skip_group_check=True), R=[pmm, va], W=[po[hg]])
                    if 'a4s4' in self.skip:
                        continue
                    for hg in range(2):
                        for hh in range(4):
                            h = 2 * hh + hg
                            k.op("dve", lambda e, hg=hg, hh=hh, h=h: e.reciprocal(
                                out=rec[:, h:h + 1], in_=po[hg][:, hh * 128 + 64:hh * 128 + 65]), R=[po[hg]], W=[rec])
                            k.op("dve", lambda e, hg=hg, hh=hh, h=h: e.tensor_scalar(
                                out=ya[:, h * 64:(h + 1) * 64], in0=po[hg][:, hh * 128:hh * 128 + 64],
                                scalar1=rec[:, h:h + 1], scalar2=None, op0=ALU.mult), R=[po[hg], rec], W=[ya])
                    ps = self.bank()
                    for j in range(4):
                        k.op("pe", lambda e, ps=ps, j=j: e.transpose(
                            ps[:, j * 128:(j + 1) * 128], ya[:, j * 128:(j + 1) * 128], self.c_ident_f[:]),
                            R=[ya, self.c_ident_f], W=[ps])
                    k.op("act", lambda e, ps=ps: e.copy(out=yaT[:], in_=ps[:].rearrange("p (j t) -> p j t", j=4)),
                         R=[ps], W=[yaT])
                    t0 = s_ * T + qt * 128
                    k.dma("sp", ya_scr[:, :, t0:t0 + 128], yaT[:], R=[yaT], W=[ya_scr])
            self.bank_rot(range(8))
        k.barrier()
        with contextlib.ExitStack() as sc:
            wo = k.sbuf([128, NCH, D], BF16, "wo", sc)
            k.dma("pool", wo[:], W["w_out_e"].t[0].rearrange("(c p) n -> p c n", p=128), R=[W["w_out_e"]], W=[wo])
            yab = k.sbuf([128, NCH, 512], BF16, "yab", sc)
            x = k.sbuf([128, NCH, 512], F32, "a5x", sc)
            if "rwkv" in self.skip:
                k.op("dve", lambda e: e.memset(yab[:], 0.0), W=[yab])
            for tb in range(ntok // 512):
                tsl = slice(tb * 512, (tb + 1) * 512)
                k.dma("sp", yab[:, 0:4, :], ya_scr[:, :, tsl], R=[ya_scr], W=[yab])
                if "rwkv" not in self.skip:
                    k.dma("sp", yab[:, 4:8, :], yb_scr[:, :, tsl], R=[yb_scr], W=[yab])
                k.dma("sp", x[:], self.xT[:, :, tsl], R=[self.xT], W=[x])
                for n in range(NCH):
                    pso = self.bank()
                    for c in range(NCH):
                        k.op("pe", lambda e, n=n, c=c, pso=pso: e.matmul(
                            pso[:], lhsT=wo[:, c, n * 128:(n + 1) * 128], rhs=yab[:, c, :],
                            start=(c == 0), stop=(c == NCH - 1)), R=[wo, yab], W=[pso])
                    k.op("dve", lambda e, n=n, pso=pso: e.tensor_tensor(
                        out=x[:, n, :], in0=x[:, n, :], in1=pso[:], op=ALU.add), R=[pso, x], W=[x])
                k.dma("sp", self.xT[:, :, tsl], x[:], R=[x], W=[self.xT])
        k.barrier()

    def phase_rwkv(self, W, ub_scr, yb_scr):
        k = self.k
        ntok = self.ntok
        T = min(SEQ, ntok)
        nseq = ntok // T
        NB = 14
        names = ["At", "rt", "Bt", "kt", "v", "W", "bon", "g"]
        scr = {n_: k.dram([128, 4, ntok], F32, "rw_" + n_) for n_ in names}
        with contextlib.ExitStack() as sc:
            w2a2 = k.sbuf([128, 512], BF16, "w2a2", sc)
            g2 = k.sbuf([128, 512], BF16, "g2", sc)
            k.dma("pool", w2a2[0:64, :], W["b_w2"].t[0], R=[W["b_w2"]], W=[w2a2])
            k.dma("pool", w2a2[64:128, :], W["b_a2"].t[0], R=[W["b_a2"]], W=[w2a2])
            k.dma("pool", g2[:], W["b_g2"].t[0], R=[W["b_g2"]], W=[g2])
            cols = k.sbuf([128, 8, 4], F32, "rwcols", sc)
            for i_, nm in enumerate(["b_w0", "b_a0", "b_k_k", "b_k_a", "b_r_k"]):
                src = W[nm].t[0]
                if nm == "b_r_k":
                    src = src.rearrange("h d -> (h d)")
                k.dma("sp", cols[:, i_, :], src.rearrange("(c p) -> p c", p=128), R=[W[nm]], W=[cols],
                      allow_slow_non_contiguous=True)
            k.op("dve", lambda e: e.tensor_scalar(out=cols[:, 5, :], in0=cols[:, 0, :], scalar1=-1.0, scalar2=None,
                                                  op0=ALU.mult), R=[cols], W=[cols])
            k.op("dve", lambda e: e.memset(cols[:, 6, :], -0.5), W=[cols])
            k.op("dve", lambda e: e.memset(cols[:, 7, :], 1.0), W=[cols])
            mu = k.sbuf([128, NB], F32, "mu", sc)
            k.dma("sp", mu[:], W["b_mu"].t[0].rearrange("(c p) -> p c", p=128), R=[W["b_mu"]], W=[mu],
                  allow_slow_non_contiguous=True)
            bones = k.sbuf([128, 128], F32, "bones", sc)
            cbo = self.inp("c_bones", [128, 128])
            k.dma("sp", bones[:], cbo[:], R=[cbo], W=[bones])
            ubh = k.sbuf([128, NB, 516], F32, "ubh", sc)
            xs = k.sbuf([128, NB, 512], F32, "xs", sc)
            twa = k.sbuf([128, 512], BF16, "twa", sc)
            sxg = k.sbuf([128, 512], BF16, "sxg", sc)
            o = {n_: k.sbuf([128, 4, 512], F32, "o_" + n_, sc) for n_ in names}
            tt_ = {n_: k.sbuf([128, 512], F32, "t_" + n_, sc) for n_ in
                   ("t1", "e2", "a", "kk", "sq", "t2", "t3", "k", "cwA", "cwB", "Wex", "Winv")}
            for tb in range(ntok // 512):
                s_, off = (tb * 512) // T, (tb * 512) % T
                k.dma("sp", ubh[:], ub_scr[:, :, s_, off:off + 516], R=[ub_scr], W=[ubh])
                for g in range(NB):
                    k.op("pool", lambda e, g=g: e.tensor_tensor(out=xs[:, g, :], in0=ubh[:, g, 3:515], in1=ubh[:, g, 4:516],
                                                                op=ALU.subtract), R=[ubh], W=[xs])
                    k.op("dve", lambda e, g=g: e.scalar_tensor_tensor(out=xs[:, g, :], in0=xs[:, g, :], scalar=mu[:, g:g + 1],
                                                                      in1=ubh[:, g, 4:516], op0=ALU.mult, op1=ALU.add),
                         R=[xs, mu, ubh], W=[xs])
                k.op("act", lambda e: e.activation(out=twa[0:64, :], in_=xs[0:64, 12, :], func=AF.Tanh), R=[xs], W=[twa])
                k.op("act", lambda e: e.copy(out=twa[64:128, :], in_=xs[64:128, 12, :]), R=[xs], W=[twa])
                k.op("act", lambda e: e.activation(out=sxg[:], in_=xs[:, 13, :], func=AF.Sigmoid), R=[xs], W=[sxg])
                for p in range(4):
                    r_, k0_, v_ = xs[:, p, :], xs[:, 4 + p, :], xs[:, 8 + p, :]
                    psl = slice(p * 128, (p + 1) * 128)
                    ps1 = self.bank()
                    k.op("pe", lambda e, ps1=ps1, psl=psl: e.matmul(ps1[:], lhsT=w2a2[0:64, psl], rhs=twa[0:64, :],
                                                                    start=True, stop=True), R=[w2a2, twa], W=[ps1])
                    ps2 = self.bank()
                    k.op("pe", lambda e, ps2=ps2, psl=psl: e.matmul(ps2[:], lhsT=w2a2[64:128, psl], rhs=twa[64:128, :],
                                                                    start=True, stop=True), R=[w2a2, twa], W=[ps2])
                    ps3 = self.bank()
                    k.op("pe", lambda e, ps3=ps3, psl=psl: e.matmul(ps3[:], lhsT=g2[:, psl], rhs=sxg[:],
                                                                    start=True, stop=True), R=[g2, sxg], W=[ps3])
                    t1, e2, a_, kk_, sq_, t2, t3, k_ = (tt_[n_] for n_ in ("t1", "e2", "a", "kk", "sq", "t2", "t3", "k"))
                    cwA, cwB, Wex, Winv = (tt_[n_] for n_ in ("cwA", "cwB", "Wex", "Winv"))
                    k.op("act", lambda e, ps1=ps1, p=p: e.activation(out=t1[:], in_=ps1[:], func=AF.Exp, scale=-1.0,
                                                                     bias=cols[:, 5, p:p + 1]), R=[ps1, cols], W=[t1])
                    k.op("act", lambda e: e.activation(out=t1[:], in_=t1[:], func=AF.Ln, scale=1.0, bias=cols[:, 7, 0:1]),
                         R=[t1, cols], W=[t1])
                    k.op("act", lambda e: e.activation(out=e2[:], in_=t1[:], func=AF.Exp, scale=-1.0, bias=cols[:, 6, 0:1]),
                         R=[t1, cols], W=[e2])
                    k.op("act", lambda e, ps2=ps2, p=p: e.activation(out=a_[:], in_=ps2[:], func=AF.Sigmoid, scale=1.0,
                                                                     bias=cols[:, 1, p:p + 1]), R=[ps2, cols], W=[a_])
                    k.op("act", lambda e, ps3=ps3, p=p: e.copy(out=o["g"][:, p, :], in_=ps3[:]), R=[ps3], W=[o["g"]])
                    k.op("dve", lambda e, p=p, k0_=k0_: e.tensor_scalar(out=kk_[:], in0=k0_, scalar1=cols[:, 2, p:p + 1],
                                                                       scalar2=None, op0=ALU.mult), R=[xs, cols], W=[kk_])
                    k.op("pool", lambda e: e.tensor_tensor(out=sq_[:], in0=kk_[:], in1=kk_[:], op=ALU.mult), R=[kk_], W=[sq_])
                    ps4 = self.bank()
                    k.op("pe", lambda e, ps4=ps4: e.matmul(ps4[:], lhsT=bones[:], rhs=sq_[:], start=True, stop=True),
                         R=[bones, sq_], W=[ps4])
                    k.op("act", lambda e, ps4=ps4: e.activation(out=t2[:], in_=ps4[:], func=AF.Sqrt), R=[ps4], W=[t2])
                    k.op("dve", lambda e: e.tensor_scalar(out=t2[:], in0=t2[:], scalar1=1e-12, scalar2=None, op0=ALU.max),
                         R=[t2], W=[t2])
                    k.op("dve", lambda e: e.reciprocal(out=t2[:], in_=t2[:]), R=[t2], W=[t2])
                    k.op("dve", lambda e: e.tensor_tensor(out=kk_[:], in0=kk_[:], in1=t2[:], op=ALU.mult), R=[kk_, t2], W=[kk_])
                    k.op("dve", lambda e, p=p: e.tensor_scalar(out=t3[:], in0=a_[:], scalar1=-1.0, scalar2=cols[:, 3, p:p + 1],
                                                              op0=ALU.add, op1=ALU.mult), R=[a_, cols], W=[t3])
                    k.op("dve", lambda e, k0_=k0_: e.scalar_tensor_tensor(out=k_[:], in0=t3[:], scalar=1.0, in1=k0_,
                                                                         op0=ALU.add, op1=ALU.mult), R=[t3, xs], W=[k_])
                    k.op("dve", lambda e, p=p, r_=r_: e.scalar_tensor_tensor(out=t3[:], in0=r_, scalar=cols[:, 4, p:p + 1],
                                                                            in1=k_[:], op0=ALU.mult, op1=ALU.mult),
                         R=[xs, cols, k_], W=[t3])
                    ps5 = self.bank()
                    k.op("pe", lambda e, ps5=ps5: e.matmul(ps5[:], lhsT=bones[:], rhs=t3[:], start=True, stop=True),
                         R=[bones, t3], W=[ps5])
                    k.op("dve", lambda e, ps5=ps5, p=p, v_=v_: e.tensor_tensor(out=o["bon"][:, p, :], in0=v_, in1=ps5[:],
                                                                              op=ALU.mult), R=[xs, ps5], W=[o["bon"]])
                    k.op("dve", lambda e: e.tensor_scalar(out=cwA[:], in0=e2[:], scalar1=-1.0, scalar2=None, op0=ALU.mult),
                         R=[e2], W=[cwA])
                    src_, dst_ = cwA, cwB
                    for j in (1, 2, 4, 8, 16, 32):
                        sv = src_[:].rearrange("p (c l) -> p c l", l=64)
                        dv = dst_[:].rearrange("p (c l) -> p c l", l=64)
                        k.op("pool", lambda e, sv=sv, dv=dv, j=j: e.tensor_copy(out=dv[:, :, 0:j], in_=sv[:, :, 0:j]),
                             R=[src_], W=[dst_])
                        k.op("dve", lambda e, sv=sv, dv=dv, j=j: e.tensor_tensor(out=dv[:, :, j:64], in0=sv[:, :, j:64],
                                                                                 in1=sv[:, :, 0:64 - j], op=ALU.add),
                             R=[src_], W=[dst_])
                        src_, dst_ = dst_, src_
                    cw = src_
                    k.op("act", lambda e, p=p, cw=cw: e.activation(out=o["W"][:, p, :], in_=cw[:], func=AF.Exp), R=[cw], W=[o["W"]])
                    k.op("act", lambda e, cw=cw: e.activation(out=Winv[:], in_=cw[:], func=AF.Exp, scale=-1.0), R=[cw], W=[Winv])
                    k.op("dve", lambda e, cw=cw: e.tensor_tensor(out=Wex[:], in0=cw[:], in1=e2[:], op=ALU.add), R=[cw, e2], W=[Wex])
                    k.op("act", lambda e: e.activation(out=Wex[:], in_=Wex[:], func=AF.Exp), R=[Wex], W=[Wex])
                    k.op("dve", lambda e, p=p: e.scalar_tensor_tensor(out=o["At"][:, p, :], in0=kk_[:], scalar=-1.0, in1=Wex[:],
                                                                     op0=ALU.mult, op1=ALU.mult), R=[kk_, Wex], W=[o["At"]])
                    k.op("dve", lambda e, p=p, r_=r_: e.tensor_tensor(out=o["rt"][:, p, :], in0=r_, in1=o["W"][:, p, :],
                                                                     op=ALU.mult), R=[xs, o["W"]], W=[o["rt"]])
                    k.op("pool", lambda e: e.tensor_tensor(out=t3[:], in0=kk_[:], in1=a_[:], op=ALU.mult), R=[kk_, a_], W=[t3])
                    k.op("dve", lambda e, p=p: e.tensor_tensor(out=o["Bt"][:, p, :], in0=t3[:], in1=Winv[:], op=ALU.mult),
                         R=[t3, Winv], W=[o["Bt"]])
                    k.op("pool", lambda e, p=p: e.tensor_tensor(out=o["kt"][:, p, :], in0=k_[:], in1=Winv[:], op=ALU.mult),
                         R=[k_, Winv], W=[o["kt"]])
                    k.op("pool", lambda e, p=p, v_=v_: e.tensor_copy(out=o["v"][:, p, :], in_=v_), R=[xs], W=[o["v"]])
                for n_ in names:
                    k.dma("sp", scr[n_][:, :, tb * 512:(tb + 1) * 512], o[n_][:], R=[o[n_]], W=[scr[n_]])
        k.barrier()
        if self.upto == "A2":
            return
        with contextlib.ExitStack() as sc:
            L = CHUNK
            ST = k.sbuf([128, 4, 64], F32, "ST", sc)
            stmp = k.sbuf([128, 4, 64], F32, "STtmp", sc)
            ups = k.sbuf([64, 64], F32, "ups", sc)
            upi = k.sbuf([64, 64], F32, "upi", sc)
            los = k.sbuf([64, 64], F32, "los", sc)
            for nm, t_ in (("c_ups", ups), ("c_tri", upi), ("c_los", los)):
                ci_ = self.inputs.get(nm) or self.inp(nm, [64, 64])
                k.dma("sp", t_[:], ci_[:], R=[ci_], W=[t_])
            gn = k.sbuf([128, 2, 4], F32, "gn", sc)
            k.dma("sp", gn[:, 0, :], W["b_gn_g"].t[0].rearrange("(c p) -> p c", p=128), R=[W["b_gn_g"]], W=[gn],
                  allow_slow_non_contiguous=True)
            k.dma("sp", gn[:, 1, :], W["b_gn_b"].t[0].rearrange("(c p) -> p c", p=128), R=[W["b_gn_b"]], W=[gn],
                  allow_slow_non_contiguous=True)
            blk = {n_: k.sbuf([128, 4, 512], F32, "b_" + n_, sc) for n_ in names}
            ybo = k.sbuf([128, 4, 512], BF16, "ybo", sc)
            def tm(nm):
                return k.sbuf([64, 4, 2, 64], F32, nm, sc)
            Pm = [tm("Pm0"), tm("Pm1")]
            Qm = [tm("Qm0"), tm("Qm1")]
            MAK, MRB, MRK = tm("MAK"), tm("MRB"), tm("MRK")
            Vt, Btk, ktk = tm("Vt"), tm("Btk"), tm("ktk")
            X, Xb, Y, Yc, Ysq = tm("X"), tm("Xb"), tm("Y"), tm("Yc"), tm("Ysq")
            st8 = k.sbuf([64, 4, 8], F32, "st8", sc)
            yT = k.sbuf([128, 4, 64], F32, "yT", sc)

            def fm_mm(dst_bank, hp, lhs_blk, rhs_blk, csl):
                hs = slice(hp * 64, (hp + 1) * 64)
                for p in range(4):
                    k.op("pe", lambda e, p=p: e.matmul(dst_bank[0:64, p * 64:(p + 1) * 64], lhsT=lhs_blk[hs, p, csl],
                                                       rhs=rhs_blk[hs, p, csl], start=True, stop=True),
                         R=[lhs_blk, rhs_blk], W=[dst_bank])

            def masked(dst, lhs_blk, rhs_blk, mask, csl):
                for hp in range(2):
                    ps = self.bank()
                    fm_mm(ps, hp, lhs_blk, rhs_blk, csl)
                    k.op("dve", lambda e, ps=ps, hp=hp: e.tensor_tensor(
                        out=dst[:, :, hp, :], in0=ps[0:64, 0:256].rearrange("t (p s) -> t p s", p=4),
                        in1=mask[:, :].unsqueeze(1).to_broadcast([64, 4, 64]), op=ALU.mult), R=[ps, mask], W=[dst])

            def tok_T(dst, src_blk, csl):
                ps = self.bank()
                for p in range(4):
                    k.op("pe", lambda e, p=p, ps=ps: e.transpose(ps[0:64, p * 128:(p + 1) * 128], src_blk[:, p, csl],
                                                                 self.c_ident_f[:]), R=[src_blk, self.c_ident_f], W=[ps])
                k.op("act", lambda e, ps=ps: e.copy(out=dst[:].rearrange("t p h v -> t (p h v)"), in_=ps[0:64, :]),
                     R=[ps], W=[dst])

            def tm_mm(ps, terms):
                for p in range(4):
                    for hp in range(2):
                        c0 = (p * 2 + hp) * 64
                        for i_, (lt, rt_) in enumerate(terms):
                            k.op("pe", lambda e, p=p, hp=hp, c0=c0, i_=i_, lt=lt, rt_=rt_: e.matmul(
                                ps[0:64, c0:c0 + 64], lhsT=lt[:, p, hp, :], rhs=rt_[:, p, hp, :],
                                start=(i_ == 0), stop=(i_ == len(terms) - 1)), R=[lt, rt_], W=[ps])

            for s_ in range(nseq):
                k.op("dve", lambda e: e.memset(ST[:], 0.0), W=[ST])
                for ch in range(T // L):
                    t0 = s_ * T + ch * L
                    if (ch * L) % 512 == 0:
                        for n_ in names:
                            k.dma("sp", blk[n_][:], scr[n_][:, :, t0:t0 + 512], R=[scr[n_]], W=[blk[n_]])
                    c0_ = (ch * L) % 512
                    csl = slice(c0_, c0_ + L)
                    At, rt, Bt, kt, vv, Wb = (blk[n_] for n_ in ("At", "rt", "Bt", "kt", "v", "W"))
                    P, Q = Pm[0], Qm[0]
                    masked(P, Bt, At, ups, csl)
                    masked(Q, At, Bt, los, csl)
                    masked(MAK, kt, At, ups, csl)
                    masked(MRB, Bt, rt, upi, csl)
                    masked(MRK, kt, rt, upi, csl)
                    tok_T(Vt, vv, csl)
                    tok_T(Btk, Bt, csl)
                    tok_T(ktk, kt, csl)
                    psb_ = self.bank()
                    tm_mm(psb_, [(MAK, Vt)])
                    k.op("act", lambda e, psb_=psb_: e.copy(out=Xb[:].rearrange("t p h v -> t (p h v)"), in_=psb_[0:64, :]),
                         R=[psb_], W=[Xb])
                    for hp in range(2):
                        hs = slice(hp * 64, (hp + 1) * 64)
                        ps = self.bank()
                        for p in range(4):
                            k.op("pe", lambda e, p=p, ps=ps, hs=hs: e.matmul(
                                ps[0:64, p * 64:(p + 1) * 64], lhsT=At[hs, p, csl], rhs=ST[hs, p, :], start=True, stop=True),
                                R=[At, ST], W=[ps])
                        k.op("dve", lambda e, ps=ps, hp=hp: e.tensor_tensor(
                            out=X[:, :, hp, :], in0=ps[0:64, 0:256].rearrange("t (p s) -> t p s", p=4),
                            in1=Xb[:, :, hp, :], op=ALU.add), R=[ps, Xb], W=[X])
                    for j in range(6):
                        ps = self.bank()
                        tm_mm(ps, [(P, X)])
                        if j < 5:
                            psP = self.bank()
                            tm_mm(psP, [(Q, P)])
                            psQ = self.bank()
                            tm_mm(psQ, [(P, Q)])
                        k.op("dve", lambda e, ps=ps: e.tensor_tensor(
                            out=X[:].rearrange("t p h v -> t (p h v)"), in0=X[:].rearrange("t p h v -> t (p h v)"),
                            in1=ps[0:64, :], op=ALU.add), R=[ps, X], W=[X])
                        if j < 5:
                            P2, Q2 = Pm[(j + 1) % 2], Qm[(j + 1) % 2]
                            k.op("act", lambda e, psP=psP, P2=P2: e.copy(out=P2[:].rearrange("t p h v -> t (p h v)"),
                                                                         in_=psP[0:64, :]), R=[psP], W=[P2])
                            k.op("dve", lambda e, psQ=psQ, Q2=Q2: e.tensor_copy(out=Q2[:].rearrange("t p h v -> t (p h v)"),
                                                                                in_=psQ[0:64, :]), R=[psQ], W=[Q2])
                            P, Q = P2, Q2
                    SA = X
                    psb_ = self.bank()
                    tm_mm(psb_, [(MRB, SA), (MRK, Vt)])
                    k.op("act", lambda e, psb_=psb_: e.copy(out=Xb[:].rearrange("t p h v -> t (p h v)"), in_=psb_[0:64, :]),
                         R=[psb_], W=[Xb])
                    for hp in range(2):
                        hs = slice(hp * 64, (hp + 1) * 64)
                        ps = self.bank()
                        for p in range(4):
                            k.op("pe", lambda e, p=p, ps=ps, hs=hs: e.matmul(
                                ps[0:64, p * 64:(p + 1) * 64], lhsT=rt[hs, p, csl], rhs=ST[hs, p, :], start=True, stop=True),
                                R=[rt, ST], W=[ps])
                        k.op("dve", lambda e, ps=ps, hp=hp: e.tensor_tensor(
                            out=Y[:, :, hp, :], in0=ps[0:64, 0:256].rearrange("t (p s) -> t p s", p=4),
                            in1=Xb[:, :, hp, :], op=ALU.add), R=[ps, Xb], W=[Y])
                    psS = self.bank()
                    for hp in range(2):
                        for p in range(4):
                            for i_, (lt, rt_) in enumerate(((Btk, SA), (ktk, Vt))):
                                k.op("pe", lambda e, p=p, hp=hp, i_=i_, lt=lt, rt_=rt_: e.matmul(
                                    psS[hp * 64:(hp + 1) * 64, p * 64:(p + 1) * 64], lhsT=lt[:, p, hp, :], rhs=rt_[:, p, hp, :],
                                    start=(i_ == 0), stop=(i_ == 1)), R=[lt, rt_], W=[psS])
                    k.op("dve", lambda e, psS=psS: e.tensor_tensor(
                        out=stmp[:], in0=ST[:], in1=psS[:, 0:256].rearrange("k (p v) -> k p v", p=4), op=ALU.add),
                        R=[ST, psS], W=[stmp])
                    for p in range(4):
                        k.op("dve", lambda e, p=p: e.tensor_scalar(
                            out=ST[:, p, :], in0=stmp[:, p, :], scalar1=Wb[:, p, c0_ + L - 1:c0_ + L], scalar2=None,
                            op0=ALU.mult), R=[stmp, Wb], W=[ST])
                    Y8 = Y[:].rearrange("t p h v -> t (p h) v")
                    k.op("dve", lambda e: e.reduce_sum(out=st8[:, 0, :], in_=Y8, axis=AX.X), R=[Y], W=[st8])
                    k.op("dve", lambda e: e.tensor_scalar(out=st8[:, 0, :], in0=st8[:, 0, :], scalar1=1.0 / 64, scalar2=None,
                                                          op0=ALU.mult), R=[st8], W=[st8])
                    k.op("dve", lambda e: e.tensor_tensor(
                        out=Yc[:].rearrange("t p h v -> t (p h) v"), in0=Y8,
                        in1=st8[:, 0, :].unsqueeze(2).to_broadcast([64, 8, 64]), op=ALU.subtract), R=[Y, st8], W=[Yc])
                    k.op("pool", lambda e: e.tensor_tensor(out=Ysq[:], in0=Yc[:], in1=Yc[:], op=ALU.mult), R=[Yc], W=[Ysq])
                    k.op("dve", lambda e: e.reduce_sum(out=st8[:, 1, :], in_=Ysq[:].rearrange("t p h v -> t (p h) v"),
                                                       axis=AX.X), R=[Ysq], W=[st8])
                    k.op("dve", lambda e: e.tensor_scalar(out=st8[:, 1, :], in0=st8[:, 1, :], scalar1=1.0 / 64, scalar2=64e-5,
                                                          op0=ALU.mult, op1=ALU.add), R=[st8], W=[st8])
                    k.op("act", lambda e: e.activation(out=st8[:, 2, :], in_=st8[:, 1, :], func=AF.Sqrt), R=[st8], W=[st8])
                    k.op("dve", lambda e: e.reciprocal(out=st8[:, 3, :], in_=st8[:, 2, :]), R=[st8], W=[st8])
                    k.op("dve", lambda e: e.tensor_tensor(
                        out=Yc[:].rearrange("t p h v -> t (p h) v"), in0=Yc[:].rearrange("t p h v -> t (p h) v"),
                        in1=st8[:, 3, :].unsqueeze(2).to_broadcast([64, 8, 64]), op=ALU.mult), R=[Yc, st8], W=[Yc])
                    ps = self.bank()
                    for p in range(4):
                        k.op("pe", lambda e, p=p, ps=ps: e.transpose(
                            ps[:, p * 64:(p + 1) * 64], Yc[:, p, :, :].rearrange("t h v -> t (h v)"),
                            self.c_ident_f[0:64, 0:64]), R=[Yc, self.c_ident_f], W=[ps])
                    for p in range(4):
                        k.op("dve", lambda e, p=p, ps=ps: e.tensor_scalar(
                            out=yT[:, p, :], in0=ps[:, p * 64:(p + 1) * 64], scalar1=gn[:, 0, p:p + 1],
                            scalar2=gn[:, 1, p:p + 1], op0=ALU.mult, op1=ALU.add), R=[ps, gn], W=[yT])
                    k.op("pool", lambda e: e.tensor_tensor(out=yT[:], in0=yT[:], in1=blk["bon"][:, :, csl], op=ALU.add),
                         R=[yT, blk["bon"]], W=[yT])
                    k.op("pool", lambda e: e.tensor_tensor(out=ybo[:, :, csl], in0=yT[:], in1=blk["g"][:, :, csl], op=ALU.mult),
                         R=[yT, blk["g"]], W=[ybo])
                    if c0_ + L == 512 or (ch + 1) * L == T:
                        b0 = t0 + L - (c0_ + L)
                        k.dma("sp", yb_scr[:, :, b0:b0 + c0_ + L], ybo[:, :, 0:c0_ + L], R=[ybo], W=[yb_scr])
        k.barrier()

    def phase_mix1(self, W):
        k = self.k
        ntok = self.ntok
        T = min(SEQ, ntok)
        nseq = ntok // T
        NI = 16
        xm_scr = k.dram([128, NI, nseq, T + 4], BF16, "xm_scr")
        zs_scr = k.dram([128, NI, ntok], BF16, "zs_scr")
        xc_scr = k.dram([128, NI, ntok], BF16, "xc_scr")
        q_scr = k.dram([128, NI, ntok], BF16, "q_scr")
        k_scr = k.dram([128, NI, ntok], BF16, "k_scr")
        Kt_scr = k.dram([ntok, 2048], BF16, "Kt_scr")
        Vt_scr = k.dram([ntok, 2048], BF16, "Vt_scr")
        g_scr = k.dram([ntok, 8], F32, "g_scr")
        y_scr = k.dram([ntok, 2048], F32, "y_scr")
        QS = 512.0 ** -0.5
        with contextlib.ExitStack() as sc:
            stage = [k.sbuf([128, 8, 512], BF16, "cst", sc) for _ in range(2)]
            wup_s = self.conv_weight(W["c_w_up"].t[0], D, 4096, "wup_s", stage)
            gcol = k.sbuf([128, NCH], F32, "gcol", sc)
            k.dma("sp", gcol[:], W["mix_norm"].t[1].rearrange("(c p) -> p c", p=128),
                  R=[W["mix_norm"]], W=[gcol], allow_slow_non_contiguous=True)
            zt = k.sbuf([128, NI, 4], BF16, "zt", sc)
            k.op("dve", lambda e: e.memset(zt[:], 0.0), W=[zt])
            for s_ in range(nseq):
                k.dma("sp", xm_scr[:, :, s_, 0:4], zt[:], R=[zt], W=[xm_scr])
            xb = [k.sbuf([128, NCH, 512], F32, "m1x", sc) for _ in range(2)]
            hT = k.sbuf([128, NCH, 512], BF16, "m1h", sc)
            sq = k.sbuf([128, NCH, 512], BF16, "m1sq", sc)
            rstd = k.sbuf([128, 512], F32, "m1rstd", sc)
            wt = [k.sbuf([128, NCH, 512], BF16, "m1w", sc) for _ in range(2)]
            xmo = k.sbuf([128, NI, 512], BF16, "m1xm", sc)
            zo = k.sbuf([128, NI, 512], BF16, "m1z", sc)
            it = 0
            for tb in range(ntok // 512):
                s_, off = (tb * 512) // T, (tb * 512) % T
                x = xb[tb % 2]
                k.dma("sp", x[:], self.xT[:, :, tb * 512:(tb + 1) * 512], R=[self.xT], W=[x])
                self.rmsnorm([(x, x[:, c, :]) for c in range(NCH)], (gcol, gcol[:, :]),
                             [(hT, hT[:, c, :]) for c in range(NCH)], 512, sq, rstd)
                for og in range(8):
                    w = wt[it % 2]
                    it += 1
                    k.dma("sp", w[:], wup_s[og], R=[wup_s], W=[w])
                    for jj in range(4):
                        ps = self.bank()
                        for c in range(NCH):
                            k.op("pe", lambda e, c=c, jj=jj, ps=ps, w=w: e.matmul(
                                ps[:], lhsT=w[:, c, jj * 128:(jj + 1) * 128], rhs=hT[:, c, :],
                                start=(c == 0), stop=(c == NCH - 1)), R=[w, hT], W=[ps])
                        if og < 4:
                            k.op("act", lambda e, ps=ps, i=og * 4 + jj: e.copy(out=xmo[:, i, :], in_=ps[:]),
                                 R=[ps], W=[xmo])
                        else:
                            k.op("act", lambda e, ps=ps, i=(og - 4) * 4 + jj: e.activation(
                                out=zo[:, i, :], in_=ps[:], func=AF.Silu), R=[ps], W=[zo])
                k.dma("sp", xm_scr[:, :, s_, 4 + off:4 + off + 512], xmo[:], R=[xmo], W=[xm_scr])
                k.dma("sp", zs_scr[:, :, tb * 512:(tb + 1) * 512], zo[:], R=[zo], W=[zs_scr])
        k.barrier()
        if self.upto == "M1":
            return
        with contextlib.ExitStack() as sc:
            bd = k.sbuf([128, 48, 128], BF16, "bd", sc)
            wqt = k.sbuf([128, 48, 4], F32, "wqt", sc)
            bdm = k.sbuf([128, 32], F32, "bdm", sc)
            cbm = self.inp("c_bdmask", [128, 32])
            k.dma("sp", bdm[:], cbm[:], R=[cbm], W=[bdm])
            for wi, wn in enumerate(["c_wq", "c_wk", "c_wv"]):
                for c_ in range(16):
                    k.dma("sp", wqt[:, wi * 16 + c_, :],
                          W[wn].t[0, c_ * 32:(c_ + 1) * 32].rearrange("g i j -> (g i) j"), R=[W[wn]], W=[wqt])
            k.op("dve", lambda e: e.memset(bd[:], 0.0), W=[bd])
            for ci in range(48):
                for j in range(4):
                    k.op("dve", lambda e, ci=ci, j=j: e.tensor_scalar(
                        out=bd[:, ci, :].rearrange("p (g j) -> p g j", j=4)[:, :, j], in0=bdm[:],
                        scalar1=wqt[:, ci, j:j + 1], scalar2=None, op0=ALU.mult), R=[bdm, wqt], W=[bd])
            wif = k.sbuf([128, 48, 8], BF16, "wif", sc)
            for i3_ in range(3):
                k.dma("pool", wif[:, i3_ * 16:(i3_ + 1) * 16, :],
                      W["c_w_if"].t[0, i3_ * 2048:(i3_ + 1) * 2048].rearrange("(c p) n -> p c n", p=128),
                      R=[W["c_w_if"]], W=[wif])
            brow = k.sbuf([1, 8], F32, "brow", sc)
            k.dma("sp", brow[0:1, 0:4], W["c_b_i"].t[0:1, :], R=[W["c_b_i"]], W=[brow])
            k.dma("sp", brow[0:1, 4:8], W["c_b_f"].t[0:1, :], R=[W["c_b_f"]], W=[brow])
            onesf = k.sbuf([1, 128], F32, "onesf", sc)
            k.op("dve", lambda e: e.memset(onesf[:], 1.0), W=[onesf])
            cw = k.sbuf([128, NI, 4], F32, "cw", sc)
            for j_ in range(4):
                k.dma("sp", cw[:, :, j_], W["c_conv_w"].t[0, j_, 0].rearrange("(c p) -> p c", p=128),
                      R=[W["c_conv_w"]], W=[cw], allow_slow_non_contiguous=True)
            cb = k.sbuf([128, NI], F32, "cb", sc)
            k.dma("sp", cb[:], W["c_conv_b"].t[0].rearrange("(c p) -> p c", p=128),
                  R=[W["c_conv_b"]], W=[cb], allow_slow_non_contiguous=True)
            xmh = k.sbuf([128, NI, 516], BF16, "xmh", sc)
            acc = [k.sbuf([128, 512], F32, "acc", sc) for _ in range(2)]
            xc = k.sbuf([128, NI, 512], BF16, "xc", sc)
            qT = k.sbuf([128, NI, 512], BF16, "qT", sc)
            qs = k.sbuf([128, NI, 512], BF16, "qs", sc)
            kT = k.sbuf([128, NI, 512], BF16, "kT", sc)
            vT = k.sbuf([128, NI, 512], BF16, "vT", sc)
            Kt = k.sbuf([128, 2048], BF16, "Kt", sc)
            Vt = k.sbuf([128, 2048], BF16, "Vt", sc)
            gt = k.sbuf([128, 8], F32, "gt", sc)
            for tb in range(ntok // 512 if 'm2loop' not in self.skip else 0):
                s_, off = (tb * 512) // T, (tb * 512) % T
                k.dma("sp", xmh[:], xm_scr[:, :, s_, off:off + 516], R=[xm_scr], W=[xmh])
                for c in range(NI):
                    a = acc[c % 2]
                    k.op("dve", lambda e, c=c, a=a: e.tensor_scalar(
                        out=a[:], in0=xmh[:, c, 1:513], scalar1=cw[:, c, 0:1], scalar2=None, op0=ALU.mult),
                        R=[xmh, cw], W=[a])
                    for j in range(1, 4):
                        k.op("dve", lambda e, c=c, a=a, j=j: e.scalar_tensor_tensor(
                            out=a[:], in0=xmh[:, c, 1 + j:1 + j + 512], scalar=cw[:, c, j:j + 1], in1=a[:],
                            op0=ALU.mult, op1=ALU.add), R=[xmh, cw, a], W=[a])
                    k.op("act", lambda e, c=c, a=a: e.activation(
                        out=xc[:, c, :], in_=a[:], func=AF.Silu, bias=cb[:, c:c + 1], scale=1.0),
                        R=[a, cb], W=[xc])
                k.dma("sp", xc_scr[:, :, tb * 512:(tb + 1) * 512], xc[:], R=[xc], W=[xc_scr])
                if 'm2qkv' in self.skip:
                    continue
                for c in range(NI):
                    ps = self.bank()
                    k.op("pe", lambda e, c=c, ps=ps: e.matmul(ps[:], lhsT=bd[:, c, :], rhs=xc[:, c, :],
                                                             start=True, stop=True), R=[bd, xc], W=[ps])
                    k.op("act", lambda e, c=c, ps=ps: e.copy(out=qT[:, c, :], in_=ps[:]), R=[ps], W=[qT])
                    if 'm2qs' not in self.skip:
                        k.op("pool", lambda e, c=c: e.tensor_scalar(
                            out=qs[:, c, :], in0=qT[:, c, :], scalar1=QS, scalar2=None, op0=ALU.mult), R=[qT], W=[qs])
                    ps = self.bank()
                    k.op("pe", lambda e, c=c, ps=ps: e.matmul(ps[:], lhsT=bd[:, 16 + c, :], rhs=xc[:, c, :],
                                                             start=True, stop=True), R=[bd, xc], W=[ps])
                    k.op("act", lambda e, c=c, ps=ps: e.copy(out=kT[:, c, :], in_=ps[:]), R=[ps], W=[kT])
                    ps = self.bank()
                    k.op("pe", lambda e, c=c, ps=ps: e.matmul(ps[:], lhsT=bd[:, 32 + c, :],
                                                             rhs=(xc[:, c, :] if 'm2v' in self.skip else xmh[:, c, 4:516]),
                                                             start=True, stop=True), R=[bd, xmh], W=[ps])
                    k.op("dve", lambda e, c=c, ps=ps: e.tensor_copy(out=vT[:, c, :], in_=ps[:]), R=[ps], W=[vT])
                k.dma("sp", q_scr[:, :, tb * 512:(tb + 1) * 512], qs[:], R=[qs], W=[q_scr])
                k.dma("sp", k_scr[:, :, tb * 512:(tb + 1) * 512], kT[:], R=[kT], W=[k_scr])
                if 'm2tok' in self.skip:
                    continue
                for tt in range(4):
                    tsl = slice(tt * 128, (tt + 1) * 128)
                    for which, dst in ((0, Kt), (1, Vt)):
                        for b4 in range(4):
                            ps = self.bank()
                            for cc in range(4):
                                c = b4 * 4 + cc
                                if which == 0:
                                    k.op("pe", lambda e, c=c, cc=cc, ps=ps: e.matmul(
                                        ps[:, cc * 128:(cc + 1) * 128], lhsT=xc[:, c, tsl], rhs=bd[:, 16 + c, :],
                                        start=True, stop=True), R=[xc, bd], W=[ps])
                                else:
                                    k.op("pe", lambda e, c=c, cc=cc, ps=ps: e.matmul(
                                        ps[:, cc * 128:(cc + 1) * 128],
                                        lhsT=xmh[:, c, 4 + tt * 128:4 + (tt + 1) * 128], rhs=bd[:, 32 + c, :],
                                        start=True, stop=True), R=[xmh, bd], W=[ps])
                            en = "act" if b4 % 2 else "dve"
                            if en == "act":
                                k.op("act", lambda e, ps=ps, b4=b4, dst=dst: e.copy(
                                    out=dst[:, b4 * 512:(b4 + 1) * 512], in_=ps[:]), R=[ps], W=[dst])
                            else:
                                k.op("dve", lambda e, ps=ps, b4=b4, dst=dst: e.tensor_copy(
                                    out=dst[:, b4 * 512:(b4 + 1) * 512], in_=ps[:]), R=[ps], W=[dst])
                    ps = self.bank()
                    for i3, src in enumerate((qT, kT, vT)):
                        for c in range(NI):
                            k.op("pe", lambda e, c=c, i3=i3, src=src, ps=ps: e.matmul(
                                ps[:, 0:8], lhsT=src[:, c, tsl], rhs=wif[:, i3 * 16 + c, :],
                                start=(i3 == 0 and c == 0), stop=False), R=[src, wif], W=[ps])
                    if 'bias' not in self.skip:
                        k.op("pe", lambda e, ps=ps: e.matmul(ps[:, 0:8], lhsT=onesf[0:1, :], rhs=brow[0:1, :],
                                                            start=False, stop=True), R=[onesf, brow], W=[ps])
                    k.op("act", lambda e, ps=ps: e.copy(out=gt[:], in_=ps[:, 0:8]), R=[ps], W=[gt])
                    r0 = tb * 512 + tt * 128
                    k.dma("sp", Kt_scr[r0:r0 + 128, :], Kt[:], R=[Kt], W=[Kt_scr])
                    k.dma("sp", Vt_scr[r0:r0 + 128, :], Vt[:], R=[Vt], W=[Vt_scr])
                    k.dma("sp", g_scr[r0:r0 + 128, :], gt[:], R=[gt], W=[g_scr])
        k.barrier()
        if self.upto == "M2":
            return
        with contextlib.ExitStack() as sc:
            L = CHUNK
            C = k.sbuf([128, NI, 512], F32, "C", sc)
            Cb = k.sbuf([128, NI, 512], BF16, "Cb", sc)
            nv = k.sbuf([128, NI], F32, "nv", sc)
            nvb = k.sbuf([128, NI], BF16, "nvb", sc)
            mbc = k.sbuf([128, 4], F32, "mbc", sc)
            tri = k.sbuf([64, 64], F32, "tri", sc)
            sel = k.sbuf([64, 128], F32, "sel", sc)
            one64 = k.sbuf([64, 64], F32, "one64", sc)
            mneg = k.sbuf([64, 64], F32, "mneg", sc)
            oneb = k.sbuf([64, 1], BF16, "oneb", sc)
            cone = k.sbuf([128, 1], F32, "cone", sc)
            for nm, t_, shp in (("c_tri", tri, [64, 64]), ("c_sel", sel, [64, 128]), ("c_mneg", mneg, [64, 64])):
                ci_ = self.inp(nm, shp)
                k.dma("sp", t_[:], ci_[:], R=[ci_], W=[t_])
            k.op("dve", lambda e: e.memset(one64[:], 1.0), W=[one64])
            k.op("dve", lambda e: e.memset(oneb[:], 1.0), W=[oneb])
            k.op("dve", lambda e: e.memset(cone[:], 1.0), W=[cone])
            qsb = k.sbuf([128, NI, 512], BF16, "qsb", sc)
            kTb = k.sbuf([128, NI, 512], BF16, "kTb", sc)
            Ktc = [k.sbuf([64, 2048], BF16, "Ktc", sc) for _ in range(2)]
            Vtc = [k.sbuf([64, 2048], BF16, "Vtc", sc) for _ in range(2)]
            gtc = [k.sbuf([64, 8], F32, "gtc", sc) for _ in range(2)]
            sm = {n_: k.sbuf([128, 4], F32, n_, sc) for n_ in
                  ("l", "b", "bL", "bm", "cv", "mt", "negm", "scv", "emt", "mnew", "ws", "dc", "tmp")}
            dmat = [k.sbuf([64, 64], F32, f"dmat{h}", sc) for h in range(4)]
            diagc = k.sbuf([64, 64], F32, "diagc", sc)
            wts = k.sbuf([64, 64], F32, "wts", sc)
            qkw = k.sbuf([64, 64], F32, "qkw", sc)
            qkwT = k.sbuf([64, 64], BF16, "qkwT", sc)
            Asb = k.sbuf([64, 512], F32, "Asb", sc)
            hh = k.sbuf([64, 512], F32, "hh", sc)
            junk = k.sbuf([64, 512], F32, "junk", sc)
            Kw = k.sbuf([64, 512], BF16, "Kw", sc)
            st4 = k.sbuf([64, 8], F32, "st4", sc)
            yt = [k.sbuf([64, 2048], F32, "yt", sc) for _ in range(2)]
            for s_ in range(nseq):
                k.op("dve", lambda e: e.memset(C[:], 0.0), W=[C])
                k.op("dve", lambda e: e.memset(Cb[:], 0.0), W=[Cb])
                k.op("dve", lambda e: e.memset(nv[:], 0.0), W=[nv])
                k.op("dve", lambda e: e.memset(nvb[:], 0.0), W=[nvb])
                k.op("dve", lambda e: e.memset(mbc[:], 0.0), W=[mbc])
                for ch in range(T // L):
                    t0 = s_ * T + ch * L
                    if (ch * L) % 512 == 0:
                        k.dma("sp", qsb[:], q_scr[:, :, t0:t0 + 512], R=[q_scr], W=[qsb])
                        k.dma("sp", kTb[:], k_scr[:, :, t0:t0 + 512], R=[k_scr], W=[kTb])
                    csl = slice((ch * L) % 512, (ch * L) % 512 + L)
                    Kc, Vc, gc, yo = Ktc[ch % 2], Vtc[ch % 2], gtc[ch % 2], yt[ch % 2]
                    k.dma("sp", Kc[:], Kt_scr[t0:t0 + L, :], R=[Kt_scr], W=[Kc])
                    k.dma("sp", Vc[:], Vt_scr[t0:t0 + L, :], R=[Vt_scr], W=[Vc])
                    k.dma("sp", gc[:], g_scr[t0:t0 + L, :], R=[g_scr], W=[gc])
                    S = sm
                    k.op("act", lambda e: e.activation(out=S["tmp"][0:64, :], in_=gc[:, 4:8], func=AF.Exp, scale=-1.0),
                         R=[gc], W=[S["tmp"]])
                    k.op("act", lambda e: e.activation(out=S["l"][0:64, :], in_=S["tmp"][0:64, :], func=AF.Ln,
                                                       bias=cone[0:64, 0:1], scale=1.0), R=[S["tmp"], cone], W=[S["l"]])
                    ps = self.bank()
                    k.op("pe", lambda e, ps=ps: e.matmul(ps[0:64, 0:4], lhsT=tri[:], rhs=S["l"][0:64, :],
                                                        start=True, stop=True), R=[tri, S["l"]], W=[ps])
                    k.op("dve", lambda e, ps=ps: e.tensor_scalar(out=S["b"][0:64, :], in0=ps[0:64, 0:4], scalar1=-1.0,
                                                                scalar2=None, op0=ALU.mult), R=[ps], W=[S["b"]])
                    ps = self.bank()
                    k.op("pe", lambda e, ps=ps: e.matmul(ps[:, 0:4], lhsT=sel[:], rhs=S["b"][0:64, :],
                                                        start=True, stop=True), R=[sel, S["b"]], W=[ps])
                    k.op("dve", lambda e, ps=ps: e.tensor_copy(out=S["bL"][:], in_=ps[:, 0:4]), R=[ps], W=[S["bL"]])
                    k.op("dve", lambda e: e.tensor_tensor(out=S["bm"][0:64, :], in0=S["b"][0:64, :], in1=mbc[0:64, :],
                                                          op=ALU.add), R=[S["b"], mbc], W=[S["bm"]])
                    k.op("dve", lambda e: e.tensor_tensor(out=S["cv"][0:64, :], in0=gc[:, 0:4], in1=S["b"][0:64, :],
                                                          op=ALU.subtract), R=[gc, S["b"]], W=[S["cv"]])
                    for h in range(4):
                        k.op("dve", lambda e, h=h: e.tensor_scalar(
                            out=diagc[:], in0=self.c_ident_f[0:64, 0:64], scalar1=S["cv"][0:64, h:h + 1], scalar2=None,
                            op0=ALU.mult), R=[self.c_ident_f, S["cv"]], W=[diagc])
                        ps = self.bank()
                        k.op("pe", lambda e, ps=ps: e.matmul(ps[0:64, 0:64], lhsT=one64[:], rhs=diagc[:],
                                                            start=True, stop=True), R=[one64, diagc], W=[ps])
                        k.op("dve", lambda e, ps=ps, h=h: e.scalar_tensor_tensor(
                            out=dmat[h][:], in0=ps[0:64, 0:64], scalar=S["b"][0:64, h:h + 1], in1=mneg[:],
                            op0=ALU.add, op1=ALU.add), R=[ps, S["b"], mneg], W=[dmat[h]])
                        k.op("dve", lambda e, h=h: e.reduce_max(out=S["tmp"][0:64, h:h + 1], in_=dmat[h][:], axis=AX.X),
                             R=[dmat[h]], W=[S["tmp"]])
                    k.op("dve", lambda e: e.tensor_tensor(out=S["mt"][0:64, :], in0=S["tmp"][0:64, :], in1=S["bm"][0:64, :],
                                                          op=ALU.max), R=[S["tmp"], S["bm"]], W=[S["mt"]])
                    k.op("dve", lambda e: e.tensor_scalar(out=S["negm"][0:64, :], in0=S["mt"][0:64, :], scalar1=-1.0,
                                                          scalar2=None, op0=ALU.mult), R=[S["mt"]], W=[S["negm"]])
                    k.op("dve", lambda e: e.tensor_tensor(out=S["scv"][0:64, :], in0=S["bm"][0:64, :], in1=S["mt"][0:64, :],
                                                          op=ALU.subtract), R=[S["bm"], S["mt"]], W=[S["scv"]])
                    k.op("act", lambda e: e.activation(out=S["scv"][0:64, :], in_=S["scv"][0:64, :], func=AF.Exp),
                         R=[S["scv"]], W=[S["scv"]])
                    k.op("act", lambda e: e.activation(out=S["emt"][0:64, :], in_=S["negm"][0:64, :], func=AF.Exp),
                         R=[S["negm"]], W=[S["emt"]])
                    ps = self.bank()
                    k.op("pe", lambda e, ps=ps: e.matmul(ps[:, 0:4], lhsT=sel[:], rhs=S["mt"][0:64, :],
                                                        start=True, stop=True), R=[sel, S["mt"]], W=[ps])
                    k.op("dve", lambda e, ps=ps: e.tensor_copy(out=S["mnew"][:], in_=ps[:, 0:4]), R=[ps], W=[S["mnew"]])
                    k.op("dve", lambda e: e.tensor_tensor(out=S["ws"][0:64, :], in0=S["bL"][0:64, :], in1=S["cv"][0:64, :],
                                                          op=ALU.add), R=[S["bL"], S["cv"]], W=[S["ws"]])
                    k.op("dve", lambda e: e.tensor_tensor(out=S["ws"][0:64, :], in0=S["ws"][0:64, :], in1=S["mnew"][0:64, :],
                                                          op=ALU.subtract), R=[S["ws"], S["mnew"]], W=[S["ws"]])
                    k.op("act", lambda e: e.activation(out=S["ws"][0:64, :], in_=S["ws"][0:64, :], func=AF.Exp),
                         R=[S["ws"]], W=[S["ws"]])
                    k.op("dve", lambda e: e.tensor_tensor(out=S["dc"][:], in0=S["bL"][:], in1=mbc[:], op=ALU.add),
                         R=[S["bL"], mbc], W=[S["dc"]])
                    k.op("dve", lambda e: e.tensor_tensor(out=S["dc"][:], in0=S["dc"][:], in1=S["mnew"][:],
                                                          op=ALU.subtract), R=[S["dc"], S["mnew"]], W=[S["dc"]])
                    k.op("act", lambda e: e.activation(out=S["dc"][:], in_=S["dc"][:], func=AF.Exp),
                         R=[S["dc"]], W=[S["dc"]])
                    k.op("dve", lambda e: e.tensor_copy(out=mbc[:], in_=S["mnew"][:]), R=[S["mnew"]], W=[mbc])
                    for h in range(4):
                        hs = slice(h * 512, (h + 1) * 512)
                        k.op("act", lambda e, h=h: e.activation(out=wts[:], in_=dmat[h][:], func=AF.Exp,
                                                                bias=S["negm"][0:64, h:h + 1], scale=1.0),
                             R=[dmat[h], S["negm"]], W=[wts])
                        ps = self.bank()
                        for dcc in range(4):
                            k.op("pe", lambda e, ps=ps, dcc=dcc, h=h: e.matmul(
                                ps[0:64, 0:64], lhsT=qsb[:, h * 4 + dcc, csl], rhs=kTb[:, h * 4 + dcc, csl],
                                start=(dcc == 0), stop=(dcc == 3)), R=[qsb, kTb], W=[ps])
                        k.op("dve", lambda e, ps=ps: e.tensor_tensor(out=qkw[:], in0=wts[:], in1=ps[0:64, 0:64],
                                                                     op=ALU.mult), R=[wts, ps], W=[qkw])
                        k.op("dve", lambda e: e.reduce_sum(out=st4[:, 0:1], in_=qkw[:], axis=AX.X), R=[qkw], W=[st4])
                        ps = self.bank()
                        k.op("pe", lambda e, ps=ps: e.transpose(ps[0:64, 0:64], qkw[:], self.c_ident_f[0:64, 0:64]),
                             R=[qkw, self.c_ident_f], W=[ps])
                        k.op("act", lambda e, ps=ps: e.copy(out=qkwT[:], in_=ps[0:64, 0:64]), R=[ps], W=[qkwT])
                        psA = self.bank()
                        k.op("pe", lambda e, psA=psA, hs=hs: e.matmul(psA[0:64, :], lhsT=qkwT[:], rhs=Vc[:, hs],
                                                                      start=True, stop=True), R=[qkwT, Vc], W=[psA])
                        psB = self.bank()
                        for dcc in range(4):
                            k.op("pe", lambda e, psB=psB, dcc=dcc, h=h: e.matmul(
                                psB[0:64, :], lhsT=qsb[:, h * 4 + dcc, csl], rhs=Cb[:, h * 4 + dcc, :],
                                start=(dcc == 0), stop=(dcc == 3)), R=[qsb, Cb], W=[psB])
                        psn = self.bank()
                        for dcc in range(4):
                            k.op("pe", lambda e, psn=psn, dcc=dcc, h=h: e.matmul(
                                psn[0:64, 0:1], lhsT=qsb[:, h * 4 + dcc, csl], rhs=nvb[:, h * 4 + dcc:h * 4 + dcc + 1],
                                start=(dcc == 0), stop=(dcc == 3)), R=[qsb, nvb], W=[psn])
                        k.op("act", lambda e, psA=psA: e.copy(out=Asb[:], in_=psA[0:64, :]), R=[psA], W=[Asb])
                        k.op("dve", lambda e, psB=psB, h=h: e.scalar_tensor_tensor(
                            out=hh[:], in0=psB[0:64, :], scalar=S["scv"][0:64, h:h + 1], in1=Asb[:],
                            op0=ALU.mult, op1=ALU.add), R=[psB, S["scv"], Asb], W=[hh])
                        k.op("dve", lambda e, psn=psn, h=h: e.scalar_tensor_tensor(
                            out=st4[:, 1:2], in0=psn[0:64, 0:1], scalar=S["scv"][0:64, h:h + 1], in1=st4[:, 0:1],
                            op0=ALU.mult, op1=ALU.add), R=[psn, S["scv"], st4], W=[st4])
                        k.op("dve", lambda e, h=h: e.tensor_scalar(
                            out=st4[:, 2:3], in0=st4[:, 1:2], scalar1=-1.0, scalar2=S["emt"][0:64, h:h + 1],
                            op0=ALU.mult, op1=ALU.max), R=[st4, S["emt"]], W=[st4])
                        k.op("dve", lambda e, h=h: e.tensor_tensor(
                            out=st4[:, 2:3], in0=st4[:, 2:3], in1=st4[:, 1:2], op=ALU.max), R=[st4], W=[st4])
                        k.op("dve", lambda e: e.reciprocal(out=st4[:, 3:4], in_=st4[:, 2:3]), R=[st4], W=[st4])
                        k.op("dve", lambda e: e.tensor_scalar(out=hh[:], in0=hh[:], scalar1=st4[:, 3:4], scalar2=None,
                                                              op0=ALU.mult), R=[hh, st4], W=[hh])
                        k.op("dve", lambda e: e.reduce_sum(out=st4[:, 4:5], in_=hh[:], axis=AX.X), R=[hh], W=[st4])
                        k.op("dve", lambda e: e.tensor_scalar(out=st4[:, 4:5], in0=st4[:, 4:5], scalar1=-1.0 / 512,
                                                              scalar2=None, op0=ALU.mult), R=[st4], W=[st4])
                        k.op("dve", lambda e: e.tensor_scalar(out=hh[:], in0=hh[:], scalar1=st4[:, 4:5], scalar2=None,
                                                              op0=ALU.add), R=[hh, st4], W=[hh])
                        k.op("dve", lambda e: e.tensor_tensor(out=junk[:], in0=hh[:], in1=hh[:], op=ALU.mult),
                             R=[hh], W=[junk])
                        k.op("dve", lambda e: e.reduce_sum(out=st4[:, 5:6], in_=junk[:], axis=AX.X), R=[junk], W=[st4])
                        k.op("dve", lambda e: e.tensor_scalar(out=st4[:, 5:6], in0=st4[:, 5:6], scalar1=1.0 / 512,
                                                              scalar2=1e-5, op0=ALU.mult, op1=ALU.add), R=[st4], W=[st4])
                        k.op("act", lambda e: e.activation(out=st4[:, 6:7], in_=st4[:, 5:6], func=AF.Sqrt), R=[st4], W=[st4])
                        k.op("dve", lambda e: e.reciprocal(out=st4[:, 7:8], in_=st4[:, 6:7]), R=[st4], W=[st4])
                        k.op("dve", lambda e, hs=hs, yo=yo: e.tensor_scalar(
                            out=yo[:, hs], in0=hh[:], scalar1=st4[:, 7:8], scalar2=None, op0=ALU.mult),
                            R=[hh, st4], W=[yo])
                        k.op("dve", lambda e, h=h, hs=hs: e.tensor_scalar(
                            out=Kw[:], in0=Kc[:, hs], scalar1=S["ws"][0:64, h:h + 1], scalar2=None, op0=ALU.mult),
                            R=[Kc, S["ws"]], W=[Kw])
                        for dcc in range(4):
                            psU = self.bank()
                            k.op("pe", lambda e, psU=psU, dcc=dcc, hs=hs: e.matmul(
                                psU[:], lhsT=Kw[:, dcc * 128:(dcc + 1) * 128], rhs=Vc[:, hs], start=True, stop=True),
                                R=[Kw, Vc], W=[psU])
                            i_ = h * 4 + dcc
                            k.op("dve", lambda e, psU=psU, i_=i_, h=h: e.scalar_tensor_tensor(
                                out=C[:, i_, :], in0=C[:, i_, :], scalar=S["dc"][:, h:h + 1], in1=psU[:],
                                op0=ALU.mult, op1=ALU.add), R=[C, S["dc"], psU], W=[C])
                            k.op("act", lambda e, i_=i_: e.copy(out=Cb[:, i_, :], in_=C[:, i_, :]), R=[C], W=[Cb])
                        psn2 = self.bank()
                        for dcc in range(4):
                            k.op("pe", lambda e, psn2=psn2, dcc=dcc: e.matmul(
                                psn2[:, dcc:dcc + 1], lhsT=Kw[:, dcc * 128:(dcc + 1) * 128], rhs=oneb[:],
                                start=True, stop=True), R=[Kw, oneb], W=[psn2])
                        k.op("dve", lambda e, psn2=psn2, h=h: e.scalar_tensor_tensor(
                            out=nv[:, h * 4:h * 4 + 4], in0=nv[:, h * 4:h * 4 + 4], scalar=S["dc"][:, h:h + 1],
                            in1=psn2[:, 0:4], op0=ALU.mult, op1=ALU.add), R=[nv, S["dc"], psn2], W=[nv])
                        k.op("act", lambda e, h=h: e.copy(out=nvb[:, h * 4:h * 4 + 4], in_=nv[:, h * 4:h * 4 + 4]),
                             R=[nv], W=[nvb])
                    k.dma("sp", y_scr[t0:t0 + L, :], yo[:], R=[yo], W=[y_scr])
        k.barrier()
        if self.upto == "M3":
            return
        with contextlib.ExitStack() as sc:
            wdn = k.sbuf([128, NI, D], BF16, "wdn", sc)
            k.dma("pool", wdn[:], W["c_w_down"].t[0].rearrange("(c p) n -> p c n", p=128), R=[W["c_w_down"]], W=[wdn])
            mhg = k.sbuf([128, NI], F32, "mhg", sc)
            skp = k.sbuf([128, NI], F32, "skp", sc)
            k.dma("sp", mhg[:], W["c_mh_g"].t[0].rearrange("(c p) -> p c", p=128), R=[W["c_mh_g"]], W=[mhg],
                  allow_slow_non_contiguous=True)
            k.dma("sp", skp[:], W["c_skip"].t[0].rearrange("(c p) -> p c", p=128), R=[W["c_skip"]], W=[skp],
                  allow_slow_non_contiguous=True)
            ytk = k.sbuf([128, 4, 2048], F32, "ytk", sc)
            xcb = k.sbuf([128, NI, 512], BF16, "xcb", sc)
            zsb = k.sbuf([128, NI, 512], BF16, "zsb", sc)
            yz = k.sbuf([128, NI, 512], BF16, "yz", sc)
            t1 = [k.sbuf([128, 512], F32, "t1", sc) for _ in range(2)]
            x = k.sbuf([128, NCH, 512], F32, "m4x", sc)
            for tb in range(ntok // 512):
                k.dma("sp", ytk[:], y_scr[tb * 512:(tb + 1) * 512, :].rearrange("(j p) n -> p j n", p=128),
                      R=[y_scr], W=[ytk])
                k.dma("sp", xcb[:], xc_scr[:, :, tb * 512:(tb + 1) * 512], R=[xc_scr], W=[xcb])
                k.dma("sp", zsb[:], zs_scr[:, :, tb * 512:(tb + 1) * 512], R=[zs_scr], W=[zsb])
                k.dma("sp", x[:], self.xT[:, :, tb * 512:(tb + 1) * 512], R=[self.xT], W=[x])
                for c in range(NI):
                    ps = self.bank()
                    for j in range(4):
                        k.op("pe", lambda e, c=c, j=j, ps=ps: e.transpose(
                            ps[:, j * 128:(j + 1) * 128], ytk[:, j, c * 128:(c + 1) * 128], self.c_ident_f[:]),
                            R=[ytk, self.c_ident_f], W=[ps])
                    t = t1[c % 2]
                    k.op("dve", lambda e, c=c, t=t: e.tensor_scalar(
                        out=t[:], in0=xcb[:, c, :], scalar1=skp[:, c:c + 1], scalar2=None, op0=ALU.mult),
                        R=[xcb, skp], W=[t])
                    k.op("dve", lambda e, c=c, t=t, ps=ps: e.scalar_tensor_tensor(
                        out=t[:], in0=ps[:], scalar=mhg[:, c:c + 1], in1=t[:], op0=ALU.mult, op1=ALU.add),
                        R=[ps, mhg, t], W=[t])
                    k.op("dve", lambda e, c=c, t=t: e.tensor_tensor(out=yz[:, c, :], in0=t[:], in1=zsb[:, c, :],
                                                                   op=ALU.mult), R=[t, zsb], W=[yz])
                for n in range(NCH):
                    po = self.bank()
                    for c in range(NI):
                        k.op("pe", lambda e, n=n, c=c, po=po: e.matmul(
                            po[:], lhsT=wdn[:, c, n * 128:(n + 1) * 128], rhs=yz[:, c, :],
                            start=(c == 0), stop=(c == NI - 1)), R=[wdn, yz], W=[po])
                    k.op("dve", lambda e, n=n, po=po: e.tensor_tensor(
                        out=x[:, n, :], in0=x[:, n, :], in1=po[:], op=ALU.add), R=[po, x], W=[x])
                k.dma("sp", self.xT[:, :, tb * 512:(tb + 1) * 512], x[:], R=[x], W=[self.xT])
        k.barrier()

    def build(self):
        ntok = self.ntok
        W = {}
        x_in = self.inp("x", [ntok, D])
        W["p"] = self.inp("p", [2, ntok, PLE_DIM])
        for name, shape in WEIGHT_SHAPES.items():
            W[name] = self.inp(name, shape)
        yt = self.nc.dram_tensor("y", [ntok, D], F32, kind="ExternalOutput")
        y_out = Buf(yt.ap(), "y")
        self.setup_consts()
        self.phase_in(x_in)
        for li in self.layers:
            if "mix" in self.stages and li == 1:
                self.phase_mix1(W)
            if "mix" in self.stages and li == 0:
                self.phase_mix0(W)
            if "ffn" in self.stages:
                self.phase_ffn(li, W)
        self.phase_out(y_out)
        self.k.wait_all("sp", [y_out] + list(self.dumps.values()))
        self.k.close()
        return self.nc


WEIGHT_SHAPES = {
    "mix_norm": [2, 1024], "a_q_gain": [1, 64], "a_k_gain": [1, 64], "w_in_e": [1, 1024, 2756],
    "b_mu": [1, 1792], "b_w0": [1, 512], "b_w2": [1, 64, 512], "b_a0": [1, 512], "b_a2": [1, 64, 512],
    "b_g2": [1, 128, 512], "b_k_k": [1, 512], "b_k_a": [1, 512], "b_r_k": [1, 8, 64], "b_gn_g": [1, 512],
    "b_gn_b": [1, 512], "w_out_e": [1, 1024, 1024], "c_w_up": [1, 1024, 4096], "c_conv_w": [1, 4, 1, 2048],
    "c_conv_b": [1, 2048], "c_wq": [1, 512, 4, 4], "c_wk": [1, 512, 4, 4], "c_wv": [1, 512, 4, 4],
    "c_w_if": [1, 6144, 8], "c_b_i": [1, 4], "c_b_f": [1, 4], "c_mh_g": [1, 2048], "c_skip": [1, 2048],
    "c_w_down": [1, 2048, 1024], "ffn_norm": [2, 1024], "ffn_w_gate": [2, 1024, 2816],
    "ffn_w_up": [2, 1024, 2816], "ffn_w_down": [2, 2816, 1024], "ple_w": [2, 256, 1024],
    "ple_norm": [2, 1024], "ple_w_gate": [2, 1024, 1024],
}


def host_consts():
    i64 = np.arange(64)
    sel = np.zeros((64, 128), np.float32)
    sel[63, :] = 1.0
    return {"c_ident": np.eye(128, dtype=np.float32),
            "c_bdmask": (np.arange(128)[:, None] // 4 == np.arange(32)[None, :]).astype(np.float32),
            "c_tri": (i64[:, None] <= i64[None, :]).astype(np.float32),
            "c_ups": (i64[:, None] < i64[None, :]).astype(np.float32),
            "c_los": (i64[:, None] > i64[None, :]).astype(np.float32),
            "c_bones": (np.arange(128)[:, None] // 64 == np.arange(128)[None, :] // 64).astype(np.float32),
            "c_bd64": ((np.arange(128)[:, None] // 64 == np.arange(128)[None, :] // 64) / 64.0).astype(np.float32),
            "c_mneg2": np.where((np.arange(128)[:, None] < 64) & (np.arange(128)[None, :] >= 64), -1e30, 0.0).astype(np.float32),
            "c_sel": sel,
            "c_mneg": np.where(i64[None, :] <= i64[:, None], 0.0, -1e30).astype(np.float32)}


def run(inputs, ntok_core, ncore=NCORE, layers=(0, 1), stages=("mix", "ffn"), trace=False, dbg=False):
    prog = Prog(ntok_core, layers=layers, stages=stages, dbg=dbg)
    nc = prog.build()
    x = np.ascontiguousarray(inputs["x"]).reshape(-1, D)
    p = np.ascontiguousarray(inputs["p"]).reshape(2, -1, PLE_DIM)
    consts = host_consts()
    in_maps = []
    for c in range(ncore):
        m = {"x": x[c * ntok_core:(c + 1) * ntok_core],
             "p": np.ascontiguousarray(p[:, c * ntok_core:(c + 1) * ntok_core])}
        for name in WEIGHT_SHAPES:
            m[name] = np.ascontiguousarray(inputs[name], dtype=np.float32)
        m.update({kk_: vv_ for kk_, vv_ in consts.items() if kk_ in prog.inputs})
        in_maps.append(m)
    res = run_bass_kernel_spmd(nc, in_maps, core_ids=list(range(ncore)), trace=trace)
    y = np.concatenate([r["y"] for r in res.results], axis=0)
    if dbg:
        res.dumps = {n: res.results[0]["dbg_" + n] for n in prog.dumps}
    return y, res


def kernel(**inputs):
    ntok_core = BATCH * SEQ // NCORE
    y, _ = run(inputs, ntok_core)
    return y.reshape(BATCH, SEQ, D).astype(np.float32)
```

```python
import contextlib
import numpy as np
import ml_dtypes
import concourse.bass as bass
import concourse.mybir as mybir
from concourse.bass_utils import run_bass_kernel_spmd

F32 = mybir.dt.float32
BF16 = mybir.dt.bfloat16
AF = mybir.ActivationFunctionType
ALU = mybir.AluOpType
AX = mybir.AxisListType

D = 1024
NCH = D // 128
SEQ = 2048
BATCH = 32
NCORE = 8
CHUNK = 64
PLE_DIM = 256
FFN_H = 2816
NHC = FFN_H // 128
NORM_EPS = 1e-6


class Sem:
    def __init__(self, handle, name, is_dma=False):
        self.h = handle
        self.name = name
        self.is_dma = is_dma


class Buf:
    def __init__(self, t, name):
        self.t = t
        self.name = name
        self.w = {}
        self.r = {}

    def __getitem__(self, key):
        return self.t[key]


class Eng:
    def __init__(self, name, h, sem):
        self.name = name
        self.h = h
        self.sem = sem
        self.cnt = 0
        self.seen = {}


class K:
    def __init__(self, nc, n_dma_sems=32):
        self.nc = nc
        self.es = contextlib.ExitStack()
        self.eng = {}
        for name, h in [("pe", nc.tensor), ("act", nc.scalar), ("dve", nc.vector),
                        ("pool", nc.gpsimd), ("sp", nc.sync)]:
            self.eng[name] = Eng(name, h, self.new_sem("e_" + name))
        self.dsems = [[self.new_sem(f"d{i}", True), 0] for i in range(n_dma_sems)]
        self.dnext = 0
        self.nbuf = 0

    def new_sem(self, name, is_dma=False):
        return Sem(self.es.enter_context(self.nc.semaphore(name)), name, is_dma)

    def sbuf(self, shape, dtype, name=None, scope=None):
        self.nbuf += 1
        name = f"{name or 'sb'}_{self.nbuf}"
        t = (scope or self.es).enter_context(self.nc.sbuf_tensor(name, list(shape), dtype))
        return Buf(t, name)

    def psum(self, shape, dtype, name=None, scope=None):
        self.nbuf += 1
        name = f"{name or 'ps'}_{self.nbuf}"
        t = (scope or self.es).enter_context(self.nc.psum_tensor(name, list(shape), dtype))
        return Buf(t, name)

    def dram(self, shape, dtype, name, kind="Internal"):
        t = self.nc.dram_tensor(name, list(shape), dtype, kind=kind)
        return Buf(t.ap(), name)

    def _deps(self, e, R, W, skip_self):
        need = {}

        def add(s, v):
            if need.get(s, 0) < v:
                need[s] = v

        for b in R:
            for s, v in b.w.items():
                add(s, v)
        for b in W:
            for s, v in b.w.items():
                add(s, v)
            for s, v in b.r.items():
                add(s, v)
        for s, v in need.items():
            if skip_self and s is e.sem:
                continue
            if e.seen.get(s, 0) >= v:
                continue
            e.h.wait_ge(s.h, v)
            e.seen[s] = v

    def _mark(self, ev, R, W, dma=False):
        s, v = ev
        for b in R:
            if b.r.get(s, 0) < v:
                b.r[s] = v
        for b in W:
            if dma:
                b.w = {ss: vv for ss, vv in b.w.items() if ss.is_dma}
                b.w[s] = v
            else:
                b.w = {s: v}
            b.r = {}

    def barrier(self):
        for e in self.eng.values():
            for o in self.eng.values():
                if o is e or o.cnt == 0:
                    continue
                if e.seen.get(o.sem, 0) < o.cnt:
                    e.h.wait_ge(o.sem.h, o.cnt)
                    e.seen[o.sem] = o.cnt
            for s, v in self.dsems:
                if v > 0 and e.seen.get(s, 0) < v:
                    e.h.wait_ge(s.h, v)
                    e.seen[s] = v

    def op(self, en, fn, R=(), W=()):
        e = self.eng[en]
        self._deps(e, R, W, skip_self=(en == "pe"))
        ins = fn(e.h)
        e.cnt += 1
        ins.then_inc(e.sem.h, 1)
        self._mark((e.sem, e.cnt), R, W)

    def dma(self, en, out, in_, R=(), W=(), **kw):
        e = self.eng[en]
        self._deps(e, R, W, skip_self=False)
        slot = self.dsems[self.dnext]
        self.dnext = (self.dnext + 1) % len(self.dsems)
        s, v = slot
        if v > 0 and e.seen.get(s, 0) < v:
            e.h.wait_ge(s.h, v)
            e.seen[s] = v
        e.h.dma_start(out=out, in_=in_, **kw).then_inc(s.h, 16)
        slot[1] = v + 16
        self._mark((s, v + 16), R, W, dma=True)

    def wait_all(self, en, bufs):
        e = self.eng[en]
        self._deps(e, bufs, (), skip_self=False)

    def close(self):
        self.es.close()


class Prog:
    def __init__(self, ntok, layers=(0, 1), stages=("mix", "ffn"), dbg=False):
        self.ntok = ntok
        self.layers = layers
        self.stages = stages
        nc = bass.Bass("TRN2", target_bir_lowering=False)
        self.nc = nc
        self.k = K(nc)
        self.inputs = {}
        self.dbg = dbg
        self.dumps = {}
        import os
        self.upto = os.environ.get('KUPTO', '')
        self.skip = os.environ.get('KSKIP', '')

    def dump(self, name, buf, ap, shape, dtype=F32):
        if not self.dbg or name in self.dumps:
            return
        t = self.nc.dram_tensor("dbg_" + name, list(shape), dtype, kind="ExternalOutput")
        b = Buf(t.ap(), "dbg_" + name)
        self.dumps[name] = b
        self.k.dma("sp", b[:], ap, R=[buf], W=[b])

    def inp(self, name, shape, dtype=F32):
        if name in self.inputs:
            return self.inputs[name]
        t = self.nc.dram_tensor(name, list(shape), dtype, kind="ExternalInput")
        b = Buf(t.ap(), name)
        self.inputs[name] = b
        return b

    def setup_consts(self):
        k = self.k
        self.c_ident_f = k.sbuf([128, 128], F32, "identf")
        self.c_ident_b = k.sbuf([128, 128], BF16, "identb")
        self.c_mean = k.sbuf([128, 128], BF16, "meanmat")
        cid = self.inp("c_ident", [128, 128])
        k.dma("sp", self.c_ident_f[:], cid[:], R=[cid], W=[self.c_ident_f])
        k.op("dve", lambda e: e.tensor_copy(out=self.c_ident_b[:], in_=self.c_ident_f[:]),
             R=[self.c_ident_f], W=[self.c_ident_b])
        k.op("dve", lambda e: e.memset(self.c_mean[:], 1.0 / D), W=[self.c_mean])
        self.c_eps = k.sbuf([128, 4], F32, "ceps")
        k.op("dve", lambda e: e.memset(self.c_eps[:, 0:1], NORM_EPS), W=[self.c_eps])
        self.ps = [k.psum([128, 512], F32, f"bank{i}") for i in range(8)]
        self.psi = 0
        self.ps_rot = list(range(8))

    def bank(self):
        b = self.ps[self.ps_rot[self.psi % len(self.ps_rot)]]
        self.psi += 1
        return b

    def conv_weight(self, w_ap, Kdim, N, name, stage_bufs):
        k = self.k
        kc = Kdim // 128
        assert kc == 8
        ng = (N + 511) // 512
        dst = k.dram([ng, 128, kc, 512], BF16, name)
        src = w_ap.rearrange("(c p) n -> p c n", p=128)
        for g in range(ng):
            n0, n1 = g * 512, min(N, (g + 1) * 512)
            st = stage_bufs[g % 2]
            k.dma("pool", st[:, 0:kc, 0:n1 - n0], src[:, :, n0:n1], W=[st])
            if n1 - n0 == 512:
                k.dma("sp", dst[g], st[:, 0:kc, :], R=[st], W=[dst])
            else:
                k.dma("sp", dst[g, :, :, 0:n1 - n0], st[:, 0:kc, 0:n1 - n0], R=[st], W=[dst])
        return dst

    def rmsnorm(self, xs, gcol, outs, n, tmp_sq, tmp_rstd, eps=NORM_EPS):
        k = self.k
        ps = self.bank()
        for c in range(NCH):
            xb, xa = xs[c]
            k.op("act", lambda e, xa=xa, c=c: e.activation(out=tmp_sq[:, c, 0:n], in_=xa, func=AF.Square),
                 R=[xb], W=[tmp_sq])
        for c in range(NCH):
            k.op("pe", lambda e, c=c: e.matmul(ps[:, 0:n], lhsT=self.c_mean[:], rhs=tmp_sq[:, c, 0:n],
                                               start=(c == 0), stop=(c == NCH - 1)),
                 R=[self.c_mean, tmp_sq], W=[ps])
        k.op("act", lambda e: e.activation(out=tmp_rstd[:, 0:n], in_=ps[:, 0:n], func=AF.Sqrt, bias=self.c_eps[:, 0:1],
                                           scale=1.0), R=[ps, self.c_eps], W=[tmp_rstd])
        k.op("dve", lambda e: e.reciprocal(out=tmp_rstd[:, 0:n], in_=tmp_rstd[:, 0:n]), R=[tmp_rstd], W=[tmp_rstd])
        self.dump("sq", tmp_sq, tmp_sq[:], [128, NCH, 512], BF16)
        self.dump("rstd", tmp_rstd, tmp_rstd[:], [128, 512], F32)
        gb, ga = gcol
        self.dump("gcol", gb, ga, [128, NCH], F32)
        for c in range(NCH):
            xb, xa = xs[c]
            ob, oa = outs[c]
            k.op("dve", lambda e, xa=xa, oa=oa, c=c: e.scalar_tensor_tensor(
                out=oa, in0=xa, scalar=ga[:, c:c + 1], in1=tmp_rstd[:, 0:n], op0=ALU.mult, op1=ALU.mult),
                R=[xb, gb, tmp_rstd], W=[ob])

    def phase_in(self, x_in):
        k = self.k
        ntok = self.ntok
        self.xT = k.dram([128, NCH, ntok], F32, "xT_scr")
        with contextlib.ExitStack() as sc:
            xin = [k.sbuf([128, 4, D], F32, "xin", sc) for _ in range(2)]
            xo = [k.sbuf([128, NCH, 512], F32, "xo", sc) for _ in range(2)]
            xv = x_in.t.rearrange("(g j p) d -> g p j d", p=128, j=4)
            for g in range(ntok // 512):
                a = xin[g % 2]
                o = xo[g % 2]
                k.dma("sp", a[:], xv[g], R=[x_in], W=[a])
                for c in range(NCH):
                    ps = self.bank()
                    for j in range(4):
                        k.op("pe", lambda e, c=c, j=j, ps=ps: e.transpose(
                            ps[:, j * 128:(j + 1) * 128], a[:, j, c * 128:(c + 1) * 128], self.c_ident_f[:]),
                            R=[a, self.c_ident_f], W=[ps])
                    en = "act" if c % 2 else "dve"
                    if en == "act":
                        k.op("act", lambda e, c=c, ps=ps: e.copy(out=o[:, c, :], in_=ps[:]), R=[ps], W=[o])
                    else:
                        k.op("dve", lambda e, c=c, ps=ps: e.tensor_copy(out=o[:, c, :], in_=ps[:]), R=[ps], W=[o])
                k.dma("sp", self.xT[:, :, g * 512:(g + 1) * 512], o[:], R=[o], W=[self.xT])
        k.barrier()

    def phase_out(self, y_out):
        k = self.k
        ntok = self.ntok
        with contextlib.ExitStack() as sc:
            xi = [k.sbuf([128, NCH, 512], F32, "oxi", sc) for _ in range(2)]
            xo = [k.sbuf([128, 4, D], F32, "oxo", sc) for _ in range(2)]
            yv = y_out.t.rearrange("(g j p) d -> g p j d", p=128, j=4)
            for g in range(ntok // 512):
                a = xi[g % 2]
                o = xo[g % 2]
                k.dma("sp", a[:], self.xT[:, :, g * 512:(g + 1) * 512], R=[self.xT], W=[a])
                for j in range(4):
                    for ch in range(2):
                        ps = self.bank()
                        for cc in range(4):
                            c = ch * 4 + cc
                            k.op("pe", lambda e, c=c, cc=cc, j=j, ps=ps: e.transpose(
                                ps[:, cc * 128:(cc + 1) * 128], a[:, c, j * 128:(j + 1) * 128], self.c_ident_f[:]),
                                R=[a, self.c_ident_f], W=[ps])
                        if ch:
                            k.op("act", lambda e, j=j, ch=ch, ps=ps: e.copy(
                                out=o[:, j, ch * 512:(ch + 1) * 512], in_=ps[:]), R=[ps], W=[o])
                        else:
                            k.op("dve", lambda e, j=j, ch=ch, ps=ps: e.tensor_copy(
                                out=o[:, j, ch * 512:(ch + 1) * 512], in_=ps[:]), R=[ps], W=[o])
                k.dma("sp", yv[g], o[:], R=[o], W=[y_out])
        k.barrier()

    def phase_ffn(self, li, W):
        k = self.k
        ntok = self.ntok
        TB = 512
        with contextlib.ExitStack() as sc:
            stage = [k.sbuf([128, 8, 512], BF16, "cst", sc) for _ in range(2)]
            wg_s = self.conv_weight(W["ffn_w_gate"].t[li], D, FFN_H, f"wg_s{li}", stage)
            wu_s = self.conv_weight(W["ffn_w_up"].t[li], D, FFN_H, f"wu_s{li}", stage)
            wd = k.sbuf([128, NHC, D], BF16, "wd", sc)
            wple = k.sbuf([128, 2, D], BF16, "wple", sc)
            wpg = k.sbuf([128, NCH, D], BF16, "wpg", sc)
            k.dma("pool", wd[:], W["ffn_w_down"].t[li].rearrange("(c p) n -> p c n", p=128),
                  R=[W["ffn_w_down"]], W=[wd])
            k.dma("pool", wple[:], W["ple_w"].t[li].rearrange("(c p) n -> p c n", p=128),
                  R=[W["ple_w"]], W=[wple])
            k.dma("pool", wpg[:], W["ple_w_gate"].t[li].rearrange("(c p) n -> p c n", p=128),
                  R=[W["ple_w_gate"]], W=[wpg])
            gcol = k.sbuf([128, 2, NCH], F32, "gcol", sc)
            k.dma("sp", gcol[:, 0, :], W["ffn_norm"].t[li].rearrange("(c p) -> p c", p=128),
                  R=[W["ffn_norm"]], W=[gcol], allow_slow_non_contiguous=True)
            k.dma("sp", gcol[:, 1, :], W["ple_norm"].t[li].rearrange("(c p) -> p c", p=128),
                  R=[W["ple_norm"]], W=[gcol], allow_slow_non_contiguous=True)
            xb = [k.sbuf([128, NCH, TB], F32, "fx", sc) for _ in range(2)]
            hT = k.sbuf([128, NCH, TB], BF16, "fh", sc)
            sq = k.sbuf([128, NCH, TB], BF16, "fsq", sc)
            rstd = k.sbuf([128, TB], F32, "frstd", sc)
            act = k.sbuf([128, NHC, TB], BF16, "fact", sc)
            wgb = [k.sbuf([128, NCH, 512], BF16, "wgb", sc) for _ in range(2)]
            wub = [k.sbuf([128, NCH, 512], BF16, "wub", sc) for _ in range(2)]
            sg = [k.sbuf([128, TB], F32, "fsg", sc) for _ in range(2)]
            pin = k.sbuf([128, 4, PLE_DIM], F32, "pin", sc)
            pT = k.sbuf([128, 2, TB], BF16, "pT", sc)
            pe_sb = k.sbuf([128, TB], F32, "pesb", sc)
            p_in = W["p"]
            pv = p_in.t[li].rearrange("(g j p) d -> g p j d", p=128, j=4)
            ngrp = (NHC + 3) // 4
            it = 0
            for tb in range(ntok // TB):
                x = xb[tb % 2]
                k.dma("sp", x[:], self.xT[:, :, tb * TB:(tb + 1) * TB], R=[self.xT], W=[x])
                k.dma("sp", pin[:], pv[tb], R=[p_in], W=[pin])
                self.dump(f"xl{li}", x, x[:], [128, NCH, TB], F32)
                self.rmsnorm([(x, x[:, c, :]) for c in range(NCH)], (gcol, gcol[:, 0, :]),
                             [(hT, hT[:, c, :]) for c in range(NCH)], TB, sq, rstd)
                self.dump(f"hT{li}", hT, hT[:], [128, NCH, TB], BF16)
                for jg in range(ngrp):
                    ncol = min(512, FFN_H - jg * 512)
                    wgt, wut = wgb[it % 2], wub[it % 2]
                    it += 1
                    k.dma("sp", wgt[:, :, 0:ncol], wg_s[jg, :, :, 0:ncol], R=[wg_s], W=[wgt])
                    k.dma("sp", wut[:, :, 0:ncol], wu_s[jg, :, :, 0:ncol], R=[wu_s], W=[wut])
                    for jj in range(ncol // 128):
                        j = jg * 4 + jj
                        pg, pu = self.bank(), self.bank()
                        for c in range(NCH):
                            k.op("pe", lambda e, c=c, jj=jj, pg=pg, wgt=wgt: e.matmul(
                                pg[:], lhsT=wgt[:, c, jj * 128:(jj + 1) * 128], rhs=hT[:, c, :],
                                start=(c == 0), stop=(c == NCH - 1)), R=[wgt, hT], W=[pg])
                        for c in range(NCH):
                            k.op("pe", lambda e, c=c, jj=jj, pu=pu, wut=wut: e.matmul(
                                pu[:], lhsT=wut[:, c, jj * 128:(jj + 1) * 128], rhs=hT[:, c, :],
                                start=(c == 0), stop=(c == NCH - 1)), R=[wut, hT], W=[pu])
                        s = sg[j % 2]
                        k.op("act", lambda e, s=s, pg=pg: e.activation(out=s[:], in_=pg[:], func=AF.Silu),
                             R=[pg], W=[s])
                        k.op("dve", lambda e, s=s, pu=pu, j=j: e.tensor_tensor(
                            out=act[:, j, :], in0=s[:], in1=pu[:], op=ALU.mult), R=[s, pu], W=[act])
                self.dump(f"act{li}", act, act[:], [128, NHC, TB], BF16)
                for n in range(NCH):
                    po = self.bank()
                    for j in range(NHC):
                        k.op("pe", lambda e, n=n, j=j, po=po: e.matmul(
                            po[:], lhsT=wd[:, j, n * 128:(n + 1) * 128], rhs=act[:, j, :],
                            start=(j == 0), stop=(j == NHC - 1)), R=[wd, act], W=[po])
                    k.op("dve", lambda e, n=n, po=po, x=x: e.tensor_tensor(
                        out=x[:, n, :], in0=x[:, n, :], in1=po[:], op=ALU.add), R=[po, x], W=[x])
                self.dump(f"xf{li}", x, x[:], [128, NCH, TB], F32)
                for j in range(4):
                    ps = self.bank()
                    for c2 in range(2):
                        k.op("pe", lambda e, j=j, c2=c2, ps=ps: e.transpose(
                            ps[:, c2 * 128:(c2 + 1) * 128], pin[:, j, c2 * 128:(c2 + 1) * 128], self.c_ident_f[:]),
                            R=[pin, self.c_ident_f], W=[ps])
                    k.op("act", lambda e, j=j, ps=ps: e.copy(
                        out=pT[:, :, j * 128:(j + 1) * 128],
                        in_=ps[:, 0:256].rearrange("p (c t) -> p c t", c=2)), R=[ps], W=[pT])
                self.rmsnorm([(x, x[:, c, :]) for c in range(NCH)], (gcol, gcol[:, 1, :]),
                             [(hT, hT[:, c, :]) for c in range(NCH)], TB, sq, rstd)
                for n in range(NCH):
                    pg, pp = self.bank(), self.bank()
                    for c in range(NCH):
                        k.op("pe", lambda e, c=c, n=n, pg=pg: e.matmul(
                            pg[:], lhsT=wpg[:, c, n * 128:(n + 1) * 128], rhs=hT[:, c, :],
                            start=(c == 0), stop=(c == NCH - 1)), R=[wpg, hT], W=[pg])
                    for c in range(2):
                        k.op("pe", lambda e, c=c, n=n, pp=pp: e.matmul(
                            pp[:], lhsT=wple[:, c, n * 128:(n + 1) * 128], rhs=pT[:, c, :],
                            start=(c == 0), stop=(c == 1)), R=[wple, pT], W=[pp])
                    s = sg[n % 2]
                    k.op("act", lambda e, s=s, pg=pg: e.activation(out=s[:], in_=pg[:], func=AF.Sigmoid),
                         R=[pg], W=[s])
                    k.op("dve", lambda e, s=s, pp=pp: e.tensor_tensor(
                        out=pe_sb[:], in0=s[:], in1=pp[:], op=ALU.mult), R=[s, pp], W=[pe_sb])
                    k.op("dve", lambda e, n=n, x=x: e.tensor_tensor(
                        out=x[:, n, :], in0=x[:, n, :], in1=pe_sb[:], op=ALU.add), R=[pe_sb, x], W=[x])
                k.dma("sp", self.xT[:, :, tb * TB:(tb + 1) * TB], x[:], R=[x], W=[self.xT])
        k.barrier()


    def bank_rot(self, idxs):
        self.ps_rot = list(idxs)
        self.psi = 0

    def phase_mix0(self, W):
        k = self.k
        ntok = self.ntok
        T = min(SEQ, ntok)
        nseq = ntok // T
        NQT = T // 128
        NB = 14
        BBASE = 964
        qn_scr = k.dram([128, 4, ntok], BF16, "qn_scr")
        kn_scr = k.dram([128, ntok], BF16, "kn_scr")
        iq_scr = k.dram([128, 2, ntok], BF16, "iq_scr")
        ik_scr = k.dram([128, ntok], BF16, "ik_scr")
        va_scr = k.dram([ntok, 65], BF16, "va_scr")
        iw_scr = k.dram([ntok, 4], F32, "iw_scr")
        ub_scr = k.dram([128, NB, nseq, T + 4], F32, "ub_scr")
        ya_scr = k.dram([128, 4, ntok], BF16, "ya_scr")
        yb_scr = k.dram([128, 4, ntok], BF16, "yb_scr")
        self.yb_scr = yb_scr
        with contextlib.ExitStack() as sc:
            stage = [k.sbuf([128, 8, 512], BF16, "cst", sc) for _ in range(2)]
            win_s = self.conv_weight(W["w_in_e"].t[0], D, 2756, "win_s", stage)
            win = k.sbuf([128, NCH, 2756], BF16, "win", sc)
            for g_ in range(6):
                nc_ = min(512, 2756 - g_ * 512)
                k.dma("sp", win[:, :, g_ * 512:g_ * 512 + nc_], win_s[g_, :, :, 0:nc_], R=[win_s], W=[win])
            gcol = k.sbuf([128, NCH], F32, "gcol", sc)
            k.dma("sp", gcol[:], W["mix_norm"].t[0].rearrange("(c p) -> p c", p=128),
                  R=[W["mix_norm"]], W=[gcol], allow_slow_non_contiguous=True)
            gqk = k.sbuf([128, 2], F32, "gqk", sc)
            for half in range(2):
                k.dma("sp", gqk[half * 64:(half + 1) * 64, 0:1], W["a_q_gain"].t[0].rearrange("(p o) -> p o", o=1),
                      R=[W["a_q_gain"]], W=[gqk])
                k.dma("sp", gqk[half * 64:(half + 1) * 64, 1:2], W["a_k_gain"].t[0].rearrange("(p o) -> p o", o=1),
                      R=[W["a_k_gain"]], W=[gqk])
            k.op("dve", lambda e: e.tensor_scalar(out=gqk[:, 0:1], in0=gqk[:, 0:1], scalar1=0.125, scalar2=None,
                                                  op0=ALU.mult), R=[gqk], W=[gqk])
            bd64f = k.sbuf([128, 128], F32, "bd64f", sc)
            bd64 = k.sbuf([128, 128], BF16, "bd64", sc)
            cb64 = self.inp("c_bd64", [128, 128])
            k.dma("sp", bd64f[:], cb64[:], R=[cb64], W=[bd64f])
            k.op("dve", lambda e: e.tensor_copy(out=bd64[:], in_=bd64f[:]), R=[bd64f], W=[bd64])
            zt = k.sbuf([128, NB, 4], F32, "zt", sc)
            k.op("dve", lambda e: e.memset(zt[:], 0.0), W=[zt])
            for s_ in range(nseq):
                k.dma("sp", ub_scr[:, :, s_, 0:4], zt[:], R=[zt], W=[ub_scr])
            xb = [k.sbuf([128, NCH, 512], F32, "a1x", sc) for _ in range(2)]
            hT = k.sbuf([128, NCH, 512], BF16, "a1h", sc)
            sq = k.sbuf([128, NCH, 512], BF16, "a1sq", sc)
            rstd = k.sbuf([128, 512], F32, "a1rstd", sc)
            qsq = k.sbuf([128, 512], BF16, "qsq", sc)
            qr = k.sbuf([128, 512], F32, "qr", sc)
            qno = k.sbuf([128, 4, 512], BF16, "qno", sc)
            kno = k.sbuf([128, 512], BF16, "kno", sc)
            iqo = k.sbuf([128, 2, 512], BF16, "iqo", sc)
            iko = k.sbuf([128, 512], BF16, "iko", sc)
            ubo = k.sbuf([128, NB, 512], F32, "ubo", sc)
            vao = k.sbuf([128, 4, 65], BF16, "vao", sc)
            iwo = k.sbuf([128, 4, 4], F32, "iwo", sc)
            k.op("dve", lambda e: e.memset(vao[:], 1.0), W=[vao])

            def proj(ps, col0, ncol, po=0):
                for c in range(NCH):
                    k.op("pe", lambda e, c=c: e.matmul(ps[po:po + ncol, :], lhsT=win[:, c, col0:col0 + ncol],
                                                      rhs=hT[:, c, :], start=(c == 0), stop=(c == NCH - 1)),
                         R=[win, hT], W=[ps])

            def qknorm(ps, gi, out_ap, out_buf):
                k.op("act", lambda e: e.activation(out=qsq[:], in_=ps[:], func=AF.Square), R=[ps], W=[qsq])
                ps2 = self.bank()
                k.op("pe", lambda e: e.matmul(ps2[:], lhsT=bd64[:], rhs=qsq[:], start=True, stop=True),
                     R=[bd64, qsq], W=[ps2])
                k.op("act", lambda e: e.activation(out=qr[:], in_=ps2[:], func=AF.Sqrt, bias=self.c_eps[:, 0:1],
                                                   scale=1.0), R=[ps2, self.c_eps], W=[qr])
                k.op("dve", lambda e: e.reciprocal(out=qr[:], in_=qr[:]), R=[qr], W=[qr])
                k.op("dve", lambda e: e.scalar_tensor_tensor(out=out_ap, in0=ps[:], scalar=gqk[:, gi:gi + 1],
                                                             in1=qr[:], op0=ALU.mult, op1=ALU.mult),
                     R=[ps, gqk, qr], W=[out_buf])

            for tb in range(ntok // 512):
                s_, off = (tb * 512) // T, (tb * 512) % T
                x = xb[tb % 2]
                k.dma("sp", x[:], self.xT[:, :, tb * 512:(tb + 1) * 512], R=[self.xT], W=[x])
                self.rmsnorm([(x, x[:, c, :]) for c in range(NCH)], (gcol, gcol[:, :]),
                             [(hT, hT[:, c, :]) for c in range(NCH)], 512, sq, rstd)
                for j in range(4):
                    ps = self.bank()
                    proj(ps, j * 128, 128)
                    qknorm(ps, 0, qno[:, j, :], qno)
                ps = self.bank()
                proj(ps, 512, 64, 0)
                proj(ps, 512, 64, 64)
                qknorm(ps, 1, kno[:], kno)
                for j in range(2):
                    ps = self.bank()
                    proj(ps, 640 + j * 128, 128)
                    k.op("act", lambda e, j=j, ps=ps: e.copy(out=iqo[:, j, :], in_=ps[:]), R=[ps], W=[iqo])
                ps = self.bank()
                proj(ps, 896, 64, 0)
                proj(ps, 896, 64, 64)
                k.op("act", lambda e, ps=ps: e.copy(out=iko[:], in_=ps[:]), R=[ps], W=[iko])
                for g in range(NB):
                    ps = self.bank()
                    proj(ps, BBASE + g * 128, 128)
                    if g % 2:
                        k.op("act", lambda e, g=g, ps=ps: e.copy(out=ubo[:, g, :], in_=ps[:]), R=[ps], W=[ubo])
                    else:
                        k.op("dve", lambda e, g=g, ps=ps: e.tensor_copy(out=ubo[:, g, :], in_=ps[:]), R=[ps], W=[ubo])
                for tt in range(4):
                    ps = self.bank()
                    for c in range(NCH):
                        k.op("pe", lambda e, c=c, tt=tt, ps=ps: e.matmul(
                            ps[:, 0:64], lhsT=hT[:, c, tt * 128:(tt + 1) * 128], rhs=win[:, c, 576:640],
                            start=(c == 0), stop=(c == NCH - 1)), R=[hT, win], W=[ps])
                    for c in range(NCH):
                        k.op("pe", lambda e, c=c, tt=tt, ps=ps: e.matmul(
                            ps[:, 64:68], lhsT=hT[:, c, tt * 128:(tt + 1) * 128], rhs=win[:, c, 960:964],
                            start=(c == 0), stop=(c == NCH - 1)), R=[hT, win], W=[ps])
                    k.op("act", lambda e, tt=tt, ps=ps: e.copy(out=vao[:, tt, 0:64], in_=ps[:, 0:64]), R=[ps], W=[vao])
                    k.op("act", lambda e, tt=tt, ps=ps: e.copy(out=iwo[:, tt, :], in_=ps[:, 64:68]), R=[ps], W=[iwo])
                tsl = slice(tb * 512, (tb + 1) * 512)
                k.dma("sp", qn_scr[:, :, tsl], qno[:], R=[qno], W=[qn_scr])
                k.dma("sp", kn_scr[:, tsl], kno[:], R=[kno], W=[kn_scr])
                k.dma("sp", iq_scr[:, :, tsl], iqo[:], R=[iqo], W=[iq_scr])
                k.dma("sp", ik_scr[:, tsl], iko[:], R=[iko], W=[ik_scr])
                k.dma("sp", ub_scr[:, :, s_, 4 + off:4 + off + 512], ubo[:], R=[ubo], W=[ub_scr])
                k.dma("sp", va_scr[tsl, :].rearrange("(j p) n -> p j n", p=128), vao[:], R=[vao], W=[va_scr])
                k.dma("sp", iw_scr[tsl, :].rearrange("(j p) n -> p j n", p=128), iwo[:], R=[iwo], W=[iw_scr])
        k.barrier()
        if self.upto == "A1":
            return
        if "rwkv" not in self.skip:
            self.phase_rwkv(W, ub_scr, yb_scr)
        with contextlib.ExitStack() as sc:
            NBK = T // 128
            KSEL = min(256, T // 4)
            qn = k.sbuf([128, 4, T], BF16, "qn", sc)
            kn = k.sbuf([128, T], BF16, "kn", sc)
            iq = k.sbuf([128, 2, T], BF16, "iq", sc)
            ik = k.sbuf([128, T], BF16, "ik", sc)
            va = k.sbuf([128, NBK, 66], BF16, "va", sc)
            iw = k.sbuf([128, NBK, 4], F32, "iw", sc)
            score = k.sbuf([128, T], F32, "score", sc)
            tmp = [k.sbuf([128, 512], F32, "stmp", sc) for _ in range(2)]
            junk = k.sbuf([128, T], BF16, "sjunk", sc)
            maskf = k.sbuf([128, T], F32, "maskf", sc)
            maskT = k.sbuf([128, NBK, 128], BF16, "maskT", sc)
            mneg2 = k.sbuf([128, 128], F32, "mneg2", sc)
            cm2 = self.inp("c_mneg2", [128, 128])
            k.dma("sp", mneg2[:], cm2[:], R=[cm2], W=[mneg2])
            bs = k.sbuf([128, 8], F32, "bs", sc)
            psb = [k.sbuf([128, 512], BF16, "psb", sc) for _ in range(2)]
            pm = [k.sbuf([128, 4, 128], BF16, "pm", sc) for _ in range(2)]
            psb2 = [[k.sbuf([128, 512], BF16, "psb2", sc) for _ in range(2)] for _ in range(2)]
            pm2 = [[k.sbuf([128, 4, 128], BF16, "pm2", sc) for _ in range(2)] for _ in range(2)]
            ya = k.sbuf([128, 512], F32, "ya", sc)
            yaT = k.sbuf([128, 4, 128], BF16, "yaT", sc)
            rec = k.sbuf([128, 8], F32, "rec", sc)
            po = [self.ps[0], self.ps[1]]
            self.bank_rot(range(2, 8))
            k.op("dve", lambda e: e.memset(va[:], 0.0), W=[va])
            for s_ in range(nseq):
                ssl = slice(s_ * T, (s_ + 1) * T)
                k.dma("sp", qn[:], qn_scr[:, :, ssl], R=[qn_scr], W=[qn])
                k.dma("sp", kn[:], kn_scr[:, ssl], R=[kn_scr], W=[kn])
                k.dma("sp", iq[:], iq_scr[:, :, ssl], R=[iq_scr], W=[iq])
                k.dma("sp", ik[:], ik_scr[:, ssl], R=[ik_scr], W=[ik])
                k.dma("sp", va[:, :, 0:65], va_scr[ssl, :].rearrange("(j p) n -> p j n", p=128), R=[va_scr], W=[va])
                k.dma("sp", iw[:], iw_scr[ssl, :].rearrange("(j p) n -> p j n", p=128), R=[iw_scr], W=[iw])
                for qt in range(NQT):
                    nb = qt + 1
                    S = nb * 128
                    qsl = slice(qt * 128, (qt + 1) * 128)
                    for kb in range(0, S, 512):
                        n = min(512, S - kb)
                        for h in range(4):
                            hp, j = h % 2, h // 2
                            ps = self.bank()
                            k.op("pe", lambda e, ps=ps, hp=hp, j=j, kb=kb, n=n: e.matmul(
                                ps[:, 0:n], lhsT=iq[hp * 64:(hp + 1) * 64, j, qsl], rhs=ik[hp * 64:(hp + 1) * 64, kb:kb + n],
                                start=True, stop=True), R=[iq, ik], W=[ps])
                            if h == 0:
                                k.op("dve", lambda e, ps=ps, kb=kb, n=n, h=h: e.tensor_scalar(
                                    out=score[:, kb:kb + n], in0=ps[:, 0:n], scalar1=0.0, scalar2=iw[:, qt, h:h + 1],
                                    op0=ALU.max, op1=ALU.mult), R=[ps, iw], W=[score])
                            else:
                                t_ = tmp[h % 2]
                                k.op("dve", lambda e, ps=ps, n=n, h=h, t_=t_: e.tensor_scalar(
                                    out=t_[:, 0:n], in0=ps[:, 0:n], scalar1=0.0, scalar2=iw[:, qt, h:h + 1],
                                    op0=ALU.max, op1=ALU.mult), R=[ps, iw], W=[t_])
                                k.op("pool", lambda e, kb=kb, n=n, t_=t_: e.tensor_tensor(
                                    out=score[:, kb:kb + n], in0=score[:, kb:kb + n], in1=t_[:, 0:n], op=ALU.add),
                                    R=[score, t_], W=[score])
                    if 'a4s1' in self.skip:
                        continue
                    k.op("dve", lambda e, S=S: e.tensor_tensor(out=score[:, S - 128:S], in0=score[:, S - 128:S],
                                                               in1=mneg2[:], op=ALU.add), R=[score, mneg2], W=[score])
                    if S > KSEL:
                        k.op("dve", lambda e, S=S: e.reduce_max(out=bs[:, 1:2], in_=score[:, 0:S], axis=AX.X),
                             R=[score], W=[bs])
                        RNG = 512.0
                        k.op("dve", lambda e: e.tensor_scalar(out=bs[:, 0:1], in0=bs[:, 1:2], scalar1=1.0 - RNG,
                                                              scalar2=None, op0=ALU.add), R=[bs], W=[bs])
                        wd_ = RNG
                        for it_ in range(20):
                            wd_ = wd_ * 0.5
                            k.op("dve", lambda e, S=S, wd_=wd_: e.tensor_scalar(
                                out=junk[:, 0:S], in0=score[:, 0:S], scalar1=bs[:, 0:1], scalar2=wd_,
                                op0=ALU.subtract, op1=ALU.is_ge), R=[score, bs], W=[junk])
                            k.op("dve", lambda e, S=S: e.reduce_sum(out=bs[:, 3:4], in_=junk[:, 0:S], axis=AX.X),
                                 R=[junk], W=[bs])
                            k.op("dve", lambda e, wd_=wd_: e.tensor_scalar(out=bs[:, 4:5], in0=bs[:, 3:4], scalar1=KSEL - 0.5,
                                                                           scalar2=wd_, op0=ALU.is_ge, op1=ALU.mult),
                                 R=[bs], W=[bs])
                            k.op("dve", lambda e: e.tensor_tensor(out=bs[:, 0:1], in0=bs[:, 0:1], in1=bs[:, 4:5],
                                                                  op=ALU.add), R=[bs], W=[bs])
                    else:
                        k.op("dve", lambda e: e.memset(bs[:, 0:1], -1e29), W=[bs])
                    if 'a4s2' in self.skip:
                        continue
                    k.op("dve", lambda e, S=S: e.tensor_scalar(out=maskf[:, 0:S], in0=score[:, 0:S], scalar1=bs[:, 0:1],
                                                               scalar2=None, op0=ALU.is_ge), R=[score, bs], W=[maskf])
                    for b4 in range(0, nb, 4):
                        nn = min(4, nb - b4)
                        ps = self.bank()
                        for bb in range(nn):
                            b = b4 + bb
                            k.op("pe", lambda e, ps=ps, bb=bb, b=b: e.transpose(
                                ps[:, bb * 128:(bb + 1) * 128], maskf[:, b * 128:(b + 1) * 128], self.c_ident_f[:]),
                                R=[maskf, self.c_ident_f], W=[ps])
                        k.op("act", lambda e, ps=ps, b4=b4, nn=nn: e.copy(
                            out=maskT[:, b4:b4 + nn, :], in_=ps[:, 0:nn * 128].rearrange("p (b t) -> p b t", t=128)),
                            R=[ps], W=[maskT])
                    if 'a4s3' in self.skip:
                        continue
                    for hg in range(2):
                        k.op("dve", lambda e, hg=hg: e.memset(po[hg][:], 0.0), W=[po[hg]])
                    def att1(b, hg):
                        bsl = slice(b * 128, (b + 1) * 128)
                        ps = self.bank()
                        for hh in range(4):
                            hp, j = hg, hh
                            k.op("pe", lambda e, ps=ps, hh=hh, hp=hp, j=j, bsl=bsl: e.matmul(
                                ps[:, hh * 128:(hh + 1) * 128], lhsT=kn[hp * 64:(hp + 1) * 64, bsl],
                                rhs=qn[hp * 64:(hp + 1) * 64, j, qsl], start=True, stop=True), R=[kn, qn], W=[ps])
                        pb = psb2[b % 2][hg]
                        pmm = pm2[b % 2][hg]
                        k.op("act", lambda e, ps=ps, pb=pb: e.activation(out=pb[:], in_=ps[:], func=AF.Exp),
                             R=[ps], W=[pb])
                        for hh in range(4):
                            k.op("pool" if hh % 2 else "dve", lambda e, pb=pb, pmm=pmm, b=b, hh=hh: e.tensor_tensor(
                                out=pmm[:, hh, :], in0=pb[:, hh * 128:(hh + 1) * 128],
                                in1=maskT[:, b, :], op=ALU.mult), R=[pb, maskT], W=[pmm])

                    def att2(b, hg):
                        pmm = pm2[b % 2][hg]
                        for hh in range(4):
                            k.op("pe", lambda e, hg=hg, hh=hh, pmm=pmm, b=b: e.matmul(
                                po[hg][:, hh * 128:hh * 128 + 66], lhsT=pmm[:, hh, :], rhs=va[:, b, 0:66],
                                start=False, stop=(b == nb - 1), skip_group_check=True), R=[pmm, va], W=[po[hg]])

                    for b in range(nb):
                        for hg in range(2):
                            att1(b, hg)
                            if b > 0:
                                att2(b - 1, hg)
                    for hg in range(2):
                        att2(nb - 1, hg)
                    if 'a4s4' in self.skip:
                        continue
                    for hg in range(2):
                        for hh in range(4):
                            h = 2 * hh + hg
                            k.op("dve", lambda e, hg=hg, hh=hh, h=h: e.reciprocal(
                                out=rec[:, h:h + 1], in_=po[hg][:, hh * 128 + 64:hh * 128 + 65]), R=[po[hg]], W=[rec])
                            k.op("dve", lambda e, hg=hg, hh=hh, h=h: e.tensor_scalar(
                                out=ya[:, h * 64:(h + 1) * 64], in0=po[hg][:, hh * 128:hh * 128 + 64],
                                scalar1=rec[:, h:h + 1], scalar2=None, op0=ALU.mult), R=[po[hg], rec], W=[ya])
                    ps = self.bank()
                    for j in range(4):
                        k.op("pe", lambda e, ps=ps, j=j: e.transpose(
                            ps[:, j * 128:(j + 1) * 128], ya[:, j * 128:(j + 1) * 128], self.c_ident_f[:]),
                            R=[ya, self.c_ident_f], W=[ps])
                    k.op("act", lambda e, ps=ps: e.copy(out=yaT[:], in_=ps[:].rearrange("p (j t) -> p j t", j=4)),
                         R=[ps], W=[yaT])
                    t0 = s_ * T + qt * 128
                    k.dma("sp", ya_scr[:, :, t0:t0 + 128], yaT[:], R=[yaT], W=[ya_scr])
            self.bank_rot(range(8))
        k.barrier()
        with contextlib.ExitStack() as sc:
            wo = k.sbuf([128, NCH, D], BF16, "wo", sc)
            k.dma("pool", wo[:], W["w_out_e"].t[0].rearrange("(c p) n -> p c n", p=128), R=[W["w_out_e"]], W=[wo])
            yab = k.sbuf([128, NCH, 512], BF16, "yab", sc)
            x = k.sbuf([128, NCH, 512], F32, "a5x", sc)
            if "rwkv" in self.skip:
                k.op("dve", lambda e: e.memset(yab[:], 0.0), W=[yab])
            for tb in range(ntok // 512):
                tsl = slice(tb * 512, (tb + 1) * 512)
                k.dma("sp", yab[:, 0:4, :], ya_scr[:, :, tsl], R=[ya_scr], W=[yab])
                if "rwkv" not in self.skip:
                    k.dma("sp", yab[:, 4:8, :], yb_scr[:, :, tsl], R=[yb_scr], W=[yab])
                k.dma("sp", x[:], self.xT[:, :, tsl], R=[self.xT], W=[x])
                for n in range(NCH):
                    pso = self.bank()
                    for c in range(NCH):
                        k.op("pe", lambda e, n=n, c=c, pso=pso: e.matmul(
                            pso[:], lhsT=wo[:, c, n * 128:(n + 1) * 128], rhs=yab[:, c, :],
                            start=(c == 0), stop=(c == NCH - 1)), R=[wo, yab], W=[pso])
                    k.op("dve", lambda e, n=n, pso=pso: e.tensor_tensor(
                        out=x[:, n, :], in0=x[:, n, :], in1=pso[:], op=ALU.add), R=[pso, x], W=[x])
                k.dma("sp", self.xT[:, :, tsl], x[:], R=[x], W=[self.xT])
        k.barrier()

    def phase_rwkv(self, W, ub_scr, yb_scr):
        k = self.k
        ntok = self.ntok
        T = min(SEQ, ntok)
        nseq = ntok // T
        NB = 14
        names = ["At", "rt", "Bt", "kt", "v", "W", "bon", "g"]
        scr = {n_: k.dram([128, 4, ntok], F32, "rw_" + n_) for n_ in names}
        with contextlib.ExitStack() as sc:
            w2a2 = k.sbuf([128, 512], BF16, "w2a2", sc)
            g2 = k.sbuf([128, 512], BF16, "g2", sc)
            k.dma("pool", w2a2[0:64, :], W["b_w2"].t[0], R=[W["b_w2"]], W=[w2a2])
            k.dma("pool", w2a2[64:128, :], W["b_a2"].t[0], R=[W["b_a2"]], W=[w2a2])
            k.dma("pool", g2[:], W["b_g2"].t[0], R=[W["b_g2"]], W=[g2])
            cols = k.sbuf([128, 8, 4], F32, "rwcols", sc)
            for i_, nm in enumerate(["b_w0", "b_a0", "b_k_k", "b_k_a", "b_r_k"]):
                src = W[nm].t[0]
                if nm == "b_r_k":
                    src = src.rearrange("h d -> (h d)")
                k.dma("sp", cols[:, i_, :], src.rearrange("(c p) -> p c", p=128), R=[W[nm]], W=[cols],
                      allow_slow_non_contiguous=True)
            k.op("dve", lambda e: e.tensor_scalar(out=cols[:, 5, :], in0=cols[:, 0, :], scalar1=-1.0, scalar2=None,
                                                  op0=ALU.mult), R=[cols], W=[cols])
            k.op("dve", lambda e: e.memset(cols[:, 6, :], -0.5), W=[cols])
            k.op("dve", lambda e: e.memset(cols[:, 7, :], 1.0), W=[cols])
            mu = k.sbuf([128, NB], F32, "mu", sc)
            k.dma("sp", mu[:], W["b_mu"].t[0].rearrange("(c p) -> p c", p=128), R=[W["b_mu"]], W=[mu],
                  allow_slow_non_contiguous=True)
            bones = k.sbuf([128, 128], F32, "bones", sc)
            cbo = self.inp("c_bones", [128, 128])
            k.dma("sp", bones[:], cbo[:], R=[cbo], W=[bones])
            ubh = k.sbuf([128, NB, 516], F32, "ubh", sc)
            xs = k.sbuf([128, NB, 512], F32, "xs", sc)
            twa = k.sbuf([128, 512], BF16, "twa", sc)
            sxg = k.sbuf([128, 512], BF16, "sxg", sc)
            o = {n_: k.sbuf([128, 4, 512], F32, "o_" + n_, sc) for n_ in names}
            tt_ = {n_: k.sbuf([128, 512], F32, "t_" + n_, sc) for n_ in
                   ("t1", "e2", "a", "kk", "sq", "t2", "t3", "k", "cwA", "cwB", "Wex", "Winv")}
            for tb in range(ntok // 512):
                s_, off = (tb * 512) // T, (tb * 512) % T
                k.dma("sp", ubh[:], ub_scr[:, :, s_, off:off + 516], R=[ub_scr], W=[ubh])
                for g in range(NB):
                    k.op("pool", lambda e, g=g: e.tensor_tensor(out=xs[:, g, :], in0=ubh[:, g, 3:515], in1=ubh[:, g, 4:516],
                                                                op=ALU.subtract), R=[ubh], W=[xs])
                    k.op("dve", lambda e, g=g: e.scalar_tensor_tensor(out=xs[:, g, :], in0=xs[:, g, :], scalar=mu[:, g:g + 1],
                                                                      in1=ubh[:, g, 4:516], op0=ALU.mult, op1=ALU.add),
                         R=[xs, mu, ubh], W=[xs])
                k.op("act", lambda e: e.activation(out=twa[0:64, :], in_=xs[0:64, 12, :], func=AF.Tanh), R=[xs], W=[twa])
                k.op("act", lambda e: e.copy(out=twa[64:128, :], in_=xs[64:128, 12, :]), R=[xs], W=[twa])
                k.op("act", lambda e: e.activation(out=sxg[:], in_=xs[:, 13, :], func=AF.Sigmoid), R=[xs], W=[sxg])
                for p in range(4):
                    r_, k0_, v_ = xs[:, p, :], xs[:, 4 + p, :], xs[:, 8 + p, :]
                    psl = slice(p * 128, (p + 1) * 128)
                    ps1 = self.bank()
                    k.op("pe", lambda e, ps1=ps1, psl=psl: e.matmul(ps1[:], lhsT=w2a2[0:64, psl], rhs=twa[0:64, :],
                                                                    start=True, stop=True), R=[w2a2, twa], W=[ps1])
                    ps2 = self.bank()
                    k.op("pe", lambda e, ps2=ps2, psl=psl: e.matmul(ps2[:], lhsT=w2a2[64:128, psl], rhs=twa[64:128, :],
                                                                    start=True, stop=True), R=[w2a2, twa], W=[ps2])
                    ps3 = self.bank()
                    k.op("pe", lambda e, ps3=ps3, psl=psl: e.matmul(ps3[:], lhsT=g2[:, psl], rhs=sxg[:],
                                                                    start=True, stop=True), R=[g2, sxg], W=[ps3])
                    t1, e2, a_, kk_, sq_, t2, t3, k_ = (tt_[n_] for n_ in ("t1", "e2", "a", "kk", "sq", "t2", "t3", "k"))
                    cwA, cwB, Wex, Winv = (tt_[n_] for n_ in ("cwA", "cwB", "Wex", "Winv"))
                    k.op("act", lambda e, ps1=ps1, p=p: e.activation(out=t1[:], in_=ps1[:], func=AF.Exp, scale=-1.0,
                                                                     bias=cols[:, 5, p:p + 1]), R=[ps1, cols], W=[t1])
                    k.op("act", lambda e: e.activation(out=t1[:], in_=t1[:], func=AF.Ln, scale=1.0, bias=cols[:, 7, 0:1]),
                         R=[t1, cols], W=[t1])
                    k.op("act", lambda e: e.activation(out=e2[:], in_=t1[:], func=AF.Exp, scale=-1.0, bias=cols[:, 6, 0:1]),
                         R=[t1, cols], W=[e2])
                    k.op("act", lambda e, ps2=ps2, p=p: e.activation(out=a_[:], in_=ps2[:], func=AF.Sigmoid, scale=1.0,
                                                                     bias=cols[:, 1, p:p + 1]), R=[ps2, cols], W=[a_])
                    k.op("act", lambda e, ps3=ps3, p=p: e.copy(out=o["g"][:, p, :], in_=ps3[:]), R=[ps3], W=[o["g"]])
                    k.op("dve", lambda e, p=p, k0_=k0_: e.tensor_scalar(out=kk_[:], in0=k0_, scalar1=cols[:, 2, p:p + 1],
                                                                       scalar2=None, op0=ALU.mult), R=[xs, cols], W=[kk_])
                    k.op("pool", lambda e: e.tensor_tensor(out=sq_[:], in0=kk_[:], in1=kk_[:], op=ALU.mult), R=[kk_], W=[sq_])
                    ps4 = self.bank()
                    k.op("pe", lambda e, ps4=ps4: e.matmul(ps4[:], lhsT=bones[:], rhs=sq_[:], start=True, stop=True),
                         R=[bones, sq_], W=[ps4])
                    k.op("act", lambda e, ps4=ps4: e.activation(out=t2[:], in_=ps4[:], func=AF.Sqrt), R=[ps4], W=[t2])
                    k.op("dve", lambda e: e.tensor_scalar(out=t2[:], in0=t2[:], scalar1=1e-12, scalar2=None, op0=ALU.max),
                         R=[t2], W=[t2])
                    k.op("dve", lambda e: e.reciprocal(out=t2[:], in_=t2[:]), R=[t2], W=[t2])
                    k.op("dve", lambda e: e.tensor_tensor(out=kk_[:], in0=kk_[:], in1=t2[:], op=ALU.mult), R=[kk_, t2], W=[kk_])
                    k.op("dve", lambda e, p=p: e.tensor_scalar(out=t3[:], in0=a_[:], scalar1=-1.0, scalar2=cols[:, 3, p:p + 1],
                                                              op0=ALU.add, op1=ALU.mult), R=[a_, cols], W=[t3])
                    k.op("dve", lambda e, k0_=k0_: e.scalar_tensor_tensor(out=k_[:], in0=t3[:], scalar=1.0, in1=k0_,
                                                                         op0=ALU.add, op1=ALU.mult), R=[t3, xs], W=[k_])
                    k.op("dve", lambda e, p=p, r_=r_: e.scalar_tensor_tensor(out=t3[:], in0=r_, scalar=cols[:, 4, p:p + 1],
                                                                            in1=k_[:], op0=ALU.mult, op1=ALU.mult),
                         R=[xs, cols, k_], W=[t3])
                    ps5 = self.bank()
                    k.op("pe", lambda e, ps5=ps5: e.matmul(ps5[:], lhsT=bones[:], rhs=t3[:], start=True, stop=True),
                         R=[bones, t3], W=[ps5])
                    k.op("dve", lambda e, ps5=ps5, p=p, v_=v_: e.tensor_tensor(out=o["bon"][:, p, :], in0=v_, in1=ps5[:],
                                                                              op=ALU.mult), R=[xs, ps5], W=[o["bon"]])
                    k.op("dve", lambda e: e.tensor_scalar(out=cwA[:], in0=e2[:], scalar1=-1.0, scalar2=None, op0=ALU.mult),
                         R=[e2], W=[cwA])
                    src_, dst_ = cwA, cwB
                    for j in (1, 2, 4, 8, 16, 32):
                        sv = src_[:].rearrange("p (c l) -> p c l", l=64)
                        dv = dst_[:].rearrange("p (c l) -> p c l", l=64)
                        k.op("pool", lambda e, sv=sv, dv=dv, j=j: e.tensor_copy(out=dv[:, :, 0:j], in_=sv[:, :, 0:j]),
                             R=[src_], W=[dst_])
                        k.op("dve", lambda e, sv=sv, dv=dv, j=j: e.tensor_tensor(out=dv[:, :, j:64], in0=sv[:, :, j:64],
                                                                                 in1=sv[:, :, 0:64 - j], op=ALU.add),
                             R=[src_], W=[dst_])
                        src_, dst_ = dst_, src_
                    cw = src_
                    k.op("act", lambda e, p=p, cw=cw: e.activation(out=o["W"][:, p, :], in_=cw[:], func=AF.Exp), R=[cw], W=[o["W"]])
                    k.op("act", lambda e, cw=cw: e.activation(out=Winv[:], in_=cw[:], func=AF.Exp, scale=-1.0), R=[cw], W=[Winv])
                    k.op("dve", lambda e, cw=cw: e.tensor_tensor(out=Wex[:], in0=cw[:], in1=e2[:], op=ALU.add), R=[cw, e2], W=[Wex])
                    k.op("act", lambda e: e.activation(out=Wex[:], in_=Wex[:], func=AF.Exp), R=[Wex], W=[Wex])
                    k.op("dve", lambda e, p=p: e.scalar_tensor_tensor(out=o["At"][:, p, :], in0=kk_[:], scalar=-1.0, in1=Wex[:],
                                                                     op0=ALU.mult, op1=ALU.mult), R=[kk_, Wex], W=[o["At"]])
                    k.op("dve", lambda e, p=p, r_=r_: e.tensor_tensor(out=o["rt"][:, p, :], in0=r_, in1=o["W"][:, p, :],
                                                                     op=ALU.mult), R=[xs, o["W"]], W=[o["rt"]])
                    k.op("pool", lambda e: e.tensor_tensor(out=t3[:], in0=kk_[:], in1=a_[:], op=ALU.mult), R=[kk_, a_], W=[t3])
                    k.op("dve", lambda e, p=p: e.tensor_tensor(out=o["Bt"][:, p, :], in0=t3[:], in1=Winv[:], op=ALU.mult),
                         R=[t3, Winv], W=[o["Bt"]])
                    k.op("pool", lambda e, p=p: e.tensor_tensor(out=o["kt"][:, p, :], in0=k_[:], in1=Winv[:], op=ALU.mult),
                         R=[k_, Winv], W=[o["kt"]])
                    k.op("pool", lambda e, p=p, v_=v_: e.tensor_copy(out=o["v"][:, p, :], in_=v_), R=[xs], W=[o["v"]])
                for n_ in names:
                    k.dma("sp", scr[n_][:, :, tb * 512:(tb + 1) * 512], o[n_][:], R=[o[n_]], W=[scr[n_]])
        k.barrier()
        if self.upto == "A2":
            return
        with contextlib.ExitStack() as sc:
            L = CHUNK
            ST = k.sbuf([128, 4, 64], F32, "ST", sc)
            stmp = k.sbuf([128, 4, 64], F32, "STtmp", sc)
            ups = k.sbuf([64, 64], F32, "ups", sc)
            upi = k.sbuf([64, 64], F32, "upi", sc)
            los = k.sbuf([64, 64], F32, "los", sc)
            for nm, t_ in (("c_ups", ups), ("c_tri", upi), ("c_los", los)):
                ci_ = self.inputs.get(nm) or self.inp(nm, [64, 64])
                k.dma("sp", t_[:], ci_[:], R=[ci_], W=[t_])
            gn = k.sbuf([128, 2, 4], F32, "gn", sc)
            k.dma("sp", gn[:, 0, :], W["b_gn_g"].t[0].rearrange("(c p) -> p c", p=128), R=[W["b_gn_g"]], W=[gn],
                  allow_slow_non_contiguous=True)
            k.dma("sp", gn[:, 1, :], W["b_gn_b"].t[0].rearrange("(c p) -> p c", p=128), R=[W["b_gn_b"]], W=[gn],
                  allow_slow_non_contiguous=True)
            blk = {n_: k.sbuf([128, 4, 512], F32, "b_" + n_, sc) for n_ in names}
            ybo = k.sbuf([128, 4, 512], BF16, "ybo", sc)
            def tm(nm):
                return k.sbuf([64, 4, 2, 64], F32, nm, sc)
            Pm = [tm("Pm0"), tm("Pm1")]
            Qm = [tm("Qm0"), tm("Qm1")]
            MAK, MRB, MRK = tm("MAK"), tm("MRB"), tm("MRK")
            Vt, Btk, ktk = tm("Vt"), tm("Btk"), tm("ktk")
            X, Xb, Y, Yc, Ysq = tm("X"), tm("Xb"), tm("Y"), tm("Yc"), tm("Ysq")
            st8 = k.sbuf([64, 4, 8], F32, "st8", sc)
            yT = k.sbuf([128, 4, 64], F32, "yT", sc)

            def fm_mm(dst_bank, hp, lhs_blk, rhs_blk, csl):
                hs = slice(hp * 64, (hp + 1) * 64)
                for p in range(4):
                    k.op("pe", lambda e, p=p: e.matmul(dst_bank[0:64, p * 64:(p + 1) * 64], lhsT=lhs_blk[hs, p, csl],
                                                       rhs=rhs_blk[hs, p, csl], start=True, stop=True),
                         R=[lhs_blk, rhs_blk], W=[dst_bank])

            def masked(dst, lhs_blk, rhs_blk, mask, csl):
                for hp in range(2):
                    ps = self.bank()
                    fm_mm(ps, hp, lhs_blk, rhs_blk, csl)
                    k.op("dve", lambda e, ps=ps, hp=hp: e.tensor_tensor(
                        out=dst[:, :, hp, :], in0=ps[0:64, 0:256].rearrange("t (p s) -> t p s", p=4),
                        in1=mask[:, :].unsqueeze(1).to_broadcast([64, 4, 64]), op=ALU.mult), R=[ps, mask], W=[dst])

            def tok_T(dst, src_blk, csl):
                ps = self.bank()
                for p in range(4):
                    k.op("pe", lambda e, p=p, ps=ps: e.transpose(ps[0:64, p * 128:(p + 1) * 128], src_blk[:, p, csl],
                                                                 self.c_ident_f[:]), R=[src_blk, self.c_ident_f], W=[ps])
                k.op("act", lambda e, ps=ps: e.copy(out=dst[:].rearrange("t p h v -> t (p h v)"), in_=ps[0:64, :]),
                     R=[ps], W=[dst])

            def tm_mm(ps, terms):
                for p in range(4):
                    for hp in range(2):
                        c0 = (p * 2 + hp) * 64
                        for i_, (lt, rt_) in enumerate(terms):
                            k.op("pe", lambda e, p=p, hp=hp, c0=c0, i_=i_, lt=lt, rt_=rt_: e.matmul(
                                ps[0:64, c0:c0 + 64], lhsT=lt[:, p, hp, :], rhs=rt_[:, p, hp, :],
                                start=(i_ == 0), stop=(i_ == len(terms) - 1)), R=[lt, rt_], W=[ps])

            for s_ in range(nseq):
                k.op("dve", lambda e: e.memset(ST[:], 0.0), W=[ST])
                for ch in range(T // L):
                    t0 = s_ * T + ch * L
                    if (ch * L) % 512 == 0:
                        for n_ in names:
                            k.dma("sp", blk[n_][:], scr[n_][:, :, t0:t0 + 512], R=[scr[n_]], W=[blk[n_]])
                    c0_ = (ch * L) % 512
                    csl = slice(c0_, c0_ + L)
                    At, rt, Bt, kt, vv, Wb = (blk[n_] for n_ in ("At", "rt", "Bt", "kt", "v", "W"))
                    P, Q = Pm[0], Qm[0]
                    masked(P, Bt, At, ups, csl)
                    masked(Q, At, Bt, los, csl)
                    masked(MAK, kt, At, ups, csl)
                    masked(MRB, Bt, rt, upi, csl)
                    masked(MRK, kt, rt, upi, csl)
                    tok_T(Vt, vv, csl)
                    tok_T(Btk, Bt, csl)
                    tok_T(ktk, kt, csl)
                    psb_ = self.bank()
                    tm_mm(psb_, [(MAK, Vt)])
                    k.op("act", lambda e, psb_=psb_: e.copy(out=Xb[:].rearrange("t p h v -> t (p h v)"), in_=psb_[0:64, :]),
                         R=[psb_], W=[Xb])
                    for hp in range(2):
                        hs = slice(hp * 64, (hp + 1) * 64)
                        ps = self.bank()
                        for p in range(4):
                            k.op("pe", lambda e, p=p, ps=ps, hs=hs: e.matmul(
                                ps[0:64, p * 64:(p + 1) * 64], lhsT=At[hs, p, csl], rhs=ST[hs, p, :], start=True, stop=True),
                                R=[At, ST], W=[ps])
                        k.op("dve", lambda e, ps=ps, hp=hp: e.tensor_tensor(
                            out=X[:, :, hp, :], in0=ps[0:64, 0:256].rearrange("t (p s) -> t p s", p=4),
                            in1=Xb[:, :, hp, :], op=ALU.add), R=[ps, Xb], W=[X])
                    for j in range(6):
                        ps = self.bank()
                        tm_mm(ps, [(P, X)])
                        if j < 5:
                            psP = self.bank()
                            tm_mm(psP, [(Q, P)])
                            psQ = self.bank()
                            tm_mm(psQ, [(P, Q)])
                        k.op("dve", lambda e, ps=ps: e.tensor_tensor(
                            out=X[:].rearrange("t p h v -> t (p h v)"), in0=X[:].rearrange("t p h v -> t (p h v)"),
                            in1=ps[0:64, :], op=ALU.add), R=[ps, X], W=[X])
                        if j < 5:
                            P2, Q2 = Pm[(j + 1) % 2], Qm[(j + 1) % 2]
                            k.op("act", lambda e, psP=psP, P2=P2: e.copy(out=P2[:].rearrange("t p h v -> t (p h v)"),
                                                                         in_=psP[0:64, :]), R=[psP], W=[P2])
                            k.op("dve", lambda e, psQ=psQ, Q2=Q2: e.tensor_copy(out=Q2[:].rearrange("t p h v -> t (p h v)"),
                                                                                in_=psQ[0:64, :]), R=[psQ], W=[Q2])
                            P, Q = P2, Q2
                    SA = X
                    psb_ = self.bank()
                    tm_mm(psb_, [(MRB, SA), (MRK, Vt)])
                    k.op("act", lambda e, psb_=psb_: e.copy(out=Xb[:].rearrange("t p h v -> t (p h v)"), in_=psb_[0:64, :]),
                         R=[psb_], W=[Xb])
                    for hp in range(2):
                        hs = slice(hp * 64, (hp + 1) * 64)
                        ps = self.bank()
                        for p in range(4):
                            k.op("pe", lambda e, p=p, ps=ps, hs=hs: e.matmul(
                                ps[0:64, p * 64:(p + 1) * 64], lhsT=rt[hs, p, csl], rhs=ST[hs, p, :], start=True, stop=True),
                                R=[rt, ST], W=[ps])
                        k.op("dve", lambda e, ps=ps, hp=hp: e.tensor_tensor(
                            out=Y[:, :, hp, :], in0=ps[0:64, 0:256].rearrange("t (p s) -> t p s", p=4),
                            in1=Xb[:, :, hp, :], op=ALU.add), R=[ps, Xb], W=[Y])
                    psS = self.bank()
                    for hp in range(2):
                        for p in range(4):
                            for i_, (lt, rt_) in enumerate(((Btk, SA), (ktk, Vt))):
                                k.op("pe", lambda e, p=p, hp=hp, i_=i_, lt=lt, rt_=rt_: e.matmul(
                                    psS[hp * 64:(hp + 1) * 64, p * 64:(p + 1) * 64], lhsT=lt[:, p, hp, :], rhs=rt_[:, p, hp, :],
                                    start=(i_ == 0), stop=(i_ == 1)), R=[lt, rt_], W=[psS])
                    k.op("dve", lambda e, psS=psS: e.tensor_tensor(
                        out=stmp[:], in0=ST[:], in1=psS[:, 0:256].rearrange("k (p v) -> k p v", p=4), op=ALU.add),
                        R=[ST, psS], W=[stmp])
                    for p in range(4):
                        k.op("dve", lambda e, p=p: e.tensor_scalar(
                            out=ST[:, p, :], in0=stmp[:, p, :], scalar1=Wb[:, p, c0_ + L - 1:c0_ + L], scalar2=None,
                            op0=ALU.mult), R=[stmp, Wb], W=[ST])
                    Y8 = Y[:].rearrange("t p h v -> t (p h) v")
                    k.op("dve", lambda e: e.reduce_sum(out=st8[:, 0, :], in_=Y8, axis=AX.X), R=[Y], W=[st8])
                    k.op("dve", lambda e: e.tensor_scalar(out=st8[:, 0, :], in0=st8[:, 0, :], scalar1=1.0 / 64, scalar2=None,
                                                          op0=ALU.mult), R=[st8], W=[st8])
                    k.op("dve", lambda e: e.tensor_tensor(
                        out=Yc[:].rearrange("t p h v -> t (p h) v"), in0=Y8,
                        in1=st8[:, 0, :].unsqueeze(2).to_broadcast([64, 8, 64]), op=ALU.subtract), R=[Y, st8], W=[Yc])
                    k.op("pool", lambda e: e.tensor_tensor(out=Ysq[:], in0=Yc[:], in1=Yc[:], op=ALU.mult), R=[Yc], W=[Ysq])
                    k.op("dve", lambda e: e.reduce_sum(out=st8[:, 1, :], in_=Ysq[:].rearrange("t p h v -> t (p h) v"),
                                                       axis=AX.X), R=[Ysq], W=[st8])
                    k.op("dve", lambda e: e.tensor_scalar(out=st8[:, 1, :], in0=st8[:, 1, :], scalar1=1.0 / 64, scalar2=64e-5,
                                                          op0=ALU.mult, op1=ALU.add), R=[st8], W=[st8])
                    k.op("act", lambda e: e.activation(out=st8[:, 2, :], in_=st8[:, 1, :], func=AF.Sqrt), R=[st8], W=[st8])
                    k.op("dve", lambda e: e.reciprocal(out=st8[:, 3, :], in_=st8[:, 2, :]), R=[st8], W=[st8])
                    k.op("dve", lambda e: e.tensor_tensor(
                        out=Yc[:].rearrange("t p h v -> t (p h) v"), in0=Yc[:].rearrange("t p h v -> t (p h) v"),
                        in1=st8[:, 3, :].unsqueeze(2).to_broadcast([64, 8, 64]), op=ALU.mult), R=[Yc, st8], W=[Yc])
                    ps = self.bank()
                    for p in range(4):
                        k.op("pe", lambda e, p=p, ps=ps: e.transpose(
                            ps[:, p * 64:(p + 1) * 64], Yc[:, p, :, :].rearrange("t h v -> t (h v)"),
                            self.c_ident_f[0:64, 0:64]), R=[Yc, self.c_ident_f], W=[ps])
                    for p in range(4):
                        k.op("dve", lambda e, p=p, ps=ps: e.tensor_scalar(
                            out=yT[:, p, :], in0=ps[:, p * 64:(p + 1) * 64], scalar1=gn[:, 0, p:p + 1],
                            scalar2=gn[:, 1, p:p + 1], op0=ALU.mult, op1=ALU.add), R=[ps, gn], W=[yT])
                    k.op("pool", lambda e: e.tensor_tensor(out=yT[:], in0=yT[:], in1=blk["bon"][:, :, csl], op=ALU.add),
                         R=[yT, blk["bon"]], W=[yT])
                    k.op("pool", lambda e: e.tensor_tensor(out=ybo[:, :, csl], in0=yT[:], in1=blk["g"][:, :, csl], op=ALU.mult),
                         R=[yT, blk["g"]], W=[ybo])
                    if c0_ + L == 512 or (ch + 1) * L == T:
                        b0 = t0 + L - (c0_ + L)
                        k.dma("sp", yb_scr[:, :, b0:b0 + c0_ + L], ybo[:, :, 0:c0_ + L], R=[ybo], W=[yb_scr])
        k.barrier()

    def phase_mix1(self, W):
        k = self.k
        ntok = self.ntok
        T = min(SEQ, ntok)
        nseq = ntok // T
        NI = 16
        xm_scr = k.dram([128, NI, nseq, T + 4], BF16, "xm_scr")
        zs_scr = k.dram([128, NI, ntok], BF16, "zs_scr")
        xc_scr = k.dram([128, NI, ntok], BF16, "xc_scr")
        q_scr = k.dram([128, NI, ntok], BF16, "q_scr")
        k_scr = k.dram([128, NI, ntok], BF16, "k_scr")
        Kt_scr = k.dram([ntok, 2048], BF16, "Kt_scr")
        Vt_scr = k.dram([ntok, 2048], BF16, "Vt_scr")
        g_scr = k.dram([ntok, 8], F32, "g_scr")
        y_scr = k.dram([ntok, 2048], F32, "y_scr")
        QS = 512.0 ** -0.5
        with contextlib.ExitStack() as sc:
            stage = [k.sbuf([128, 8, 512], BF16, "cst", sc) for _ in range(2)]
            wup_s = self.conv_weight(W["c_w_up"].t[0], D, 4096, "wup_s", stage)
            gcol = k.sbuf([128, NCH], F32, "gcol", sc)
            k.dma("sp", gcol[:], W["mix_norm"].t[1].rearrange("(c p) -> p c", p=128),
                  R=[W["mix_norm"]], W=[gcol], allow_slow_non_contiguous=True)
            zt = k.sbuf([128, NI, 4], BF16, "zt", sc)
            k.op("dve", lambda e: e.memset(zt[:], 0.0), W=[zt])
            for s_ in range(nseq):
                k.dma("sp", xm_scr[:, :, s_, 0:4], zt[:], R=[zt], W=[xm_scr])
            xb = [k.sbuf([128, NCH, 512], F32, "m1x", sc) for _ in range(2)]
            hT = k.sbuf([128, NCH, 512], BF16, "m1h", sc)
            sq = k.sbuf([128, NCH, 512], BF16, "m1sq", sc)
            rstd = k.sbuf([128, 512], F32, "m1rstd", sc)
            wt = [k.sbuf([128, NCH, 512], BF16, "m1w", sc) for _ in range(2)]
            xmo = k.sbuf([128, NI, 512], BF16, "m1xm", sc)
            zo = k.sbuf([128, NI, 512], BF16, "m1z", sc)
            it = 0
            for tb in range(ntok // 512):
                s_, off = (tb * 512) // T, (tb * 512) % T
                x = xb[tb % 2]
                k.dma("sp", x[:], self.xT[:, :, tb * 512:(tb + 1) * 512], R=[self.xT], W=[x])
                self.rmsnorm([(x, x[:, c, :]) for c in range(NCH)], (gcol, gcol[:, :]),
                             [(hT, hT[:, c, :]) for c in range(NCH)], 512, sq, rstd)
                for og in range(8):
                    w = wt[it % 2]
                    it += 1
                    k.dma("sp", w[:], wup_s[og], R=[wup_s], W=[w])
                    for jj in range(4):
                        ps = self.bank()
                        for c in range(NCH):
                            k.op("pe", lambda e, c=c, jj=jj, ps=ps, w=w: e.matmul(
                                ps[:], lhsT=w[:, c, jj * 128:(jj + 1) * 128], rhs=hT[:, c, :],
                                start=(c == 0), stop=(c == NCH - 1)), R=[w, hT], W=[ps])
                        if og < 4:
                            k.op("act", lambda e, ps=ps, i=og * 4 + jj: e.copy(out=xmo[:, i, :], in_=ps[:]),
                                 R=[ps], W=[xmo])
                        else:
                            k.op("act", lambda e, ps=ps, i=(og - 4) * 4 + jj: e.activation(
                                out=zo[:, i, :], in_=ps[:], func=AF.Silu), R=[ps], W=[zo])
                k.dma("sp", xm_scr[:, :, s_, 4 + off:4 + off + 512], xmo[:], R=[xmo], W=[xm_scr])
                k.dma("sp", zs_scr[:, :, tb * 512:(tb + 1) * 512], zo[:], R=[zo], W=[zs_scr])
        k.barrier()
        if self.upto == "M1":
            return
        with contextlib.ExitStack() as sc:
            bd = k.sbuf([128, 48, 128], BF16, "bd", sc)
            wqt = k.sbuf([128, 48, 4], F32, "wqt", sc)
            bdm = k.sbuf([128, 32], F32, "bdm", sc)
            cbm = self.inp("c_bdmask", [128, 32])
            k.dma("sp", bdm[:], cbm[:], R=[cbm], W=[bdm])
            for wi, wn in enumerate(["c_wq", "c_wk", "c_wv"]):
                for c_ in range(16):
                    k.dma("sp", wqt[:, wi * 16 + c_, :],
                          W[wn].t[0, c_ * 32:(c_ + 1) * 32].rearrange("g i j -> (g i) j"), R=[W[wn]], W=[wqt])
            k.op("dve", lambda e: e.memset(bd[:], 0.0), W=[bd])
            for ci in range(48):
                for j in range(4):
                    k.op("dve", lambda e, ci=ci, j=j: e.tensor_scalar(
                        out=bd[:, ci, :].rearrange("p (g j) -> p g j", j=4)[:, :, j], in0=bdm[:],
                        scalar1=wqt[:, ci, j:j + 1], scalar2=None, op0=ALU.mult), R=[bdm, wqt], W=[bd])
            wif = k.sbuf([128, 48, 8], BF16, "wif", sc)
            for i3_ in range(3):
                k.dma("pool", wif[:, i3_ * 16:(i3_ + 1) * 16, :],
                      W["c_w_if"].t[0, i3_ * 2048:(i3_ + 1) * 2048].rearrange("(c p) n -> p c n", p=128),
                      R=[W["c_w_if"]], W=[wif])
            brow = k.sbuf([1, 8], F32, "brow", sc)
            k.dma("sp", brow[0:1, 0:4], W["c_b_i"].t[0:1, :], R=[W["c_b_i"]], W=[brow])
            k.dma("sp", brow[0:1, 4:8], W["c_b_f"].t[0:1, :], R=[W["c_b_f"]], W=[brow])
            onesf = k.sbuf([1, 128], F32, "onesf", sc)
            k.op("dve", lambda e: e.memset(onesf[:], 1.0), W=[onesf])
            cw = k.sbuf([128, NI, 4], F32, "cw", sc)
            for j_ in range(4):
                k.dma("sp", cw[:, :, j_], W["c_conv_w"].t[0, j_, 0].rearrange("(c p) -> p c", p=128),
                      R=[W["c_conv_w"]], W=[cw], allow_slow_non_contiguous=True)
            cb = k.sbuf([128, NI], F32, "cb", sc)
            k.dma("sp", cb[:], W["c_conv_b"].t[0].rearrange("(c p) -> p c", p=128),
                  R=[W["c_conv_b"]], W=[cb], allow_slow_non_contiguous=True)
            xmh = k.sbuf([128, NI, 516], BF16, "xmh", sc)
            acc = [k.sbuf([128, 512], F32, "acc", sc) for _ in range(2)]
            xc = k.sbuf([128, NI, 512], BF16, "xc", sc)
            qT = k.sbuf([128, NI, 512], BF16, "qT", sc)
            qs = k.sbuf([128, NI, 512], BF16, "qs", sc)
            kT = k.sbuf([128, NI, 512], BF16, "kT", sc)
            vT = k.sbuf([128, NI, 512], BF16, "vT", sc)
            Kt = k.sbuf([128, 2048], BF16, "Kt", sc)
            Vt = k.sbuf([128, 2048], BF16, "Vt", sc)
            gt = k.sbuf([128, 8], F32, "gt", sc)
            for tb in range(ntok // 512 if 'm2loop' not in self.skip else 0):
                s_, off = (tb * 512) // T, (tb * 512) % T
                k.dma("sp", xmh[:], xm_scr[:, :, s_, off:off + 516], R=[xm_scr], W=[xmh])
                for c in range(NI):
                    a = acc[c % 2]
                    k.op("dve", lambda e, c=c, a=a: e.tensor_scalar(
                        out=a[:], in0=xmh[:, c, 1:513], scalar1=cw[:, c, 0:1], scalar2=None, op0=ALU.mult),
                        R=[xmh, cw], W=[a])
                    for j in range(1, 4):
                        k.op("dve", lambda e, c=c, a=a, j=j: e.scalar_tensor_tensor(
                            out=a[:], in0=xmh[:, c, 1 + j:1 + j + 512], scalar=cw[:, c, j:j + 1], in1=a[:],
                            op0=ALU.mult, op1=ALU.add), R=[xmh, cw, a], W=[a])
                    k.op("act", lambda e, c=c, a=a: e.activation(
                        out=xc[:, c, :], in_=a[:], func=AF.Silu, bias=cb[:, c:c + 1], scale=1.0),
                        R=[a, cb], W=[xc])
                k.dma("sp", xc_scr[:, :, tb * 512:(tb + 1) * 512], xc[:], R=[xc], W=[xc_scr])
                if 'm2qkv' in self.skip:
                    continue
                for c in range(NI):
                    ps = self.bank()
                    k.op("pe", lambda e, c=c, ps=ps: e.matmul(ps[:], lhsT=bd[:, c, :], rhs=xc[:, c, :],
                                                             start=True, stop=True), R=[bd, xc], W=[ps])
                    k.op("act", lambda e, c=c, ps=ps: e.copy(out=qT[:, c, :], in_=ps[:]), R=[ps], W=[qT])
                    if 'm2qs' not in self.skip:
                        k.op("pool", lambda e, c=c: e.tensor_scalar(
                            out=qs[:, c, :], in0=qT[:, c, :], scalar1=QS, scalar2=None, op0=ALU.mult), R=[qT], W=[qs])
                    ps = self.bank()
                    k.op("pe", lambda e, c=c, ps=ps: e.matmul(ps[:], lhsT=bd[:, 16 + c, :], rhs=xc[:, c, :],
                                                             start=True, stop=True), R=[bd, xc], W=[ps])
                    k.op("act", lambda e, c=c, ps=ps: e.copy(out=kT[:, c, :], in_=ps[:]), R=[ps], W=[kT])
                    ps = self.bank()
                    k.op("pe", lambda e, c=c, ps=ps: e.matmul(ps[:], lhsT=bd[:, 32 + c, :],
                                                             rhs=(xc[:, c, :] if 'm2v' in self.skip else xmh[:, c, 4:516]),
                                                             start=True, stop=True), R=[bd, xmh], W=[ps])
                    k.op("dve", lambda e, c=c, ps=ps: e.tensor_copy(out=vT[:, c, :], in_=ps[:]), R=[ps], W=[vT])
                k.dma("sp", q_scr[:, :, tb * 512:(tb + 1) * 512], qs[:], R=[qs], W=[q_scr])
                k.dma("sp", k_scr[:, :, tb * 512:(tb + 1) * 512], kT[:], R=[kT], W=[k_scr])
                if 'm2tok' in self.skip:
                    continue
                for tt in range(4):
                    tsl = slice(tt * 128, (tt + 1) * 128)
                    for which, dst in ((0, Kt), (1, Vt)):
                        for b4 in range(4):
                            ps = self.bank()
                            for cc in range(4):
                                c = b4 * 4 + cc
                                if which == 0:
                                    k.op("pe", lambda e, c=c, cc=cc, ps=ps: e.matmul(
                                        ps[:, cc * 128:(cc + 1) * 128], lhsT=xc[:, c, tsl], rhs=bd[:, 16 + c, :],
                                        start=True, stop=True), R=[xc, bd], W=[ps])
                                else:
                                    k.op("pe", lambda e, c=c, cc=cc, ps=ps: e.matmul(
                                        ps[:, cc * 128:(cc + 1) * 128],
                                        lhsT=xmh[:, c, 4 + tt * 128:4 + (tt + 1) * 128], rhs=bd[:, 32 + c, :],
                                        start=True, stop=True), R=[xmh, bd], W=[ps])
                            en = "act" if b4 % 2 else "dve"
                            if en == "act":
                                k.op("act", lambda e, ps=ps, b4=b4, dst=dst: e.copy(
                                    out=dst[:, b4 * 512:(b4 + 1) * 512], in_=ps[:]), R=[ps], W=[dst])
                            else:
                                k.op("dve", lambda e, ps=ps, b4=b4, dst=dst: e.tensor_copy(
                                    out=dst[:, b4 * 512:(b4 + 1) * 512], in_=ps[:]), R=[ps], W=[dst])
                    ps = self.bank()
                    for i3, src in enumerate((qT, kT, vT)):
                        for c in range(NI):
                            k.op("pe", lambda e, c=c, i3=i3, src=src, ps=ps: e.matmul(
                                ps[:, 0:8], lhsT=src[:, c, tsl], rhs=wif[:, i3 * 16 + c, :],
                                start=(i3 == 0 and c == 0), stop=False), R=[src, wif], W=[ps])
                    if 'bias' not in self.skip:
                        k.op("pe", lambda e, ps=ps: e.matmul(ps[:, 0:8], lhsT=onesf[0:1, :], rhs=brow[0:1, :],
                                                            start=False, stop=True), R=[onesf, brow], W=[ps])
                    k.op("act", lambda e, ps=ps: e.copy(out=gt[:], in_=ps[:, 0:8]), R=[ps], W=[gt])
                    r0 = tb * 512 + tt * 128
                    k.dma("sp", Kt_scr[r0:r0 + 128, :], Kt[:], R=[Kt], W=[Kt_scr])
                    k.dma("sp", Vt_scr[r0:r0 + 128, :], Vt[:], R=[Vt], W=[Vt_scr])
                    k.dma("sp", g_scr[r0:r0 + 128, :], gt[:], R=[gt], W=[g_scr])
        k.barrier()
        if self.upto == "M2":
            return
        with contextlib.ExitStack() as sc:
            L = CHUNK
            C = k.sbuf([128, NI, 512], F32, "C", sc)
            Cb = k.sbuf([128, NI, 512], BF16, "Cb", sc)
            nv = k.sbuf([128, NI], F32, "nv", sc)
            nvb = k.sbuf([128, NI], BF16, "nvb", sc)
            mbc = k.sbuf([128, 4], F32, "mbc", sc)
            tri = k.sbuf([64, 64], F32, "tri", sc)
            sel = k.sbuf([64, 128], F32, "sel", sc)
            one64 = k.sbuf([64, 64], F32, "one64", sc)
            mneg = k.sbuf([64, 64], F32, "mneg", sc)
            oneb = k.sbuf([64, 1], BF16, "oneb", sc)
            cone = k.sbuf([128, 1], F32, "cone", sc)
            for nm, t_, shp in (("c_tri", tri, [64, 64]), ("c_sel", sel, [64, 128]), ("c_mneg", mneg, [64, 64])):
                ci_ = self.inp(nm, shp)
                k.dma("sp", t_[:], ci_[:], R=[ci_], W=[t_])
            k.op("dve", lambda e: e.memset(one64[:], 1.0), W=[one64])
            k.op("dve", lambda e: e.memset(oneb[:], 1.0), W=[oneb])
            k.op("dve", lambda e: e.memset(cone[:], 1.0), W=[cone])
            qsb = k.sbuf([128, NI, 512], BF16, "qsb", sc)
            kTb = k.sbuf([128, NI, 512], BF16, "kTb", sc)
            Ktc = [k.sbuf([64, 2048], BF16, "Ktc", sc) for _ in range(2)]
            Vtc = [k.sbuf([64, 2048], BF16, "Vtc", sc) for _ in range(2)]
            gtc = [k.sbuf([64, 8], F32, "gtc", sc) for _ in range(2)]
            sm = {n_: k.sbuf([128, 4], F32, n_, sc) for n_ in
                  ("l", "b", "bL", "bm", "cv", "mt", "negm", "scv", "emt", "mnew", "ws", "dc", "tmp")}
            dmat = [k.sbuf([64, 64], F32, f"dmat{h}", sc) for h in range(4)]
            diagc = k.sbuf([64, 64], F32, "diagc", sc)
            wts = k.sbuf([64, 64], F32, "wts", sc)
            qkw = k.sbuf([64, 64], F32, "qkw", sc)
            qkwT = k.sbuf([64, 64], BF16, "qkwT", sc)
            Asb = k.sbuf([64, 512], F32, "Asb", sc)
            hh = k.sbuf([64, 512], F32, "hh", sc)
            junk = k.sbuf([64, 512], F32, "junk", sc)
            Kw = k.sbuf([64, 512], BF16, "Kw", sc)
            st4 = k.sbuf([64, 8], F32, "st4", sc)
            yt = [k.sbuf([64, 2048], F32, "yt", sc) for _ in range(2)]
            hb = [(k.sbuf([64, 64], F32, "wts", sc), k.sbuf([64, 64], F32, "qkw", sc), k.sbuf([64, 64], BF16, "qkwT", sc),
                   k.sbuf([64, 512], F32, "Asb", sc), k.sbuf([64, 512], F32, "hh", sc), k.sbuf([64, 512], F32, "junk", sc),
                   k.sbuf([64, 512], BF16, "Kw", sc), k.sbuf([64, 8], F32, "st4", sc)) for _ in range(2)]
            for s_ in range(nseq):
                k.op("dve", lambda e: e.memset(C[:], 0.0), W=[C])
                k.op("dve", lambda e: e.memset(Cb[:], 0.0), W=[Cb])
                k.op("dve", lambda e: e.memset(nv[:], 0.0), W=[nv])
                k.op("dve", lambda e: e.memset(nvb[:], 0.0), W=[nvb])
                k.op("dve", lambda e: e.memset(mbc[:], 0.0), W=[mbc])
                for ch in range(T // L):
                    t0 = s_ * T + ch * L
                    if (ch * L) % 512 == 0:
                        k.dma("sp", qsb[:], q_scr[:, :, t0:t0 + 512], R=[q_scr], W=[qsb])
                        k.dma("sp", kTb[:], k_scr[:, :, t0:t0 + 512], R=[k_scr], W=[kTb])
                    csl = slice((ch * L) % 512, (ch * L) % 512 + L)
                    Kc, Vc, gc, yo = Ktc[ch % 2], Vtc[ch % 2], gtc[ch % 2], yt[ch % 2]
                    k.dma("sp", Kc[:], Kt_scr[t0:t0 + L, :], R=[Kt_scr], W=[Kc])
                    k.dma("sp", Vc[:], Vt_scr[t0:t0 + L, :], R=[Vt_scr], W=[Vc])
                    k.dma("sp", gc[:], g_scr[t0:t0 + L, :], R=[g_scr], W=[gc])
                    S = sm
                    k.op("act", lambda e: e.activation(out=S["tmp"][0:64, :], in_=gc[:, 4:8], func=AF.Exp, scale=-1.0),
                         R=[gc], W=[S["tmp"]])
                    k.op("act", lambda e: e.activation(out=S["l"][0:64, :], in_=S["tmp"][0:64, :], func=AF.Ln,
                                                       bias=cone[0:64, 0:1], scale=1.0), R=[S["tmp"], cone], W=[S["l"]])
                    ps = self.bank()
                    k.op("pe", lambda e, ps=ps: e.matmul(ps[0:64, 0:4], lhsT=tri[:], rhs=S["l"][0:64, :],
                                                        start=True, stop=True), R=[tri, S["l"]], W=[ps])
                    k.op("dve", lambda e, ps=ps: e.tensor_scalar(out=S["b"][0:64, :], in0=ps[0:64, 0:4], scalar1=-1.0,
                                                                scalar2=None, op0=ALU.mult), R=[ps], W=[S["b"]])
                    ps = self.bank()
                    k.op("pe", lambda e, ps=ps: e.matmul(ps[:, 0:4], lhsT=sel[:], rhs=S["b"][0:64, :],
                                                        start=True, stop=True), R=[sel, S["b"]], W=[ps])
                    k.op("dve", lambda e, ps=ps: e.tensor_copy(out=S["bL"][:], in_=ps[:, 0:4]), R=[ps], W=[S["bL"]])
                    k.op("dve", lambda e: e.tensor_tensor(out=S["bm"][0:64, :], in0=S["b"][0:64, :], in1=mbc[0:64, :],
                                                          op=ALU.add), R=[S["b"], mbc], W=[S["bm"]])
                    k.op("dve", lambda e: e.tensor_tensor(out=S["cv"][0:64, :], in0=gc[:, 0:4], in1=S["b"][0:64, :],
                                                          op=ALU.subtract), R=[gc, S["b"]], W=[S["cv"]])
                    for h in range(4):
                        k.op("dve", lambda e, h=h: e.tensor_scalar(
                            out=diagc[:], in0=self.c_ident_f[0:64, 0:64], scalar1=S["cv"][0:64, h:h + 1], scalar2=None,
                            op0=ALU.mult), R=[self.c_ident_f, S["cv"]], W=[diagc])
                        ps = self.bank()
                        k.op("pe", lambda e, ps=ps: e.matmul(ps[0:64, 0:64], lhsT=one64[:], rhs=diagc[:],
                                                            start=True, stop=True), R=[one64, diagc], W=[ps])
                        k.op("dve", lambda e, ps=ps, h=h: e.scalar_tensor_tensor(
                            out=dmat[h][:], in0=ps[0:64, 0:64], scalar=S["b"][0:64, h:h + 1], in1=mneg[:],
                            op0=ALU.add, op1=ALU.add), R=[ps, S["b"], mneg], W=[dmat[h]])
                        k.op("dve", lambda e, h=h: e.reduce_max(out=S["tmp"][0:64, h:h + 1], in_=dmat[h][:], axis=AX.X),
                             R=[dmat[h]], W=[S["tmp"]])
                    k.op("dve", lambda e: e.tensor_tensor(out=S["mt"][0:64, :], in0=S["tmp"][0:64, :], in1=S["bm"][0:64, :],
                                                          op=ALU.max), R=[S["tmp"], S["bm"]], W=[S["mt"]])
                    k.op("dve", lambda e: e.tensor_scalar(out=S["negm"][0:64, :], in0=S["mt"][0:64, :], scalar1=-1.0,
                                                          scalar2=None, op0=ALU.mult), R=[S["mt"]], W=[S["negm"]])
                    k.op("dve", lambda e: e.tensor_tensor(out=S["scv"][0:64, :], in0=S["bm"][0:64, :], in1=S["mt"][0:64, :],
                                                          op=ALU.subtract), R=[S["bm"], S["mt"]], W=[S["scv"]])
                    k.op("act", lambda e: e.activation(out=S["scv"][0:64, :], in_=S["scv"][0:64, :], func=AF.Exp),
                         R=[S["scv"]], W=[S["scv"]])
                    k.op("act", lambda e: e.activation(out=S["emt"][0:64, :], in_=S["negm"][0:64, :], func=AF.Exp),
                         R=[S["negm"]], W=[S["emt"]])
                    ps = self.bank()
                    k.op("pe", lambda e, ps=ps: e.matmul(ps[:, 0:4], lhsT=sel[:], rhs=S["mt"][0:64, :],
                                                        start=True, stop=True), R=[sel, S["mt"]], W=[ps])
                    k.op("dve", lambda e, ps=ps: e.tensor_copy(out=S["mnew"][:], in_=ps[:, 0:4]), R=[ps], W=[S["mnew"]])
                    k.op("dve", lambda e: e.tensor_tensor(out=S["ws"][0:64, :], in0=S["bL"][0:64, :], in1=S["cv"][0:64, :],
                                                          op=ALU.add), R=[S["bL"], S["cv"]], W=[S["ws"]])
                    k.op("dve", lambda e: e.tensor_tensor(out=S["ws"][0:64, :], in0=S["ws"][0:64, :], in1=S["mnew"][0:64, :],
                                                          op=ALU.subtract), R=[S["ws"], S["mnew"]], W=[S["ws"]])
                    k.op("act", lambda e: e.activation(out=S["ws"][0:64, :], in_=S["ws"][0:64, :], func=AF.Exp),
                         R=[S["ws"]], W=[S["ws"]])
                    k.op("dve", lambda e: e.tensor_tensor(out=S["dc"][:], in0=S["bL"][:], in1=mbc[:], op=ALU.add),
                         R=[S["bL"], mbc], W=[S["dc"]])
                    k.op("dve", lambda e: e.tensor_tensor(out=S["dc"][:], in0=S["dc"][:], in1=S["mnew"][:],
                                                          op=ALU.subtract), R=[S["dc"], S["mnew"]], W=[S["dc"]])
                    k.op("act", lambda e: e.activation(out=S["dc"][:], in_=S["dc"][:], func=AF.Exp),
                         R=[S["dc"]], W=[S["dc"]])
                    k.op("dve", lambda e: e.tensor_copy(out=mbc[:], in_=S["mnew"][:]), R=[S["mnew"]], W=[mbc])
                    def head_gen(h, slot):
                        wts, qkw, qkwT, Asb, hh, junk, Kw, st4 = hb[slot]
                        hs = slice(h * 512, (h + 1) * 512)
                        k.op("act", lambda e, h=h: e.activation(out=wts[:], in_=dmat[h][:], func=AF.Exp,
                                                                bias=S["negm"][0:64, h:h + 1], scale=1.0),
                             R=[dmat[h], S["negm"]], W=[wts])
                        ps = self.bank()
                        for dcc in range(4):
                            k.op("pe", lambda e, ps=ps, dcc=dcc, h=h: e.matmul(
                                ps[0:64, 0:64], lhsT=qsb[:, h * 4 + dcc, csl], rhs=kTb[:, h * 4 + dcc, csl],
                                start=(dcc == 0), stop=(dcc == 3)), R=[qsb, kTb], W=[ps])
                        yield
                        k.op("dve", lambda e, ps=ps: e.tensor_tensor(out=qkw[:], in0=wts[:], in1=ps[0:64, 0:64],
                                                                     op=ALU.mult), R=[wts, ps], W=[qkw])
                        k.op("dve", lambda e: e.reduce_sum(out=st4[:, 0:1], in_=qkw[:], axis=AX.X), R=[qkw], W=[st4])
                        yield
                        ps = self.bank()
                        k.op("pe", lambda e, ps=ps: e.transpose(ps[0:64, 0:64], qkw[:], self.c_ident_f[0:64, 0:64]),
                             R=[qkw, self.c_ident_f], W=[ps])
                        k.op("act", lambda e, ps=ps: e.copy(out=qkwT[:], in_=ps[0:64, 0:64]), R=[ps], W=[qkwT])
                        yield
                        psA = self.bank()
                        k.op("pe", lambda e, psA=psA, hs=hs: e.matmul(psA[0:64, :], lhsT=qkwT[:], rhs=Vc[:, hs],
                                                                      start=True, stop=True), R=[qkwT, Vc], W=[psA])
                        psB = self.bank()
                        for dcc in range(4):
                            k.op("pe", lambda e, psB=psB, dcc=dcc, h=h: e.matmul(
                                psB[0:64, :], lhsT=qsb[:, h * 4 + dcc, csl], rhs=Cb[:, h * 4 + dcc, :],
                                start=(dcc == 0), stop=(dcc == 3)), R=[qsb, Cb], W=[psB])
                        psn = self.bank()
                        for dcc in range(4):
                            k.op("pe", lambda e, psn=psn, dcc=dcc, h=h: e.matmul(
                                psn[0:64, 0:1], lhsT=qsb[:, h * 4 + dcc, csl], rhs=nvb[:, h * 4 + dcc:h * 4 + dcc + 1],
                                start=(dcc == 0), stop=(dcc == 3)), R=[qsb, nvb], W=[psn])
                        yield
                        k.op("act", lambda e, psA=psA: e.copy(out=Asb[:], in_=psA[0:64, :]), R=[psA], W=[Asb])
                        k.op("dve", lambda e, psB=psB, h=h: e.scalar_tensor_tensor(
                            out=hh[:], in0=psB[0:64, :], scalar=S["scv"][0:64, h:h + 1], in1=Asb[:],
                            op0=ALU.mult, op1=ALU.add), R=[psB, S["scv"], Asb], W=[hh])
                        k.op("dve", lambda e, psn=psn, h=h: e.scalar_tensor_tensor(
                            out=st4[:, 1:2], in0=psn[0:64, 0:1], scalar=S["scv"][0:64, h:h + 1], in1=st4[:, 0:1],
                            op0=ALU.mult, op1=ALU.add), R=[psn, S["scv"], st4], W=[st4])
                        k.op("dve", lambda e, h=h: e.tensor_scalar(
                            out=st4[:, 2:3], in0=st4[:, 1:2], scalar1=-1.0, scalar2=S["emt"][0:64, h:h + 1],
                            op0=ALU.mult, op1=ALU.max), R=[st4, S["emt"]], W=[st4])
                        k.op("dve", lambda e, h=h: e.tensor_tensor(
                            out=st4[:, 2:3], in0=st4[:, 2:3], in1=st4[:, 1:2], op=ALU.max), R=[st4], W=[st4])
                        k.op("dve", lambda e: e.reciprocal(out=st4[:, 3:4], in_=st4[:, 2:3]), R=[st4], W=[st4])
                        k.op("dve", lambda e: e.tensor_scalar(out=hh[:], in0=hh[:], scalar1=st4[:, 3:4], scalar2=None,
                                                              op0=ALU.mult), R=[hh, st4], W=[hh])
                        k.op("dve", lambda e: e.reduce_sum(out=st4[:, 4:5], in_=hh[:], axis=AX.X), R=[hh], W=[st4])
                        k.op("dve", lambda e: e.tensor_scalar(out=st4[:, 4:5], in0=st4[:, 4:5], scalar1=-1.0 / 512,
                                                              scalar2=None, op0=ALU.mult), R=[st4], W=[st4])
                        k.op("dve", lambda e: e.tensor_scalar(out=hh[:], in0=hh[:], scalar1=st4[:, 4:5], scalar2=None,
                                                              op0=ALU.add), R=[hh, st4], W=[hh])
                        k.op("dve", lambda e: e.tensor_tensor(out=junk[:], in0=hh[:], in1=hh[:], op=ALU.mult),
                             R=[hh], W=[junk])
                        k.op("dve", lambda e: e.reduce_sum(out=st4[:, 5:6], in_=junk[:], axis=AX.X), R=[junk], W=[st4])
                        k.op("dve", lambda e: e.tensor_scalar(out=st4[:, 5:6], in0=st4[:, 5:6], scalar1=1.0 / 512,
                                                              scalar2=1e-5, op0=ALU.mult, op1=ALU.add), R=[st4], W=[st4])
                        k.op("act", lambda e: e.activation(out=st4[:, 6:7], in_=st4[:, 5:6], func=AF.Sqrt), R=[st4], W=[st4])
                        k.op("dve", lambda e: e.reciprocal(out=st4[:, 7:8], in_=st4[:, 6:7]), R=[st4], W=[st4])
                        k.op("dve", lambda e, hs=hs, yo=yo: e.tensor_scalar(
                            out=yo[:, hs], in0=hh[:], scalar1=st4[:, 7:8], scalar2=None, op0=ALU.mult),
                            R=[hh, st4], W=[yo])
                        yield
                        k.op("dve", lambda e, h=h, hs=hs: e.tensor_scalar(
                            out=Kw[:], in0=Kc[:, hs], scalar1=S["ws"][0:64, h:h + 1], scalar2=None, op0=ALU.mult),
                            R=[Kc, S["ws"]], W=[Kw])
                        for dcc in range(4):
                            psU = self.bank()
                            k.op("pe", lambda e, psU=psU, dcc=dcc, hs=hs: e.matmul(
                                psU[:], lhsT=Kw[:, dcc * 128:(dcc + 1) * 128], rhs=Vc[:, hs], start=True, stop=True),
                                R=[Kw, Vc], W=[psU])
                            i_ = h * 4 + dcc
                            k.op("dve", lambda e, psU=psU, i_=i_, h=h: e.scalar_tensor_tensor(
                                out=C[:, i_, :], in0=C[:, i_, :], scalar=S["dc"][:, h:h + 1], in1=psU[:],
                                op0=ALU.mult, op1=ALU.add), R=[C, S["dc"], psU], W=[C])
                            k.op("act", lambda e, i_=i_: e.copy(out=Cb[:, i_, :], in_=C[:, i_, :]), R=[C], W=[Cb])
                            yield
                        psn2 = self.bank()
                        for dcc in range(4):
                            k.op("pe", lambda e, psn2=psn2, dcc=dcc: e.matmul(
                                psn2[:, dcc:dcc + 1], lhsT=Kw[:, dcc * 128:(dcc + 1) * 128], rhs=oneb[:],
                                start=True, stop=True), R=[Kw, oneb], W=[psn2])
                        k.op("dve", lambda e, psn2=psn2, h=h: e.scalar_tensor_tensor(
                            out=nv[:, h * 4:h * 4 + 4], in0=nv[:, h * 4:h * 4 + 4], scalar=S["dc"][:, h:h + 1],
                            in1=psn2[:, 0:4], op0=ALU.mult, op1=ALU.add), R=[nv, S["dc"], psn2], W=[nv])
                        k.op("act", lambda e, h=h: e.copy(out=nvb[:, h * 4:h * 4 + 4], in_=nv[:, h * 4:h * 4 + 4]),
                             R=[nv], W=[nvb])
                        yield
                    for pair_ in ((0, 1), (2, 3)):
                        gens_ = [head_gen(h_, i_) for i_, h_ in enumerate(pair_)]
                        live_ = list(gens_)
                        while live_:
                            nxt_ = []
                            for g_ in live_:
                                try:
                                    next(g_)
                                    nxt_.append(g_)
                                except StopIteration:
                                    pass
                            live_ = nxt_
                    k.dma("sp", y_scr[t0:t0 + L, :], yo[:], R=[yo], W=[y_scr])
        k.barrier()
        if self.upto == "M3":
            return
        with contextlib.ExitStack() as sc:
            wdn = k.sbuf([128, NI, D], BF16, "wdn", sc)
            k.dma("pool", wdn[:], W["c_w_down"].t[0].rearrange("(c p) n -> p c n", p=128), R=[W["c_w_down"]], W=[wdn])
            mhg = k.sbuf([128, NI], F32, "mhg", sc)
            skp = k.sbuf([128, NI], F32, "skp", sc)
            k.dma("sp", mhg[:], W["c_mh_g"].t[0].rearrange("(c p) -> p c", p=128), R=[W["c_mh_g"]], W=[mhg],
                  allow_slow_non_contiguous=True)
            k.dma("sp", skp[:], W["c_skip"].t[0].rearrange("(c p) -> p c", p=128), R=[W["c_skip"]], W=[skp],
                  allow_slow_non_contiguous=True)
            ytk = k.sbuf([128, 4, 2048], F32, "ytk", sc)
            xcb = k.sbuf([128, NI, 512], BF16, "xcb", sc)
            zsb = k.sbuf([128, NI, 512], BF16, "zsb", sc)
            yz = k.sbuf([128, NI, 512], BF16, "yz", sc)
            t1 = [k.sbuf([128, 512], F32, "t1", sc) for _ in range(2)]
            x = k.sbuf([128, NCH, 512], F32, "m4x", sc)
            for tb in range(ntok // 512):
                k.dma("sp", ytk[:], y_scr[tb * 512:(tb + 1) * 512, :].rearrange("(j p) n -> p j n", p=128),
                      R=[y_scr], W=[ytk])
                k.dma("sp", xcb[:], xc_scr[:, :, tb * 512:(tb + 1) * 512], R=[xc_scr], W=[xcb])
                k.dma("sp", zsb[:], zs_scr[:, :, tb * 512:(tb + 1) * 512], R=[zs_scr], W=[zsb])
                k.dma("sp", x[:], self.xT[:, :, tb * 512:(tb + 1) * 512], R=[self.xT], W=[x])
                for c in range(NI):
                    ps = self.bank()
                    for j in range(4):
                        k.op("pe", lambda e, c=c, j=j, ps=ps: e.transpose(
                            ps[:, j * 128:(j + 1) * 128], ytk[:, j, c * 128:(c + 1) * 128], self.c_ident_f[:]),
                            R=[ytk, self.c_ident_f], W=[ps])
                    t = t1[c % 2]
                    k.op("dve", lambda e, c=c, t=t: e.tensor_scalar(
                        out=t[:], in0=xcb[:, c, :], scalar1=skp[:, c:c + 1], scalar2=None, op0=ALU.mult),
                        R=[xcb, skp], W=[t])
                    k.op("dve", lambda e, c=c, t=t, ps=ps: e.scalar_tensor_tensor(
                        out=t[:], in0=ps[:], scalar=mhg[:, c:c + 1], in1=t[:], op0=ALU.mult, op1=ALU.add),
                        R=[ps, mhg, t], W=[t])
                    k.op("dve", lambda e, c=c, t=t: e.tensor_tensor(out=yz[:, c, :], in0=t[:], in1=zsb[:, c, :],
                                                                   op=ALU.mult), R=[t, zsb], W=[yz])
                for n in range(NCH):
                    po = self.bank()
                    for c in range(NI):
                        k.op("pe", lambda e, n=n, c=c, po=po: e.matmul(
                            po[:], lhsT=wdn[:, c, n * 128:(n + 1) * 128], rhs=yz[:, c, :],
                            start=(c == 0), stop=(c == NI - 1)), R=[wdn, yz], W=[po])
                    k.op("dve", lambda e, n=n, po=po: e.tensor_tensor(
                        out=x[:, n, :], in0=x[:, n, :], in1=po[:], op=ALU.add), R=[po, x], W=[x])
                k.dma("sp", self.xT[:, :, tb * 512:(tb + 1) * 512], x[:], R=[x], W=[self.xT])
        k.barrier()

    def build(self):
        ntok = self.ntok
        W = {}
        x_in = self.inp("x", [ntok, D])
        W["p"] = self.inp("p", [2, ntok, PLE_DIM])
        for name, shape in WEIGHT_SHAPES.items():
            W[name] = self.inp(name, shape)
        yt = self.nc.dram_tensor("y", [ntok, D], F32, kind="ExternalOutput")
        y_out = Buf(yt.ap(), "y")
        self.setup_consts()
        self.phase_in(x_in)
        for li in self.layers:
            if "mix" in self.stages and li == 1:
                self.phase_mix1(W)
            if "mix" in self.stages and li == 0:
                self.phase_mix0(W)
            if "ffn" in self.stages:
                self.phase_ffn(li, W)
        self.phase_out(y_out)
        self.k.wait_all("sp", [y_out] + list(self.dumps.values()))
        self.k.close()
        return self.nc


WEIGHT_SHAPES = {
    "mix_norm": [2, 1024], "a_q_gain": [1, 64], "a_k_gain": [1, 64], "w_in_e": [1, 1024, 2756],
    "b_mu": [1, 1792], "b_w0": [1, 512], "b_w2": [1, 64, 512], "b_a0": [1, 512], "b_a2": [1, 64, 512],
    "b_g2": [1, 128, 512], "b_k_k": [1, 512], "b_k_a": [1, 512], "b_r_k": [1, 8, 64], "b_gn_g": [1, 512],
    "b_gn_b": [1, 512], "w_out_e": [1, 1024, 1024], "c_w_up": [1, 1024, 4096], "c_conv_w": [1, 4, 1, 2048],
    "c_conv_b": [1, 2048], "c_wq": [1, 512, 4, 4], "c_wk": [1, 512, 4, 4], "c_wv": [1, 512, 4, 4],
    "c_w_if": [1, 6144, 8], "c_b_i": [1, 4], "c_b_f": [1, 4], "c_mh_g": [1, 2048], "c_skip": [1, 2048],
    "c_w_down": [1, 2048, 1024], "ffn_norm": [2, 1024], "ffn_w_gate": [2, 1024, 2816],
    "ffn_w_up": [2, 1024, 2816], "ffn_w_down": [2, 2816, 1024], "ple_w": [2, 256, 1024],
    "ple_norm": [2, 1024], "ple_w_gate": [2, 1024, 1024],
}


def host_consts():
    i64 = np.arange(64)
    sel = np.zeros((64, 128), np.float32)
    sel[63, :] = 1.0
    return {"c_ident": np.eye(128, dtype=np.float32),
            "c_bdmask": (np.arange(128)[:, None] // 4 == np.arange(32)[None, :]).astype(np.float32),
            "c_tri": (i64[:, None] <= i64[None, :]).astype(np.float32),
            "c_ups": (i64[:, None] < i64[None, :]).astype(np.float32),
            "c_los": (i64[:, None] > i64[None, :]).astype(np.float32),
            "c_bones": (np.arange(128)[:, None] // 64 == np.arange(128)[None, :] // 64).astype(np.float32),
            "c_bd64": ((np.arange(128)[:, None] // 64 == np.arange(128)[None, :] // 64) / 64.0).astype(np.float32),
            "c_mneg2": np.where((np.arange(128)[:, None] < 64) & (np.arange(128)[None, :] >= 64), -1e30, 0.0).astype(np.float32),
            "c_sel": sel,
            "c_mneg": np.where(i64[None, :] <= i64[:, None], 0.0, -1e30).astype(np.float32)}


def run(inputs, ntok_core, ncore=NCORE, layers=(0, 1), stages=("mix", "ffn"), trace=False, dbg=False):
    prog = Prog(ntok_core, layers=layers, stages=stages, dbg=dbg)
    nc = prog.build()
    x = np.ascontiguousarray(inputs["x"]).reshape(-1, D)
    p = np.ascontiguousarray(inputs["p"]).reshape(2, -1, PLE_DIM)
    consts = host_consts()
    in_maps = []
    for c in range(ncore):
        m = {"x": x[c * ntok_core:(c + 1) * ntok_core],
             "p": np.ascontiguousarray(p[:, c * ntok_core:(c + 1) * ntok_core])}
        for name in WEIGHT_SHAPES:
            m[name] = np.ascontiguousarray(inputs[name], dtype=np.float32)
        m.update({kk_: vv_ for kk_, vv_ in consts.items() if kk_ in prog.inputs})
        in_maps.append(m)
    res = run_bass_kernel_spmd(nc, in_maps, core_ids=list(range(ncore)), trace=trace)
    y = np.concatenate([r["y"] for r in res.results], axis=0)
    if dbg:
        res.dumps = {n: res.results[0]["dbg_" + n] for n in prog.dumps}
    return y, res


def kernel(**inputs):
    ntok_core = BATCH * SEQ // NCORE
    y, _ = run(inputs, ntok_core)
    return y.reshape(BATCH, SEQ, D).astype(np.float32)
```
